# Optimizing a Trainium2 kernel written in Bass

```python
import jax, jax.numpy as jnp
from jax import lax
import numpy as np

D_MODEL = 2048
BATCH = 8
SEQ = 4096
DEPTH = 2

GRID_W = 64
CTX_LEN = 256
N_MOD = 6
MLP_CHUNK = 128
MLP_INNER = D_MODEL
MLP_GROUPS = 16
MLP_GROUP_DIM = MLP_INNER // MLP_GROUPS
NA_HEAD_DIM = 128
NA_HEADS = D_MODEL // NA_HEAD_DIM
NA_ROWS = 8
NA_COLS = 16
NA_QB = 16
FFN_DIM = 5632
MOE_EXPERTS = 8
MOE_TOP_K = 2
MOE_DIM = 7168
MOE_BLOCK = 256
N_EVEN = (DEPTH + 1) // 2
N_ODD = DEPTH // 2
EPS = 1e-6
NEG_INF = -1e30

kernel_name = "hybrid_gmlp_natten_moe_dit"


def rms_norm(t, w):
    tf = t.astype(jnp.float32)
    y = tf * lax.rsqrt(jnp.mean(tf * tf, axis=-1, keepdims=True) + EPS)
    return (y * w.astype(jnp.float32)).astype(t.dtype)


def _modulate(t, w, shift, scale):
    return rms_norm(t, w) * (1 + scale) + shift


def swiglu(t, w1, w3, w2):
    return (jax.nn.silu(t @ w1) * (t @ w3)) @ w2


def chunk_mlp(h, w_in, g_v, w_s, b_s, w_out):
    b, l, _ = h.shape
    z = jax.nn.gelu(h @ w_in, approximate=False)
    u, v = jnp.split(z, 2, axis=-1)
    v = rms_norm(v, g_v).reshape(b, l // MLP_CHUNK, MLP_CHUNK, MLP_GROUPS, MLP_GROUP_DIM)
    v = jnp.einsum('gpq,bnqgc->bnpgc', w_s, v) + b_s.T[:, :, None]
    return (u * v.reshape(b, l, MLP_INNER)) @ w_out


def _column_blocks():
    ncb = GRID_W // NA_QB
    kwb = min(NA_QB + NA_COLS, GRID_W)
    qcol = np.arange(GRID_W).reshape(ncb, NA_QB)
    cs = np.clip(qcol - NA_COLS // 2, 0, GRID_W - NA_COLS)
    kc0 = np.clip(np.arange(ncb) * NA_QB - NA_COLS // 2, 0, GRID_W - kwb)
    colblk = kc0[:, None] + np.arange(kwb)
    kcol = colblk[:, None, :]
    inwin = (kcol >= cs[..., None]) & (kcol < cs[..., None] + NA_COLS)
    dc = np.clip(kcol - qcol[:, :, None] + NA_COLS - 1, 0, 2 * NA_COLS - 2)
    return colblk, dc, inwin, ncb, kwb


def neighbourhood_attention(h, hc, w_qkv, g_q, g_k, rpb, w_o, ctx_out):
    b, l, _ = h.shape
    n_ctx = hc.shape[1]
    rows = l // GRID_W
    kh = min(NA_ROWS, rows)
    scale = NA_HEAD_DIM ** -0.5

    def proj(t):
        qkv = (t @ w_qkv).reshape(t.shape[0], t.shape[1], 3, NA_HEADS, NA_HEAD_DIM)
        return rms_norm(qkv[:, :, 0], g_q) * scale, rms_norm(qkv[:, :, 1], g_k), qkv[:, :, 2]

    q, k, v = proj(h)
    qc, kc, vc = proj(hc)
    colblk, dc, inwin, ncb, kwb = _column_blocks()
    qg = q.reshape(b, rows, GRID_W, NA_HEADS, NA_HEAD_DIM).transpose(1, 0, 2, 3, 4)
    kg = k.reshape(b, rows, GRID_W, NA_HEADS, NA_HEAD_DIM)
    vg = v.reshape(b, rows, GRID_W, NA_HEADS, NA_HEAD_DIM)
    rpb32 = rpb.astype(jnp.float32)
    n_loc = kh * kwb

    def row_attend(args):
        q_r, r = args
        rs = jnp.clip(r - kh // 2, 0, rows - kh)
        kb = lax.dynamic_slice_in_dim(kg, rs, kh, axis=1)[:, :, colblk]
        vb = lax.dynamic_slice_in_dim(vg, rs, kh, axis=1)[:, :, colblk]
        qb = q_r.reshape(b, ncb, NA_QB, NA_HEADS, NA_HEAD_DIM)
        s_loc = jnp.einsum('bnqhd,binjhd->bnhqij', qb, kb).astype(jnp.float32)
        dr = rs + jnp.arange(kh) - r + (NA_ROWS - 1)
        bias = rpb32[:, dr][:, :, dc].transpose(2, 0, 3, 1, 4)
        s_loc = jnp.where(inwin[:, None, :, None, :], s_loc + bias, NEG_INF)
        s_ctx = jnp.einsum('bnqhd,bkhd->bnhqk', qb, kc).astype(jnp.float32)
        s = jnp.concatenate([s_loc.reshape(b, ncb, NA_HEADS, NA_QB, n_loc), s_ctx], axis=-1)
        p = jax.nn.softmax(s, axis=-1).astype(v.dtype)
        p_loc = p[..., :n_loc].reshape(b, ncb, NA_HEADS, NA_QB, kh, kwb)
        o = (jnp.einsum('bnhqij,binjhd->bnqhd', p_loc, vb)
             + jnp.einsum('bnhqk,bkhd->bnqhd', p[..., n_loc:], vc))
        return o.reshape(b, GRID_W, NA_HEADS, NA_HEAD_DIM)

    o = lax.map(row_attend, (qg, jnp.arange(rows)))
    y = o.transpose(1, 0, 2, 3, 4).reshape(b, l, D_MODEL) @ w_o
    yc = None
    if ctx_out:
        sc = jnp.einsum('bqhd,bkhd->bhqk', qc, kc).astype(jnp.float32)
        pc = jax.nn.softmax(sc, axis=-1).astype(vc.dtype)
        yc = jnp.einsum('bhqk,bkhd->bqhd', pc, vc).reshape(b, n_ctx, D_MODEL) @ w_o
    return y, yc


def moe_swiglu(t, w_router, w1, w3, w2):
    n_tok, d = t.shape
    logits = (t @ w_router).astype(jnp.float32)
    top_v, top_i = lax.top_k(logits, MOE_TOP_K)
    gates = jax.nn.softmax(top_v, axis=-1)
    n_asg = n_tok * MOE_TOP_K
    e_flat = top_i.reshape(-1)
    order = jnp.argsort(e_flat, stable=True)
    e_sorted = e_flat[order]
    tok_sorted = (order // MOE_TOP_K).astype(jnp.int32)
    gate_sorted = gates.reshape(-1)[order]
    counts = jnp.bincount(e_flat, length=MOE_EXPERTS)
    padded = (counts + MOE_BLOCK - 1) // MOE_BLOCK * MOE_BLOCK
    pad_end = jnp.cumsum(padded)
    pad_start = pad_end - padded
    grp_start = jnp.cumsum(counts) - counts
    slot = pad_start[e_sorted] + jnp.arange(n_asg) - grp_start[e_sorted]
    n_blocks = -(-n_asg // MOE_BLOCK) + MOE_EXPERTS
    slot_tok = jnp.zeros((n_blocks * MOE_BLOCK,), jnp.int32).at[slot].set(tok_sorted)
    blk_e = jnp.minimum(jnp.searchsorted(pad_end, jnp.arange(n_blocks) * MOE_BLOCK, side='right'),
                        MOE_EXPERTS - 1)
    xs = t[slot_tok].reshape(n_blocks, MOE_BLOCK, d)

    def expert_block(args):
        xb, e = args
        return swiglu(xb, w1[e], w3[e], w2[e])

    ys = lax.map(expert_block, (xs, blk_e)).reshape(n_blocks * MOE_BLOCK, d)
    return jnp.zeros_like(t).at[tok_sorted].add(ys[slot] * gate_sorted[:, None].astype(t.dtype))


def setup_inputs(seed: int = 0) -> dict:
    key = jax.random.key(seed)
    ks = jax.random.split(key, 26)
    f32 = jnp.float32

    def nrm(k, shape, s):
        return jax.random.normal(k, shape, f32) * s

    d = D_MODEL
    return {
        "x": nrm(ks[0], (BATCH, SEQ, d), 1.0),
        "c": nrm(ks[1], (BATCH, d), 1.0),
        "ctx": nrm(ks[2], (BATCH, CTX_LEN, d), 1.0),
        "c_ctx": nrm(ks[3], (d,), 1.0),
        "w_ada": nrm(ks[4], (DEPTH, d, N_MOD * d), 0.5 * d ** -0.5),
        "b_ada": nrm(ks[5], (DEPTH, N_MOD * d), 0.02),
        "norm_w": 1.0 + nrm(ks[6], (DEPTH, 2, d), 0.02),
        "mlp_w_in": nrm(ks[7], (N_EVEN, d, 2 * MLP_INNER), d ** -0.5),
        "mlp_g_v": 1.0 + nrm(ks[8], (N_EVEN, MLP_INNER), 0.02),
        "mlp_w_s": nrm(ks[9], (N_EVEN, MLP_GROUPS, MLP_CHUNK, MLP_CHUNK), MLP_CHUNK ** -0.5),
        "mlp_b_s": 1.0 + nrm(ks[10], (N_EVEN, MLP_GROUPS, MLP_CHUNK), 0.02),
        "mlp_w_out": nrm(ks[11], (N_EVEN, MLP_INNER, d), MLP_INNER ** -0.5),
        "ffn_w1": nrm(ks[12], (N_EVEN, d, FFN_DIM), d ** -0.5),
        "ffn_w3": nrm(ks[13], (N_EVEN, d, FFN_DIM), d ** -0.5),
        "ffn_w2": nrm(ks[14], (N_EVEN, FFN_DIM, d), FFN_DIM ** -0.5),
        "na_w_qkv": nrm(ks[15], (N_ODD, d, 3 * d), d ** -0.5),
        "na_g_q": 1.0 + nrm(ks[16], (N_ODD, NA_HEAD_DIM), 0.02),
        "na_g_k": 1.0 + nrm(ks[17], (N_ODD, NA_HEAD_DIM), 0.02),
        "na_rpb": nrm(ks[18], (N_ODD, NA_HEADS, 2 * NA_ROWS - 1, 2 * NA_COLS - 1), 0.1),
        "na_w_o": nrm(ks[19], (N_ODD, d, d), d ** -0.5),
        "moe_w_router": nrm(ks[20], (N_ODD, d, MOE_EXPERTS), d ** -0.5),
        "moe_w1": nrm(ks[21], (N_ODD, MOE_EXPERTS, d, MOE_DIM), d ** -0.5),
        "moe_w3": nrm(ks[22], (N_ODD, MOE_EXPERTS, d, MOE_DIM), d ** -0.5),
        "moe_w2": nrm(ks[23], (N_ODD, MOE_EXPERTS, MOE_DIM, d), MOE_DIM ** -0.5),
    }


def reference(x, c, ctx, c_ctx, w_ada, b_ada, norm_w, mlp_w_in, mlp_g_v, mlp_w_s, mlp_b_s,
              mlp_w_out, ffn_w1, ffn_w3, ffn_w2, na_w_qkv, na_g_q, na_g_k, na_rpb, na_w_o,
              moe_w_router, moe_w1, moe_w3, moe_w2):
    b, l, d = x.shape
    xc = ctx
    for i in range(DEPTH):
        j = i // 2
        ctx_out = i != DEPTH - 1
        odd = i % 2 == 1
        mod = jax.nn.silu(c) @ w_ada[i] + b_ada[i]
        sh1, sc1, g1, sh2, sc2, g2 = jnp.split(mod[:, None, :], N_MOD, axis=-1)
        if ctx_out or odd:
            mod_c = jax.nn.silu(c_ctx) @ w_ada[i] + b_ada[i]
            csh1, csc1, cg1, csh2, csc2, cg2 = jnp.split(mod_c, N_MOD, axis=-1)
            hc = _modulate(xc, norm_w[i, 0], csh1, csc1)
        h = _modulate(x, norm_w[i, 0], sh1, sc1)
        if not odd:
            y = chunk_mlp(h, mlp_w_in[j], mlp_g_v[j], mlp_w_s[j], mlp_b_s[j], mlp_w_out[j])
            yc = chunk_mlp(hc, mlp_w_in[j], mlp_g_v[j], mlp_w_s[j], mlp_b_s[j], mlp_w_out[j]) if ctx_out else None
        else:
            y, yc = neighbourhood_attention(h, hc, na_w_qkv[j], na_g_q[j], na_g_k[j], na_rpb[j],
                                            na_w_o[j], ctx_out)
        x = x + g1 * y
        toks = _modulate(x, norm_w[i, 1], sh2, sc2).reshape(b * l, d)
        if ctx_out:
            xc = xc + cg1 * yc
            toks = jnp.concatenate(
                [toks, _modulate(xc, norm_w[i, 1], csh2, csc2).reshape(-1, d)], axis=0)
        if not odd:
            f = swiglu(toks, ffn_w1[j], ffn_w3[j], ffn_w2[j])
        else:
            f = moe_swiglu(toks, moe_w_router[j], moe_w1[j], moe_w3[j], moe_w2[j])
        x = x + g2 * f[:b * l].reshape(b, l, d)
        if ctx_out:
            xc = xc + cg2 * f[b * l:].reshape(xc.shape)
    return x
```

```python
import contextlib
import numpy as np
import concourse.bass as bass
import concourse.mybir as mybir
from concourse.bass_utils import run_bass_kernel_spmd

F32 = mybir.dt.float32
BF16 = mybir.dt.bfloat16
I32 = mybir.dt.int32
AF = mybir.ActivationFunctionType
ALU = mybir.AluOpType
AX = mybir.AxisListType

D = 2048
KC = 16
SEQ = 4096
CTX = 256
FFN = 5632
NMOD = 6
EPS = 1e-6
ENGS = ("pe", "act", "dve", "pool", "sp")


class Buf:
    def __init__(self, name, t, n=1):
        self.name = name
        self.t = t
        self.n = n
        self.lw = [None] * n
        self.rd = [dict() for _ in range(n)]
        self.sem = None
        self.dmacnt = 0

    def __getitem__(self, k):
        return self.t[k]

    def r(self, lo=0, hi=None):
        return (self, lo, self.n if hi is None else hi)


class Op:
    __slots__ = ("eng", "fn", "deps", "sig", "sigval", "dma_buf", "phase")

    def __init__(self, eng, fn, dma_buf):
        self.eng = eng
        self.fn = fn
        self.deps = None
        self.sig = False
        self.sigval = None
        self.dma_buf = dma_buf


class Prog:
    def __init__(self, nc):
        self.nc = nc
        self.ops = {e: [] for e in ENGS}
        self.allops = []
        self.bufs = []
        self.pending = {e: set() for e in ENGS}
        self.since_barrier = []
        self.last = {e: None for e in ENGS}
        self.stack = contextlib.ExitStack()
        self.pstack = None
        self.banks = []
        self.bi = 0
        self.uid = 0
        self.phase_id = 0

    def init_psum(self):
        for i in range(8):
            t = self.stack.enter_context(self.nc.psum_tensor("bank%d" % i, [128, 512], F32))
            b = Buf("bank%d" % i, t, 1)
            self.bufs.append(b)
            self.banks.append(b)

    def bank(self):
        b = self.banks[self.bi % 8]
        self.bi += 1
        return b

    @contextlib.contextmanager
    def phase(self):
        self.barrier()
        self.phase_id += 1
        self.pstack = contextlib.ExitStack()
        try:
            yield
        finally:
            self.barrier()
            self.pstack.close()
            self.pstack = None

    def sb(self, name, shape, dt, n=1):
        self.uid += 1
        nm = "%s_%d" % (name, self.uid)
        st = self.pstack if self.pstack is not None else self.stack
        t = st.enter_context(self.nc.sbuf_tensor(nm, list(shape), dt))
        b = Buf(nm, t, n)
        self.bufs.append(b)
        return b

    def dr(self, name, shape, dt, kind="Internal", n=1):
        t = self.nc.dram_tensor(name, list(shape), dt, kind=kind)
        b = Buf(name, t, n)
        self.bufs.append(b)
        return b

    def barrier(self):
        deps = set(o for o in self.last.values() if o is not None)
        deps.update(self.since_barrier)
        self.since_barrier = []
        for e in ENGS:
            self.pending[e] = set(deps)

    def op(self, eng, fn, reads=(), writes=(), dma=None):
        o = Op(eng, fn, dma)
        o.phase = self.phase_id
        deps = set()
        rkey = eng if dma is None else ("dma", id(dma))
        for (b, lo, hi) in reads:
            for c in range(lo, hi):
                w = b.lw[c]
                if w is not None:
                    deps.add(w)
                b.rd[c][rkey] = o
        for (b, lo, hi) in writes:
            for c in range(lo, hi):
                w = b.lw[c]
                if w is not None:
                    deps.add(w)
                rd = b.rd[c]
                if rd:
                    deps.update(rd.values())
                    b.rd[c] = dict()
                b.lw[c] = o
        if self.pending[eng]:
            deps.update(self.pending[eng])
            self.pending[eng] = set()
        deps.discard(o)
        if eng == "pe":
            deps = {d for d in deps if not (d.eng == "pe" and d.dma_buf is None)}
        for d in deps:
            d.sig = True
        o.deps = deps
        self.ops[eng].append(o)
        self.allops.append(o)
        self.last[eng] = o
        if dma is not None:
            self.since_barrier.append(o)
        return o

    def dma(self, eng, out_ap, in_ap, sbuf_buf, reads=(), writes=(), **kw):
        def fn(e):
            return e.dma_start(out=out_ap, in_=in_ap, **kw)
        return self.op(eng, fn, reads, writes, dma=sbuf_buf)

    def emit(self):
        nc = self.nc
        st = self.stack
        esem = {}
        for e in ENGS:
            esem[e] = st.enter_context(nc.semaphore("s_" + e))
        free = []
        cur_phase = None
        phase_bufs = []
        for o in self.allops:
            if o.phase != cur_phase:
                for b in phase_bufs:
                    free.append(b.semrec)
                phase_bufs = []
                cur_phase = o.phase
            if o.dma_buf is not None:
                b = o.dma_buf
                if b.sem is None:
                    rec = free.pop() if free else [st.enter_context(nc.semaphore("d_" + b.name)), 0]
                    b.semrec = rec
                    b.sem = rec[0]
                    b.dmacnt = rec[1]
                    phase_bufs.append(b)
                b.dmacnt += 16
                b.semrec[1] = b.dmacnt
                o.sigval = (b.sem, b.dmacnt)
        for e in ENGS:
            cnt = 0
            for o in self.ops[e]:
                if o.dma_buf is not None:
                    pass
                elif o.sig:
                    cnt += 1
                    o.sigval = (esem[e], cnt)
            assert cnt < 65000, (e, cnt)
        block = st.enter_context(nc.Block())
        prog = self

        def run(e, eng):
            waited = {}
            for o in prog.ops[e]:
                need = {}
                for d in o.deps:
                    sem, val = d.sigval
                    k = id(sem)
                    if k not in need or need[k][1] < val:
                        need[k] = (sem, val)
                for k, (sem, val) in need.items():
                    if waited.get(k, 0) < val:
                        eng.wait_ge(sem, val)
                        waited[k] = val
                inst = o.fn(eng)
                if o.sigval is not None:
                    sem, val = o.sigval
                    inst.then_inc(sem, 16 if o.dma_buf is not None else 1)
            if e == "sp":
                for b in prog.bufs:
                    if b.sem is not None and waited.get(id(b.sem), 0) < b.dmacnt:
                        eng.wait_ge(b.sem, b.dmacnt)

        @block.tensor
        def _(eng):
            run("pe", eng)

        @block.scalar
        def _(eng):
            run("act", eng)

        @block.vector
        def _(eng):
            run("dve", eng)

        @block.gpsimd
        def _(eng):
            run("pool", eng)

        @block.sync
        def _(eng):
            run("sp", eng)

        st.close()


class NS:
    pass


def make_ident(p, dt=BF16):
    ident = p.sb("ident", [128, 128], dt)
    p.op("pool", lambda e: e.memset(ident[:], 0.0), writes=[ident.r()])
    p.op("pool", lambda e: e.affine_select(out=ident[:], in_=ident[:], pattern=[[-1, 128]],
                                           compare_op=ALU.not_equal, fill=1.0, base=0,
                                           channel_multiplier=1),
         reads=[ident.r()], writes=[ident.r()])
    return ident


def load_cols(p, dst, src_ap_1d):
    p.dma("sp", dst[:], src_ap_1d.rearrange("(kc p) -> p kc", p=128), dst, writes=[dst.r()],
          allow_slow_non_contiguous=True)


def norm_T(p, S, xres, nsub, Ac, Bc, outT):
    T = nsub * 128
    for j in range(nsub):
        p.op("act", lambda e, j=j: e.activation(out=S.junk[:], in_=xres[:, j, :], func=AF.Square,
                                                accum_out=S.ss[:, j:j + 1]),
             reads=[xres.r(j, j + 1)], writes=[S.ss.r()])
    p.op("act", lambda e: e.activation(out=S.rstd[:, 0:nsub], in_=S.ss[:, 0:nsub], func=AF.Sqrt,
                                       scale=1.0 / D, bias=EPS),
         reads=[S.ss.r()], writes=[S.rstd.r()])
    p.op("dve", lambda e: e.reciprocal(out=S.rstd[:, 0:nsub], in_=S.rstd[:, 0:nsub]),
         reads=[S.rstd.r()], writes=[S.rstd.r()])
    for j in range(nsub):
        p.op("dve", lambda e, j=j: e.tensor_scalar(out=S.xn[:, j, :], in0=xres[:, j, :],
                                                   scalar1=S.rstd[:, j:j + 1], scalar2=None,
                                                   op0=ALU.mult),
             reads=[xres.r(j, j + 1), S.rstd.r()], writes=[S.xn.r(j, j + 1)])
    for kc in range(KC):
        bank = p.bank()
        bb = bank[:].bitcast(BF16)
        for j in range(nsub):
            p.op("pe", lambda e, j=j, kc=kc, bb=bb: e.transpose(
                out=bb[:, j * 128:(j + 1) * 128], in_=S.xn[:, j, kc * 128:(kc + 1) * 128],
                identity=S.ident[:]),
                reads=[S.xn.r(j, j + 1), S.ident.r()], writes=[bank.r()])
        p.op("act", lambda e, kc=kc, bb=bb: e.activation(
            out=outT[:, kc, 0:T], in_=bb[:, 0:T], func=AF.Identity,
            scale=Ac[:, kc:kc + 1], bias=Bc[:, kc:kc + 1]),
            reads=[bank.r(), Ac.r(), Bc.r()], writes=[outT.r(kc, kc + 1)])


def wload(p, S, src_ap, nk, eng="pool"):
    slot = S.wslots[S.wi % len(S.wslots)]
    S.wi += 1
    p.dma(eng, slot[:, 0:nk, :], src_ap, slot, writes=[slot.r()])
    return slot


def wview(w_d, r0, nk, c0, nc_=512):
    return w_d[r0:r0 + nk * 128, c0:c0 + nc_].rearrange("(k p) n -> p k n", p=128)


def phase_adaln(p, cT_d, w_ada_d, b_ada_d, modd, layers=(0, 1)):
    with p.phase():
        cT = p.sb("cT", [128, 32], F32)
        brow = p.sb("brow", [2, NMOD * D], F32)
        modr = p.sb("modr", [2, NMOD * D], F32)
        ws = [p.sb("wada%d" % i, [128, KC, 512], F32) for i in range(2)]
        p.dma("sp", cT[:], cT_d[:], cT, writes=[cT.r()])
        p.op("act", lambda e: e.activation(out=cT[:], in_=cT[:], func=AF.Silu),
             reads=[cT.r()], writes=[cT.r()])
        cT3 = cT[:].rearrange("p (k j) -> p k j", j=2)
        wi = 0
        for i in layers:
            p.dma("sp", brow[:], b_ada_d[i:i + 1, :].partition_broadcast(2), brow, writes=[brow.r()])
            for nt in range(NMOD * D // 512):
                slot = ws[wi % 2]
                wi += 1
                p.dma("sp", slot[:], wview(w_ada_d, i * D, KC, nt * 512), slot, writes=[slot.r()])
                bank = p.bank()
                for kc in range(KC):
                    p.op("pe", lambda e, kc=kc, slot=slot, bank=bank: e.matmul(
                        bank[0:2, :], lhsT=cT3[:, kc, :], rhs=slot[:, kc, :],
                        start=(kc == 0), stop=(kc == KC - 1)),
                        reads=[cT.r(), slot.r()], writes=[bank.r()])
                p.op("dve", lambda e, nt=nt, bank=bank: e.tensor_tensor(
                    out=modr[:, nt * 512:(nt + 1) * 512], in0=bank[0:2, :],
                    in1=brow[:, nt * 512:(nt + 1) * 512], op=ALU.add),
                    reads=[bank.r(), brow.r()], writes=[modr.r()])
            p.dma("sp", modd[i], modr[:], modr, reads=[modr.r()], writes=[modd.r()])


def load_modset(p, S, modd, nwT_d, layer, s):
    m = modd.t
    rd = [modd.r()]
    for (dst, off) in ((S.B1c, 0), (S.sc1, 1), (S.B2c, 3), (S.sc2, 4)):
        p.dma("sp", dst[:], m[layer, s, off * D:(off + 1) * D].rearrange("(kc p) -> p kc", p=128),
              dst, reads=rd, writes=[dst.r()], allow_slow_non_contiguous=True)
    for (dst, off) in ((S.G1b, 2), (S.G2b, 5)):
        p.dma("sp", dst[:], m[layer, s:s + 1, off * D:(off + 1) * D].partition_broadcast(128),
              dst, reads=rd, writes=[dst.r()])
    for (A, sc, k) in ((S.A1c, S.sc1, 0), (S.A2c, S.sc2, 1)):
        p.op("dve", lambda e, A=A, sc=sc, k=k: e.scalar_tensor_tensor(
            out=A[:], in0=sc[:], scalar=1.0, in1=S.nwc[:, k, :], op0=ALU.add, op1=ALU.mult),
            reads=[sc.r(), S.nwc.r()], writes=[A.r()])


def alloc_mod(p, S, nwT_d, layer):
    for nm in ("A1c", "B1c", "A2c", "B2c", "sc1", "sc2"):
        setattr(S, nm, p.sb(nm, [128, KC], F32))
    S.G1b = p.sb("G1b", [128, D], F32)
    S.G2b = p.sb("G2b", [128, D], F32)
    S.nwc = p.sb("nwc", [128, 2, KC], F32)
    p.dma("sp", S.nwc[:], nwT_d[layer], S.nwc, writes=[S.nwc.r()])


def resid_update(p, S, bank, xres, j, nt, Gb):
    cs = slice(nt * 512, (nt + 1) * 512)
    tmp = S.tmps[S.ti % len(S.tmps)]
    S.ti += 1
    p.op("dve", lambda e, tmp=tmp: e.tensor_tensor(out=tmp[:], in0=bank[:], in1=Gb[:, cs], op=ALU.mult),
         reads=[bank.r(), Gb.r()], writes=[tmp.r()])
    p.op("dve", lambda e, tmp=tmp: e.tensor_tensor(out=xres[:, j, cs], in0=xres[:, j, cs], in1=tmp[:],
                                                    op=ALU.add),
         reads=[tmp.r(), xres.r(j, j + 1)], writes=[xres.r(j, j + 1)])


def phase_l0(p, W, x_d, ctx_d, x1_d, xc1_d, modd, tiles=None):
    with p.phase():
        S = NS()
        S.ident = make_ident(p)
        alloc_mod(p, S, W.nwT, 0)
        S.junk = p.sb("junk", [128, D], BF16)
        S.ss = p.sb("ss", [128, 4], F32)
        S.rstd = p.sb("rstd", [128, 4], F32)
        S.xn = p.sb("xn", [128, 4, D], BF16, n=4)
        xres = p.sb("xres", [128, 4, D], F32, n=4)
        actT = p.sb("actT", [128, KC, 512], BF16, n=KC)
        big = p.sb("big", [128, 48, 512], BF16, n=48)
        S.wslots = [p.sb("wslot%d" % i, [128, KC, 512], BF16) for i in range(3)]
        S.wi = 0
        S.tmps = [p.sb("tmp%d" % i, [128, 512], F32) for i in range(2)]
        S.ti = 0
        gvb = p.sb("gvb", [128, D], F32)
        wsT = p.sb("wsT", [128, 16, 128], BF16)
        bsr = p.sb("bsr", [1, 16, 128], F32)
        ones = p.sb("ones", [1, 128], F32)
        p.dma("sp", gvb[:], W.gv[0:1, :].partition_broadcast(128), gvb, writes=[gvb.r()])
        p.dma("pool", wsT[:], W.wsT[:], wsT, writes=[wsT.r()])
        p.dma("sp", bsr[:], W.bs[:], bsr, writes=[bsr.r()])
        p.op("dve", lambda e: e.memset(ones[:], 1.0), writes=[ones.r()])

        if tiles is None:
            tiles = [(0, t * 512, 4) for t in range(SEQ // 512)] + [(1, 0, 2)]
        def do_tile(s, t0, nsub, newset):
            T = nsub * 128
            src = x_d if s == 0 else ctx_d
            dst = x1_d if s == 0 else xc1_d
            if newset:
                load_modset(p, S, modd, W.nwT, 0, s)
            p.dma("sp", xres[:, 0:nsub, :], src[t0:t0 + T, :].rearrange("(j p) d -> p j d", p=128),
                  xres, reads=[src.r()], writes=[xres.r(0, nsub)])
            norm_T(p, S, xres, nsub, S.A1c, S.B1c, actT)
            for fg in range(4):
                slot = wload(p, S, wview(W.w_in, 0, KC, fg * 512), KC)
                for fi in range(4):
                    ft = fg * 4 + fi
                    bank = p.bank()
                    for kc in range(KC):
                        p.op("pe", lambda e, kc=kc, fi=fi, slot=slot, bank=bank: e.matmul(
                            bank[:, 0:T], lhsT=slot[:, kc, fi * 128:(fi + 1) * 128], rhs=actT[:, kc, 0:T],
                            start=(kc == 0), stop=(kc == KC - 1)),
                            reads=[slot.r(), actT.r(kc, kc + 1)], writes=[bank.r()])
                    p.op("act", lambda e, ft=ft, bank=bank: e.activation(
                        out=big[:, ft, 0:T], in_=bank[:, 0:T], func=AF.Gelu),
                        reads=[bank.r()], writes=[big.r(ft, ft + 1)])
            for nt in range(4):
                slot = wload(p, S, wview(W.w_in, 0, KC, D + nt * 512), KC)
                for j in range(nsub):
                    bank = p.bank()
                    for kc in range(KC):
                        p.op("pe", lambda e, kc=kc, j=j, slot=slot, bank=bank: e.matmul(
                            bank[:], lhsT=actT[:, kc, j * 128:(j + 1) * 128], rhs=slot[:, kc, :],
                            start=(kc == 0), stop=(kc == KC - 1)),
                            reads=[slot.r(), actT.r(kc, kc + 1)], writes=[bank.r()])
                    c = 16 + 4 * j + nt
                    p.op("act", lambda e, c=c, bank=bank: e.activation(
                        out=big[:, c, :], in_=bank[:], func=AF.Gelu),
                        reads=[bank.r()], writes=[big.r(c, c + 1)])
            for j in range(nsub):
                vj = big[:, 16 + 4 * j:20 + 4 * j, :]
                p.op("act", lambda e, j=j, vj=vj: e.activation(
                    out=S.junk[:].rearrange("p (a b) -> p a b", a=4), in_=vj, func=AF.Square,
                    accum_out=S.ss[:, j:j + 1]),
                    reads=[big.r(16 + 4 * j, 20 + 4 * j)], writes=[S.ss.r()])
            p.op("act", lambda e: e.activation(out=S.rstd[:, 0:nsub], in_=S.ss[:, 0:nsub], func=AF.Sqrt,
                                               scale=1.0 / D, bias=EPS),
                 reads=[S.ss.r()], writes=[S.rstd.r()])
            p.op("dve", lambda e: e.reciprocal(out=S.rstd[:, 0:nsub], in_=S.rstd[:, 0:nsub]),
                 reads=[S.rstd.r()], writes=[S.rstd.r()])
            for j in range(nsub):
                vj = big[:, 16 + 4 * j:20 + 4 * j, :]
                p.op("dve", lambda e, j=j, vj=vj: e.scalar_tensor_tensor(
                    out=vj, in0=vj, scalar=S.rstd[:, j:j + 1],
                    in1=gvb[:].rearrange("p (a b) -> p a b", a=4), op0=ALU.mult, op1=ALU.mult),
                    reads=[big.r(16 + 4 * j, 20 + 4 * j), S.rstd.r(), gvb.r()],
                    writes=[big.r(16 + 4 * j, 20 + 4 * j)])
            for g in range(16):
                bank = p.bank()
                p.op("pe", lambda e, g=g, bank=bank: e.matmul(
                    bank[:, 0:T].rearrange("p (j q) -> p j q", q=128), lhsT=ones[0:1, :],
                    rhs=bsr[0:1, g:g + 1, :].to_broadcast([1, nsub, 128]),
                    start=True, stop=False),
                    reads=[ones.r(), bsr.r()], writes=[bank.r()])
                for j in range(nsub):
                    c = 16 + 4 * j + g // 4
                    p.op("pe", lambda e, g=g, j=j, c=c, bank=bank: e.matmul(
                        bank[:, j * 128:(j + 1) * 128],
                        lhsT=big[:, c, (g % 4) * 128:(g % 4 + 1) * 128], rhs=wsT[:, g, :],
                        start=False, stop=(j == nsub - 1)),
                        reads=[big.r(c, c + 1), wsT.r()], writes=[bank.r()])
                p.op("dve", lambda e, g=g, bank=bank: e.tensor_tensor(
                    out=big[:, 32 + g, 0:T], in0=bank[:, 0:T], in1=big[:, g, 0:T], op=ALU.mult),
                    reads=[bank.r(), big.r(g, g + 1)], writes=[big.r(32 + g, 33 + g)])
            for nt in range(4):
                slot = wload(p, S, wview(W.w_out, 0, KC, nt * 512), KC)
                for j in range(nsub):
                    bank = p.bank()
                    for kc in range(KC):
                        p.op("pe", lambda e, kc=kc, j=j, slot=slot, bank=bank: e.matmul(
                            bank[:], lhsT=big[:, 32 + kc, j * 128:(j + 1) * 128], rhs=slot[:, kc, :],
                            start=(kc == 0), stop=(kc == KC - 1)),
                            reads=[slot.r(), big.r(32 + kc, 33 + kc)], writes=[bank.r()])
                    resid_update(p, S, bank, xres, j, nt, S.G1b)
            norm_T(p, S, xres, nsub, S.A2c, S.B2c, actT)
            for fg in range(FFN // 512):
                s1 = wload(p, S, wview(W.w1, 0, KC, fg * 512), KC)
                s3 = wload(p, S, wview(W.w3, 0, KC, fg * 512), KC)
                for fi in range(4):
                    ft = fg * 4 + fi
                    bA = p.bank()
                    bB = p.bank()
                    for (bk, sl) in ((bA, s1), (bB, s3)):
                        for kc in range(KC):
                            p.op("pe", lambda e, kc=kc, fi=fi, sl=sl, bk=bk: e.matmul(
                                bk[:, 0:T], lhsT=sl[:, kc, fi * 128:(fi + 1) * 128], rhs=actT[:, kc, 0:T],
                                start=(kc == 0), stop=(kc == KC - 1)),
                                reads=[sl.r(), actT.r(kc, kc + 1)], writes=[bk.r()])
                    tmp = S.tmps[S.ti % 2]
                    S.ti += 1
                    p.op("act", lambda e, bA=bA, tmp=tmp: e.activation(out=tmp[:, 0:T], in_=bA[:, 0:T], func=AF.Silu),
                         reads=[bA.r()], writes=[tmp.r()])
                    p.op("dve", lambda e, bB=bB, tmp=tmp, ft=ft: e.tensor_tensor(
                        out=big[:, ft, 0:T], in0=bB[:, 0:T], in1=tmp[:, 0:T], op=ALU.mult),
                        reads=[bB.r(), tmp.r()], writes=[big.r(ft, ft + 1)])
            for nt in range(4):
                bks = [p.bank() for _ in range(nsub)]
                for kg in range(4):
                    slot = wload(p, S, wview(W.w2, kg * 11 * 128, 11, nt * 512), 11)
                    for j in range(nsub):
                        for i in range(11):
                            kc = kg * 11 + i
                            p.op("pe", lambda e, kc=kc, i=i, j=j, slot=slot, bk=bks[j]: e.matmul(
                                bk[:], lhsT=big[:, kc, j * 128:(j + 1) * 128], rhs=slot[:, i, :],
                                start=(kc == 0), stop=(kc == 43)),
                                reads=[slot.r(), big.r(kc, kc + 1)], writes=[bks[j].r()])
                for j in range(nsub):
                    resid_update(p, S, bks[j], xres, j, nt, S.G2b)
            p.dma("sp", dst[t0:t0 + T, :].rearrange("(j p) d -> p j d", p=128), xres[:, 0:nsub, :],
                  xres, reads=[xres.r(0, nsub)], writes=[dst.r()])

        cur_set = None
        for (s_, t0_, nsub_) in tiles:
            do_tile(s_, t0_, nsub_, s_ != cur_set)
            cur_set = s_


def declare_l0_weights(p, kind="ExternalInput"):
    W = NS()
    W.nwT = p.dr("nwT", [2, 128, 2, KC], F32, kind=kind)
    W.w_in = p.dr("mlp_w_in", [D, 2 * D], F32, kind=kind)
    W.gv = p.dr("mlp_g_v", [1, D], F32, kind=kind)
    W.wsT = p.dr("mlp_wsT", [128, 16, 128], F32, kind=kind)
    W.bs = p.dr("mlp_b_s", [1, 16, 128], F32, kind=kind)
    W.w_out = p.dr("mlp_w_out", [D, D], F32, kind=kind)
    W.w1 = p.dr("ffn_w1", [D, FFN], F32, kind=kind)
    W.w3 = p.dr("ffn_w3", [D, FFN], F32, kind=kind)
    W.w2 = p.dr("ffn_w2", [FFN, D], F32, kind=kind)
    return W


def host_l0_weights(inp):
    nw = inp["norm_w"]
    nwT = np.ascontiguousarray(nw.reshape(2, 2, KC, 128).transpose(0, 3, 1, 2))
    return dict(
        nwT=nwT,
        mlp_w_in=np.ascontiguousarray(inp["mlp_w_in"][0]),
        mlp_g_v=np.ascontiguousarray(inp["mlp_g_v"][0:1]),
        mlp_wsT=np.ascontiguousarray(inp["mlp_w_s"][0].transpose(2, 0, 1)),
        mlp_b_s=np.ascontiguousarray(inp["mlp_b_s"][0][None]),
        mlp_w_out=np.ascontiguousarray(inp["mlp_w_out"][0]),
        ffn_w1=np.ascontiguousarray(inp["ffn_w1"][0]),
        ffn_w3=np.ascontiguousarray(inp["ffn_w3"][0]),
        ffn_w2=np.ascontiguousarray(inp["ffn_w2"][0]),
    )


def host_cT(inp, b):
    cc = np.stack([inp["c"][b], inp["c_ctx"]], axis=-1)
    return np.ascontiguousarray(cc.reshape(KC, 128, 2).transpose(1, 0, 2).reshape(128, 32))


def build_l0(tiles=None, layers=(0, 1)):
    nc = bass.Bass("TRN2", target_bir_lowering=False)
    p = Prog(nc)
    p.init_psum()
    cT_d = p.dr("cT", [128, 32], F32, kind="ExternalInput")
    w_ada_d = p.dr("w_ada", [2 * D, NMOD * D], F32, kind="ExternalInput")
    b_ada_d = p.dr("b_ada", [2, NMOD * D], F32, kind="ExternalInput")
    x_d = p.dr("x", [SEQ, D], F32, kind="ExternalInput")
    ctx_d = p.dr("ctx", [CTX, D], F32, kind="ExternalInput")
    W = declare_l0_weights(p)
    modd = p.dr("modd", [2, 2, NMOD * D], F32, kind="ExternalOutput")
    x1_d = p.dr("x1", [SEQ, D], F32, kind="ExternalOutput")
    xc1_d = p.dr("xc1", [CTX, D], F32, kind="ExternalOutput")
    phase_adaln(p, cT_d, w_ada_d, b_ada_d, modd, layers)
    phase_l0(p, W, x_d, ctx_d, x1_d, xc1_d, modd, tiles)
    p.emit()
    return nc


NH = 16
NKEY = SEQ + CTX
NEG = -30000.0


class BankPool:
    def __init__(self, p, ids):
        self.b = [p.banks[i] for i in ids]
        self.i = 0

    def next(self):
        b = self.b[self.i % len(self.b)]
        self.i += 1
        return b


def l1_variant(g, j):
    if g == 0:
        return j, j
    if g == 7:
        return 26 + j, 14 + j
    return 4 * g - 2 + j, 6 + j


def l1_ntiles(g):
    return 6 if g in (0, 7) else 8


def host_bias_table(rpb):
    out = np.empty((NH, 20, 128, 512), np.float32)
    kk = np.arange(128)
    qq = np.arange(512)
    for g in (0, 1, 7):
        for j in range(l1_ntiles(g)):
            kt, var = l1_variant(g, j)
            kr = (2 * kt + kk // 64)[:, None]
            kc = (kk % 64)[:, None]
            qr = (8 * g + qq // 64)[None, :]
            qc = (qq % 64)[None, :]
            rs = np.clip(qr - 4, 0, 56)
            cs = np.clip(qc - 8, 0, 48)
            valid = (kr >= rs) & (kr < rs + 8) & (kc >= cs) & (kc < cs + 16)
            dr = np.clip(kr - qr + 7, 0, 14)
            dc = np.clip(kc - qc + 15, 0, 30)
            out[:, var] = np.where(valid[None], rpb[:, dr, dc], np.float32(NEG))
    return out


def phase_l1a(p, W, x1_d, xc1_d, qT_d, kT_d, v_d, modd, tiles=None):
    with p.phase():
        S = NS()
        S.ident = make_ident(p)
        alloc_mod(p, S, W.nwT, 1)
        S.junk = p.sb("junk", [128, D], BF16)
        S.ss = p.sb("ss", [128, 4], F32)
        S.rstd = p.sb("rstd", [128, 4], F32)
        S.xn = p.sb("xn", [128, 4, D], BF16, n=4)
        xres = p.sb("xres", [128, 4, D], F32, n=4)
        actT = p.sb("actT", [128, KC, 512], BF16, n=KC)
        S.wslots = [p.sb("wslot%d" % i, [128, KC, 512], BF16) for i in range(3)]
        S.wi = 0
        qk_sb = [p.sb("qsb", [128, NH, 512], BF16, n=NH), p.sb("ksb", [128, NH, 512], BF16, n=NH)]
        v_sb = p.sb("vsb", [128, 4, D], BF16, n=4)
        sqs = [p.sb("sq%d" % i, [128, 512], BF16) for i in range(2)]
        rr = [p.sb("rr%d" % i, [128, 512], F32) for i in range(2)]
        ones = p.sb("onesb", [128, 128], BF16)
        gcol = p.sb("gcol", [128, 2], F32)
        p.op("dve", lambda e: e.memset(ones[:], 1.0), writes=[ones.r()])
        p.dma("sp", gcol[:], W.gqk[:], gcol, writes=[gcol.r()])
        p.op("dve", lambda e: e.tensor_scalar(out=gcol[:, 0:1], in0=gcol[:, 0:1], scalar1=128.0 ** -0.5,
                                              scalar2=None, op0=ALU.mult),
             reads=[gcol.r()], writes=[gcol.r()])
        if tiles is None:
            tiles = [(0, t * 512, 4) for t in range(SEQ // 512)] + [(1, 0, 2)]
        cnt_box = [0]

        def do_tile(s, t0, nsub, newset):
            T = nsub * 128
            src = x1_d if s == 0 else xc1_d
            if newset:
                load_modset(p, S, modd, W.nwT, 1, s)
            p.dma("sp", xres[:, 0:nsub, :], src[t0:t0 + T, :].rearrange("(j p) d -> p j d", p=128),
                  xres, reads=[src.r()], writes=[xres.r(0, nsub)])
            norm_T(p, S, xres, nsub, S.A1c, S.B1c, actT)
            parts = (0, 1) if s == 0 else (1,)
            pend = None

            def finish(pd):
                (part, h, bankQ, sq, r) = pd
                bankS = p.bank()
                p.op("pe", lambda e: e.matmul(bankS[:, 0:T], lhsT=ones[:], rhs=sq[:, 0:T], start=True, stop=True),
                     reads=[ones.r(), sq.r()], writes=[bankS.r()])
                p.op("act", lambda e: e.activation(out=r[:, 0:T], in_=bankS[:, 0:T], func=AF.Sqrt,
                                                   scale=1.0 / 128, bias=EPS),
                     reads=[bankS.r()], writes=[r.r()])
                p.op("dve", lambda e: e.reciprocal(out=r[:, 0:T], in_=r[:, 0:T]), reads=[r.r()], writes=[r.r()])
                p.op("dve", lambda e: e.scalar_tensor_tensor(
                    out=qk_sb[part][:, h, 0:T], in0=bankQ[:, 0:T], scalar=gcol[:, part:part + 1],
                    in1=r[:, 0:T], op0=ALU.mult, op1=ALU.mult),
                    reads=[bankQ.r(), gcol.r(), r.r()], writes=[qk_sb[part].r(h, h + 1)])

            for part in parts:
                for hg in range(4):
                    slot = wload(p, S, wview(W.w_qkv, 0, KC, part * D + hg * 512), KC)
                    for hi in range(4):
                        h = hg * 4 + hi
                        bankQ = p.bank()
                        for kc in range(KC):
                            p.op("pe", lambda e, kc=kc, hi=hi, slot=slot, bankQ=bankQ: e.matmul(
                                bankQ[:, 0:T], lhsT=slot[:, kc, hi * 128:(hi + 1) * 128], rhs=actT[:, kc, 0:T],
                                start=(kc == 0), stop=(kc == KC - 1)),
                                reads=[slot.r(), actT.r(kc, kc + 1)], writes=[bankQ.r()])
                        sq = sqs[cnt_box[0] % 2]
                        r = rr[cnt_box[0] % 2]
                        cnt_box[0] += 1
                        p.op("act", lambda e, sq=sq, bankQ=bankQ: e.activation(out=sq[:, 0:T], in_=bankQ[:, 0:T],
                                                                               func=AF.Square),
                             reads=[bankQ.r()], writes=[sq.r()])
                        if pend is not None:
                            finish(pend)
                        pend = (part, h, bankQ, sq, r)
            finish(pend)
            for nt in range(4):
                slot = wload(p, S, wview(W.w_qkv, 0, KC, 2 * D + nt * 512), KC)
                for j in range(nsub):
                    bank = p.bank()
                    for kc in range(KC):
                        p.op("pe", lambda e, kc=kc, j=j, slot=slot, bank=bank: e.matmul(
                            bank[:], lhsT=actT[:, kc, j * 128:(j + 1) * 128], rhs=slot[:, kc, :],
                            start=(kc == 0), stop=(kc == KC - 1)),
                            reads=[slot.r(), actT.r(kc, kc + 1)], writes=[bank.r()])
                    p.op("act", lambda e, j=j, nt=nt, bank=bank: e.activation(
                        out=v_sb[:, j, nt * 512:(nt + 1) * 512], in_=bank[:], func=AF.Copy),
                        reads=[bank.r()], writes=[v_sb.r(j, j + 1)])
            kcol = t0 if s == 0 else SEQ + t0
            if s == 0:
                p.dma("sp", qT_d[:, :, t0:t0 + T].rearrange("h p t -> p h t"), qk_sb[0][:, :, 0:T],
                      qk_sb[0], reads=[qk_sb[0].r()], writes=[qT_d.r()])
            p.dma("sp", kT_d[:, :, kcol:kcol + T].rearrange("h p t -> p h t"), qk_sb[1][:, :, 0:T],
                  qk_sb[1], reads=[qk_sb[1].r()], writes=[kT_d.r()])
            p.dma("sp", v_d[kcol:kcol + T, :].rearrange("(j p) d -> p j d", p=128), v_sb[:, 0:nsub, :],
                  v_sb, reads=[v_sb.r(0, nsub)], writes=[v_d.r()])

        cur_set = None
        for (s_, t0_, nsub_) in tiles:
            do_tile(s_, t0_, nsub_, s_ != cur_set)
            cur_set = s_


def phase_l1b(p, W, qT_d, kT_d, v_d, OT_d, heads=None, groups=None):
    with p.phase():
        ones = p.sb("onesb", [128, 128], BF16)
        p.op("dve", lambda e: e.memset(ones[:], 1.0), writes=[ones.r()])
        NB = 2
        qh = [p.sb("qh%d" % i, [128, SEQ], BF16) for i in range(NB)]
        kh = [p.sb("kh%d" % i, [128, NKEY], BF16) for i in range(NB)]
        vh = [p.sb("vh%d" % i, [128, NKEY // 128, 128], BF16) for i in range(NB)]
        tb = [p.sb("tb%d" % i, [128, 20, 512], F32) for i in range(NB)]
        OTs = [p.sb("OTs%d" % i, [128, SEQ], BF16) for i in range(NB)]
        pTs = [p.sb("pT%d" % i, [128, 512], BF16) for i in range(4)]
        tmps = [p.sb("tmpa%d" % i, [128, 512], F32) for i in range(3)]
        rDs = [p.sb("rD%d" % i, [128, 512], F32) for i in range(2)]
        od_pool = BankPool(p, [0, 1, 2, 3])
        s_pool = BankPool(p, [4, 5, 6, 7])
        ci = 0
        if heads is None:
            heads = list(range(NH))
        if groups is None:
            groups = list(range(8))
        ci_box = [0]

        def do_head(hn, h):
            b_ = hn % NB
            q_, k_, v_, t_, O_ = qh[b_], kh[b_], vh[b_], tb[b_], OTs[b_]
            p.dma("sp", q_[:], qT_d[h], q_, reads=[qT_d.r()], writes=[q_.r()])
            p.dma("sp", k_[:], kT_d[h], k_, reads=[kT_d.r()], writes=[k_.r()])
            p.dma("sp", v_[:], v_d[:, h * 128:(h + 1) * 128].rearrange("(kt p) c -> p kt c", p=128), v_,
                  reads=[v_d.r()], writes=[v_.r()])
            p.dma("sp", t_[:], W.tbl[h].rearrange("v p q -> p v q"), t_, writes=[t_.r()])
            def do_group(g):
                tl = [l1_variant(g, j) for j in range(l1_ntiles(g))] + [(32, None), (33, None)]
                bankO = od_pool.next()
                bankD = od_pool.next()
                pend = None
                n = len(tl)

                def pv(pd):
                    (idx, kt, pT) = pd
                    p.op("pe", lambda e: e.matmul(bankO[:], lhsT=v_[:, kt, :], rhs=pT[:], start=(idx == 0),
                                                  stop=(idx == n - 1)),
                         reads=[v_.r(), pT.r()], writes=[bankO.r()])
                    p.op("pe", lambda e: e.matmul(bankD[:], lhsT=ones[:], rhs=pT[:], start=(idx == 0),
                                                  stop=(idx == n - 1)),
                         reads=[ones.r(), pT.r()], writes=[bankD.r()])

                for idx, (kt, var) in enumerate(tl):
                    bankS = s_pool.next()
                    p.op("pe", lambda e, kt=kt, bankS=bankS: e.matmul(
                        bankS[:], lhsT=k_[:, kt * 128:(kt + 1) * 128], rhs=q_[:, g * 512:(g + 1) * 512],
                        start=True, stop=True),
                        reads=[k_.r(), q_.r()], writes=[bankS.r()])
                    ci = ci_box[0]
                    pT = pTs[ci % 4]
                    if var is not None:
                        tmp = tmps[ci % 3]
                        p.op("dve", lambda e, var=var, bankS=bankS, tmp=tmp: e.tensor_tensor(
                            out=tmp[:], in0=bankS[:], in1=t_[:, var, :], op=ALU.add),
                            reads=[bankS.r(), t_.r()], writes=[tmp.r()])
                        p.op("act", lambda e, tmp=tmp, pT=pT: e.activation(out=pT[:], in_=tmp[:], func=AF.Exp),
                             reads=[tmp.r()], writes=[pT.r()])
                    else:
                        p.op("act", lambda e, bankS=bankS, pT=pT: e.activation(out=pT[:], in_=bankS[:], func=AF.Exp),
                             reads=[bankS.r()], writes=[pT.r()])
                    ci_box[0] += 1
                    if pend is not None:
                        pv(pend)
                    pend = (idx, kt, pT)
                pv(pend)
                rD = rDs[ci_box[0] % 2]
                p.op("dve", lambda e, rD=rD: e.reciprocal(out=rD[:], in_=bankD[:]), reads=[bankD.r()], writes=[rD.r()])
                p.op("dve", lambda e, rD=rD, g=g: e.tensor_tensor(out=O_[:, g * 512:(g + 1) * 512], in0=bankO[:],
                                                                   in1=rD[:], op=ALU.mult),
                     reads=[bankO.r(), rD.r()], writes=[O_.r()])
            for g_ in groups:
                do_group(g_)
            p.dma("sp", OT_d[h], O_[:], O_, reads=[O_.r()], writes=[OT_d.r()])

        for hn_, h_ in enumerate(heads):
            do_head(hn_, h_)


def phase_l1c(p, W, x1_d, OT_d, x2_d, toks_d, rt_d, modd, ntiles=None):
    with p.phase():
        S = NS()
        xres = p.sb("xres", [128, 4, D], F32, n=4)
        OTt = p.sb("OTt", [128, NH, 512], BF16)
        S.wslots = [p.sb("wslot%d" % i, [128, KC, 512], BF16) for i in range(2)]
        S.wi = 0
        S.tmps = [p.sb("tmp%d" % i, [128, 512], F32) for i in range(2)]
        S.ti = 0
        G1b = p.sb("G1b", [128, D], F32)
        A2b = p.sb("A2b", [128, D], F32)
        B2b = p.sb("B2b", [128, D], F32)
        nwb = p.sb("nwb", [128, D], BF16)
        wrb = p.sb("wrb", [128, 8, D], F32)
        tk = [p.sb("tk%d" % i, [128, D], F32) for i in range(2)]
        ss = p.sb("ss", [128, 4], F32)
        rstd = p.sb("rstd", [128, 4], F32)
        lg = p.sb("lg", [128, 8], F32)
        srt = p.sb("srt", [128, 8], F32)
        dl = p.sb("dl", [128, 1], F32)
        rt = p.sb("rt", [128, 4, 18], F32)
        m = modd.t
        p.dma("sp", G1b[:], m[1, 0:1, 2 * D:3 * D].partition_broadcast(128), G1b, reads=[modd.r()], writes=[G1b.r()])
        p.dma("sp", B2b[:], m[1, 0:1, 3 * D:4 * D].partition_broadcast(128), B2b, reads=[modd.r()], writes=[B2b.r()])
        p.dma("sp", A2b[:], m[1, 0:1, 4 * D:5 * D].partition_broadcast(128), A2b, reads=[modd.r()], writes=[A2b.r()])
        p.dma("sp", tk[0][:], W.nwrow[1:2, :].partition_broadcast(128), tk[0], writes=[tk[0].r()])
        p.op("dve", lambda e: e.scalar_tensor_tensor(out=A2b[:], in0=A2b[:], scalar=1.0, in1=tk[0][:],
                                                     op0=ALU.add, op1=ALU.mult),
             reads=[A2b.r(), tk[0].r()], writes=[A2b.r()])
        p.dma("sp", wrb[:].rearrange("p e d -> p (e d)"),
              W.wrT[:].rearrange("(o e) d -> o (e d)", o=1).partition_broadcast(128), wrb, writes=[wrb.r()])
        if ntiles is None:
            ntiles = list(range(SEQ // 512))
        ti_box = [0]

        def do_tile(t):
            t0 = t * 512
            p.dma("sp", xres[:], x1_d[t0:t0 + 512, :].rearrange("(j p) d -> p j d", p=128),
                  xres, reads=[x1_d.r()], writes=[xres.r()])
            p.dma("sp", OTt[:], OT_d[:, :, t0:t0 + 512].rearrange("h p t -> p h t"), OTt,
                  reads=[OT_d.r()], writes=[OTt.r()])
            for nt in range(4):
                slot = wload(p, S, wview(W.w_o, 0, KC, nt * 512), KC)
                for j in range(4):
                    bank = p.bank()
                    for h in range(NH):
                        p.op("pe", lambda e, h=h, j=j, slot=slot, bank=bank: e.matmul(
                            bank[:], lhsT=OTt[:, h, j * 128:(j + 1) * 128], rhs=slot[:, h, :],
                            start=(h == 0), stop=(h == NH - 1)),
                            reads=[slot.r(), OTt.r()], writes=[bank.r()])
                    resid_update(p, S, bank, xres, j, nt, G1b)
            p.dma("sp", x2_d[t0:t0 + 512, :].rearrange("(j p) d -> p j d", p=128), xres[:], xres,
                  reads=[xres.r()], writes=[x2_d.r()])
            for j in range(4):
                p.op("act", lambda e, j=j: e.activation(out=nwb[:], in_=xres[:, j, :], func=AF.Square,
                                                        accum_out=ss[:, j:j + 1]),
                     reads=[xres.r(j, j + 1)], writes=[ss.r()])
            p.op("act", lambda e: e.activation(out=rstd[:], in_=ss[:], func=AF.Sqrt, scale=1.0 / D, bias=EPS),
                 reads=[ss.r()], writes=[rstd.r()])
            p.op("dve", lambda e: e.reciprocal(out=rstd[:], in_=rstd[:]), reads=[rstd.r()], writes=[rstd.r()])
            for j in range(4):
                tkb = tk[ti_box[0] % 2]
                ti_box[0] += 1
                p.op("dve", lambda e, j=j, tkb=tkb: e.scalar_tensor_tensor(
                    out=tkb[:], in0=xres[:, j, :], scalar=rstd[:, j:j + 1], in1=A2b[:], op0=ALU.mult, op1=ALU.mult),
                    reads=[xres.r(j, j + 1), rstd.r(), A2b.r()], writes=[tkb.r()])
                p.op("dve", lambda e, tkb=tkb: e.tensor_tensor(out=tkb[:], in0=tkb[:], in1=B2b[:], op=ALU.add),
                     reads=[tkb.r(), B2b.r()], writes=[tkb.r()])
                p.dma("pool", toks_d[t0 + j * 128:t0 + (j + 1) * 128, :], tkb[:], tkb, reads=[tkb.r()],
                      writes=[toks_d.r()])
                for e_ in range(8):
                    p.op("dve", lambda e, e_=e_, tkb=tkb: e.scalar_tensor_tensor(
                        out=nwb[:], in0=tkb[:], scalar=1.0, in1=wrb[:, e_, :], op0=ALU.mult, op1=ALU.mult,
                        accum_out=lg[:, e_:e_ + 1]),
                        reads=[tkb.r(), wrb.r()], writes=[lg.r()])
                p.op("dve", lambda e: e.max(out=srt[:], in_=lg[:]), reads=[lg.r()], writes=[srt.r()])
                p.op("dve", lambda e, j=j: e.tensor_scalar(out=rt[:, j, 0:8], in0=lg[:], scalar1=srt[:, 0:1],
                                                           scalar2=None, op0=ALU.is_equal),
                     reads=[lg.r(), srt.r()], writes=[rt.r()])
                p.op("dve", lambda e, j=j: e.tensor_scalar(out=rt[:, j, 8:16], in0=lg[:], scalar1=srt[:, 1:2],
                                                           scalar2=None, op0=ALU.is_equal),
                     reads=[lg.r(), srt.r()], writes=[rt.r()])
                p.op("dve", lambda e: e.tensor_tensor(out=dl[:], in0=srt[:, 0:1], in1=srt[:, 1:2], op=ALU.subtract),
                     reads=[srt.r()], writes=[dl.r()])
                p.op("act", lambda e, j=j: e.activation(out=rt[:, j, 16:17], in_=dl[:], func=AF.Sigmoid),
                     reads=[dl.r()], writes=[rt.r()])
                p.op("act", lambda e, j=j: e.activation(out=rt[:, j, 17:18], in_=dl[:], func=AF.Sigmoid, scale=-1.0),
                     reads=[dl.r()], writes=[rt.r()])
            p.dma("sp", rt_d[t0:t0 + 512, :].rearrange("(j p) c -> p j c", p=128), rt[:], rt,
                  reads=[rt.r()], writes=[rt_d.r()])

        for t_ in ntiles:
            do_tile(t_)


def declare_l1_weights(p, kind="ExternalInput"):
    W = NS()
    W.nwT = p.dr("nwT1", [2, 128, 2, KC], F32, kind=kind)
    W.nwrow = p.dr("nwrow", [2, D], F32, kind=kind)
    W.w_qkv = p.dr("na_w_qkv", [D, 3 * D], F32, kind=kind)
    W.gqk = p.dr("na_gqk", [128, 2], F32, kind=kind)
    W.tbl = p.dr("na_tbl", [NH, 20, 128, 512], F32, kind=kind)
    W.w_o = p.dr("na_w_o", [D, D], F32, kind=kind)
    W.wrT = p.dr("moe_wrT", [8, D], F32, kind=kind)
    return W


def host_l1_weights(inp):
    nw = inp["norm_w"]
    nwT = np.ascontiguousarray(nw.reshape(2, 2, KC, 128).transpose(0, 3, 1, 2))
    return dict(
        nwT1=nwT,
        nwrow=np.ascontiguousarray(nw[1]),
        na_w_qkv=np.ascontiguousarray(inp["na_w_qkv"][0]),
        na_gqk=np.ascontiguousarray(np.stack([inp["na_g_q"][0], inp["na_g_k"][0]], axis=-1)),
        na_tbl=host_bias_table(inp["na_rpb"][0]),
        na_w_o=np.ascontiguousarray(inp["na_w_o"][0]),
        moe_wrT=np.ascontiguousarray(inp["moe_w_router"][0].T),
    )


NE = 8
MOE = 7168
BS = 512
NBLK = SEQ * 2 // BS + NE
NSLOT = NBLK * BS
NM = 56
IOA = bass.IndirectOffsetOnAxis


def phase_moe(p, W, toks_d, rt_d, ys_d, stab_d, idx_d, nblk=NBLK):
    with p.phase():
        ident = make_ident(p)
        ones = p.sb("onesb", [128, 128], BF16)
        p.op("dve", lambda e: e.memset(ones[:], 1.0), writes=[ones.r()])
        rt_all = p.sb("rt_all", [128, 32, 18], F32)
        p.dma("sp", rt_all[:], rt_d[:, :].rearrange("(i p) c -> p i c", p=128), rt_all,
              reads=[rt_d.r()], writes=[rt_all.r()])
        Mb = p.sb("Mb", [128, 32, 8], BF16)
        p.op("dve", lambda e: e.tensor_tensor(out=Mb[:], in0=rt_all[:, :, 0:8], in1=rt_all[:, :, 8:16], op=ALU.add),
             reads=[rt_all.r()], writes=[Mb.r()])
        Ls = p.sb("Ls", [128, 128], BF16)
        p.op("pool", lambda e: e.memset(Ls[:], 0.0), writes=[Ls.r()])
        p.op("pool", lambda e: e.affine_select(out=Ls[:], in_=Ls[:], pattern=[[-1, 128]], compare_op=ALU.is_ge,
                                               fill=1.0, base=0, channel_multiplier=1),
             reads=[Ls.r()], writes=[Ls.r()])
        bankC = p.banks[0]
        bankW = p.banks[1]
        p.op("pe", lambda e: e.matmul(bankC[:, 0:256], lhsT=ones[:], rhs=Mb[:].rearrange("p i e -> p (i e)"),
                                      start=True, stop=True),
             reads=[ones.r(), Mb.r()], writes=[bankC.r()])
        for i in range(32):
            p.op("pe", lambda e, i=i: e.matmul(bankW[:, i * 8:(i + 1) * 8], lhsT=Ls[:], rhs=Mb[:, i, :],
                                               start=True, stop=True),
                 reads=[Ls.r(), Mb.r()], writes=[bankW.r()])
        cs = p.sb("cs", [128, 32, 8], F32)
        off = p.sb("off", [128, 32, 8], F32)
        p.op("act", lambda e: e.activation(out=cs[:].rearrange("p i e -> p (i e)"), in_=bankC[:, 0:256], func=AF.Copy),
             reads=[bankC.r()], writes=[cs.r()])
        p.op("dve", lambda e: e.memset(off[:, 0, :], 0.0), writes=[off.r()])
        for i in range(1, 32):
            p.op("dve", lambda e, i=i: e.tensor_tensor(out=off[:, i, :], in0=off[:, i - 1, :], in1=cs[:, i - 1, :],
                                                       op=ALU.add),
                 reads=[off.r(), cs.r()], writes=[off.r()])
        cnt = p.sb("cnt", [128, 8], F32)
        p.op("dve", lambda e: e.tensor_tensor(out=cnt[:], in0=off[:, 31, :], in1=cs[:, 31, :], op=ALU.add),
             reads=[off.r(), cs.r()], writes=[cnt.r()])
        io8 = p.sb("io8", [128, 8], I32)
        thr = p.sb("thr", [128, 8], F32)
        p.op("pool", lambda e: e.iota(io8[:], pattern=[[BS, 8]], base=0, channel_multiplier=0), writes=[io8.r()])
        p.op("dve", lambda e: e.tensor_copy(out=thr[:], in_=io8[:]), reads=[io8.r()], writes=[thr.r()])
        cmp = p.sb("cmp", [128, 8, 8], F32)
        p.op("dve", lambda e: e.tensor_tensor(out=cmp[:], in0=cnt[:].unsqueeze(2).to_broadcast([128, 8, 8]),
                                              in1=thr[:].unsqueeze(1).to_broadcast([128, 8, 8]), op=ALU.is_gt),
             reads=[cnt.r(), thr.r()], writes=[cmp.r()])
        pad = p.sb("pad", [128, 8], F32)
        p.op("dve", lambda e: e.tensor_reduce(out=pad[:], in_=cmp[:], axis=AX.X, op=ALU.add),
             reads=[cmp.r()], writes=[pad.r()])
        p.op("dve", lambda e: e.tensor_scalar(out=pad[:], in0=pad[:], scalar1=float(BS), scalar2=None, op0=ALU.mult),
             reads=[pad.r()], writes=[pad.r()])
        pst = p.sb("pst", [128, 8], F32)
        pen = p.sb("pen", [128, 8], F32)
        p.op("dve", lambda e: e.memset(pst[:, 0:1], 0.0), writes=[pst.r()])
        for e_ in range(1, 8):
            p.op("dve", lambda e, e_=e_: e.tensor_tensor(out=pst[:, e_:e_ + 1], in0=pst[:, e_ - 1:e_],
                                                         in1=pad[:, e_ - 1:e_], op=ALU.add),
                 reads=[pst.r(), pad.r()], writes=[pst.r()])
        p.op("dve", lambda e: e.tensor_tensor(out=pen[:], in0=pst[:], in1=pad[:], op=ALU.add),
             reads=[pst.r(), pad.r()], writes=[pen.r()])
        iob = p.sb("iob", [128, NBLK], I32)
        bthr = p.sb("bthr", [128, NBLK], F32)
        p.op("pool", lambda e: e.iota(iob[:], pattern=[[BS, NBLK]], base=0, channel_multiplier=0), writes=[iob.r()])
        p.op("dve", lambda e: e.tensor_copy(out=bthr[:], in_=iob[:]), reads=[iob.r()], writes=[bthr.r()])
        cmp2 = p.sb("cmp2", [128, NBLK, 8], F32)
        p.op("dve", lambda e: e.tensor_tensor(out=cmp2[:], in0=pen[:].unsqueeze(1).to_broadcast([128, NBLK, 8]),
                                              in1=bthr[:].unsqueeze(2).to_broadcast([128, NBLK, 8]), op=ALU.is_le),
             reads=[pen.r(), bthr.r()], writes=[cmp2.r()])
        be = p.sb("be", [128, NBLK], F32)
        p.op("dve", lambda e: e.tensor_reduce(out=be[:], in_=cmp2[:], axis=AX.X, op=ALU.add),
             reads=[cmp2.r()], writes=[be.r()])
        p.op("dve", lambda e: e.tensor_scalar(out=be[:], in0=be[:], scalar1=float(NE - 1), scalar2=float(MOE),
                                              op0=ALU.min, op1=ALU.mult),
             reads=[be.r()], writes=[be.r()])
        slotf = p.sb("slotf", [128, 32, 8], F32)
        p.op("dve", lambda e: e.tensor_tensor(out=slotf[:].rearrange("p i e -> p (i e)"), in0=bankW[:, 0:256],
                                              in1=off[:].rearrange("p i e -> p (i e)"), op=ALU.add),
             reads=[bankW.r(), off.r()], writes=[slotf.r()])
        p.op("dve", lambda e: e.tensor_tensor(out=slotf[:], in0=slotf[:],
                                              in1=pst[:].unsqueeze(1).to_broadcast([128, 32, 8]), op=ALU.add),
             reads=[slotf.r(), pst.r()], writes=[slotf.r()])
        tmp3 = p.sb("tmp3", [128, 32, 8], F32)
        slab = p.sb("slab", [128, 64], F32)
        idxab = p.sb("idxab", [128, 64], I32)
        for k in range(2):
            p.op("dve", lambda e, k=k: e.tensor_tensor(out=tmp3[:], in0=rt_all[:, :, 8 * k:8 * k + 8], in1=slotf[:],
                                                       op=ALU.mult),
                 reads=[rt_all.r(), slotf.r()], writes=[tmp3.r()])
            p.op("dve", lambda e, k=k: e.tensor_reduce(out=slab[:, 32 * k:32 * k + 32], in_=tmp3[:], axis=AX.X,
                                                       op=ALU.add),
                 reads=[tmp3.r()], writes=[slab.r()])
        p.op("dve", lambda e: e.tensor_copy(out=idxab[:], in_=slab[:]), reads=[slab.r()], writes=[idxab.r()])
        p.dma("sp", idx_d[:, :], idxab[:], idxab, reads=[idxab.r()], writes=[idx_d.r()])
        tokid = p.sb("tokid", [128, 32], I32)
        p.op("pool", lambda e: e.iota(tokid[:], pattern=[[128, 32]], base=0, channel_multiplier=1), writes=[tokid.r()])
        rows = p.sb("rows", [128, 64, 2], I32)
        for k in range(2):
            p.op("dve", lambda e, k=k: e.tensor_copy(out=rows[:, 32 * k:32 * k + 32, 0:1], in_=tokid[:].unsqueeze(2)),
                 reads=[tokid.r()], writes=[rows.r()])
            p.op("dve", lambda e, k=k: e.tensor_copy(out=rows[:, 32 * k:32 * k + 32, 1:2].bitcast(F32),
                                                     in_=rt_all[:, :, 16 + k:17 + k]),
                 reads=[rt_all.r()], writes=[rows.r()])
        zer = p.sb("zer", [128, NSLOT // 128 * 2], I32)
        p.op("dve", lambda e: e.memset(zer[:], 0), writes=[zer.r()])
        p.dma("sp", stab_d[:, :].rearrange("(p a) c -> p (a c)", p=128), zer[:], zer, reads=[zer.r()],
              writes=[stab_d.r()])
        for i in range(64):
            p.op("pool", lambda e, i=i: e.indirect_dma_start(
                out=stab_d[:, :], out_offset=IOA(ap=idxab[:, i:i + 1], axis=0), in_=rows[:, i, :], in_offset=None),
                reads=[idxab.r(), rows.r()], writes=[stab_d.r()], dma=rows)
        wi_i = p.sb("wi_i", [128, NM], I32)
        wi_f = p.sb("wi_f", [128, NM], F32)
        p.op("pool", lambda e: e.iota(wi_i[:], pattern=[[128, NM]], base=0, channel_multiplier=1), writes=[wi_i.r()])
        p.op("dve", lambda e: e.tensor_copy(out=wi_f[:], in_=wi_i[:]), reads=[wi_i.r()], writes=[wi_f.r()])
        widx_f = p.sb("widx_f", [128, NBLK, NM], F32)
        widx = p.sb("widx", [128, NBLK, NM], I32)
        p.op("dve", lambda e: e.tensor_tensor(out=widx_f[:], in0=wi_f[:].unsqueeze(1).to_broadcast([128, NBLK, NM]),
                                              in1=be[:].unsqueeze(2).to_broadcast([128, NBLK, NM]), op=ALU.add),
             reads=[wi_f.r(), be.r()], writes=[widx_f.r()])
        p.op("dve", lambda e: e.tensor_copy(out=widx[:], in_=widx_f[:]), reads=[widx_f.r()], writes=[widx.r()])

        sidxs = [p.sb("sidx%d" % i, [128, 4, 2], I32) for i in range(2)]
        xs_tok = p.sb("xs_tok", [128, 4, D], BF16)
        xsT = p.sb("xsT", [128, KC, BS], BF16, n=KC)
        hT = p.sb("hT", [128, NM, BS], BF16, n=NM)
        wsl = [p.sb("mslot%d" % i, [128, KC, 512], BF16, n=4) for i in range(3)]
        ys_sb = p.sb("ys_sb", [128, 4, D], F32, n=4)
        tmps = [p.sb("mtmp%d" % i, [128, BS], F32) for i in range(2)]
        st = NS()
        st.wi = 0
        st.ti = 0

        def wl(src_d, b, m, nq):
            slot = wsl[st.wi % len(wsl)]
            st.wi += 1
            for kq in range(nq):
                p.op("pool", lambda e, kq=kq, slot=slot: e.indirect_dma_start(
                    out=slot[:, 4 * kq:4 * kq + 4, :].rearrange("p a b -> p (a b)"), out_offset=None,
                    in_=src_d[:, :], in_offset=IOA(ap=widx[:, b, m + kq:m + kq + 1], axis=0)),
                    reads=[widx.r()], writes=[slot.r(kq, kq + 1)], dma=slot)
            return slot

        def do_block(b):
            sidx = sidxs[b % 2]
            p.dma("sp", sidx[:], stab_d[b * BS:(b + 1) * BS, :].rearrange("(j p) c -> p j c", p=128), sidx,
                  reads=[stab_d.r()], writes=[sidx.r()])
            for j in range(4):
                p.op("pool", lambda e, j=j: e.indirect_dma_start(
                    out=xs_tok[:, j, :], out_offset=None, in_=toks_d[:, :],
                    in_offset=IOA(ap=sidx[:, j, 0:1], axis=0)),
                    reads=[sidx.r(), toks_d.r()], writes=[xs_tok.r()], dma=xs_tok)
            for kc in range(KC):
                bank = p.bank()
                bb = bank[:].bitcast(BF16)
                for j in range(4):
                    p.op("pe", lambda e, j=j, kc=kc, bb=bb: e.transpose(
                        out=bb[:, j * 128:(j + 1) * 128], in_=xs_tok[:, j, kc * 128:(kc + 1) * 128], identity=ident[:]),
                        reads=[xs_tok.r(), ident.r()], writes=[bank.r()])
                p.op("act", lambda e, kc=kc, bb=bb: e.activation(out=xsT[:, kc, :], in_=bb[:, 0:BS], func=AF.Copy),
                     reads=[bank.r()], writes=[xsT.r(kc, kc + 1)])
            for fg in range(14):
                s1 = wl(W.w1r, b, fg * 4, 4)
                s3 = wl(W.w3r, b, fg * 4, 4)
                for fi in range(4):
                    ft = fg * 4 + fi
                    bA = p.bank()
                    bB = p.bank()
                    for (bk, sl) in ((bA, s1), (bB, s3)):
                        for kc in range(KC):
                            p.op("pe", lambda e, kc=kc, fi=fi, sl=sl, bk=bk: e.matmul(
                                bk[:], lhsT=sl[:, kc, fi * 128:(fi + 1) * 128], rhs=xsT[:, kc, :],
                                start=(kc == 0), stop=(kc == KC - 1)),
                                reads=[sl.r(), xsT.r(kc, kc + 1)], writes=[bk.r()])
                    tmp = tmps[st.ti % 2]
                    st.ti += 1
                    p.op("act", lambda e, bA=bA, tmp=tmp: e.activation(out=tmp[:], in_=bA[:], func=AF.Silu),
                         reads=[bA.r()], writes=[tmp.r()])
                    p.op("dve", lambda e, bB=bB, tmp=tmp, ft=ft: e.tensor_tensor(
                        out=hT[:, ft, :], in0=bB[:], in1=tmp[:], op=ALU.mult),
                        reads=[bB.r(), tmp.r()], writes=[hT.r(ft, ft + 1)])
            for nt in range(4):
                bks = [p.bank() for _ in range(4)]
                for jg in range(14):
                    slot = wl(W.w2r, b, nt * 14 + jg, 1)
                    for j in range(4):
                        for i in range(4):
                            jt = jg * 4 + i
                            p.op("pe", lambda e, jt=jt, i=i, j=j, slot=slot, bk=bks[j]: e.matmul(
                                bk[:], lhsT=hT[:, jt, j * 128:(j + 1) * 128], rhs=slot[:, i, :],
                                start=(jt == 0), stop=(jt == NM - 1)),
                                reads=[slot.r(0, 1), hT.r(jt, jt + 1)], writes=[bks[j].r()])
                for j in range(4):
                    p.op("dve", lambda e, j=j, nt=nt, bk=bks[j]: e.tensor_scalar(
                        out=ys_sb[:, j, nt * 512:(nt + 1) * 512], in0=bk[:],
                        scalar1=sidx[:, j, 1:2].bitcast(F32), scalar2=None, op0=ALU.mult),
                        reads=[bks[j].r(), sidx.r()], writes=[ys_sb.r(j, j + 1)])
            p.dma("sp", ys_d[b * BS:(b + 1) * BS, :].rearrange("(j p) d -> p j d", p=128), ys_sb[:], ys_sb,
                  reads=[ys_sb.r()], writes=[ys_d.r()])

        for b_ in range(nblk):
            do_block(b_)


def phase_combine(p, x2_d, ys_d, idx_d, out_d, modd, ntiles=32):
    with p.phase():
        G2b = p.sb("G2b", [128, D], F32)
        idx = p.sb("idx", [128, 64], I32)
        p.dma("sp", G2b[:], modd.t[1, 0:1, 5 * D:6 * D].partition_broadcast(128), G2b, reads=[modd.r()],
              writes=[G2b.r()])
        p.dma("sp", idx[:], idx_d[:, :], idx, reads=[idx_d.r()], writes=[idx.r()])
        xo = [p.sb("xo%d" % i, [128, D], F32) for i in range(2)]
        yA = [p.sb("yA%d" % i, [128, D], F32) for i in range(2)]
        yB = [p.sb("yB%d" % i, [128, D], F32) for i in range(2)]

        def do_tile(i):
            x_, a_, b_ = xo[i % 2], yA[i % 2], yB[i % 2]
            p.dma("sp", x_[:], x2_d[i * 128:(i + 1) * 128, :], x_, reads=[x2_d.r()], writes=[x_.r()])
            p.op("pool", lambda e: e.indirect_dma_start(out=a_[:], out_offset=None, in_=ys_d[:, :],
                                                        in_offset=IOA(ap=idx[:, i:i + 1], axis=0)),
                 reads=[idx.r(), ys_d.r()], writes=[a_.r()], dma=a_)
            p.op("pool", lambda e: e.indirect_dma_start(out=b_[:], out_offset=None, in_=ys_d[:, :],
                                                        in_offset=IOA(ap=idx[:, 32 + i:33 + i], axis=0)),
                 reads=[idx.r(), ys_d.r()], writes=[b_.r()], dma=b_)
            p.op("dve", lambda e: e.tensor_tensor(out=a_[:], in0=a_[:], in1=b_[:], op=ALU.add),
                 reads=[a_.r(), b_.r()], writes=[a_.r()])
            p.op("dve", lambda e: e.tensor_tensor(out=a_[:], in0=a_[:], in1=G2b[:], op=ALU.mult),
                 reads=[a_.r(), G2b.r()], writes=[a_.r()])
            p.op("dve", lambda e: e.tensor_tensor(out=x_[:], in0=x_[:], in1=a_[:], op=ALU.add),
                 reads=[a_.r(), x_.r()], writes=[x_.r()])
            p.dma("sp", out_d[i * 128:(i + 1) * 128, :], x_[:], x_, reads=[x_.r()], writes=[out_d.r()])

        for i_ in range(ntiles):
            do_tile(i_)


def declare_moe_weights(p, kind="ExternalInput"):
    W = NS()
    W.w1r = p.dr("moe_w1r", [NE * MOE, D], F32, kind=kind)
    W.w3r = p.dr("moe_w3r", [NE * MOE, D], F32, kind=kind)
    W.w2r = p.dr("moe_w2r", [NE * MOE, D], F32, kind=kind)
    return W


def host_moe_weights(inp):
    def r13(w):
        return np.ascontiguousarray(
            w.reshape(NE, 4, 4, 128, 14, 512).transpose(0, 4, 1, 3, 2, 5)).reshape(NE * MOE, D)

    def r2(w):
        return np.ascontiguousarray(
            w.reshape(NE, 14, 4, 128, 4, 512).transpose(0, 4, 1, 3, 2, 5)).reshape(NE * MOE, D)

    return dict(moe_w1r=r13(inp["moe_w1"][0]), moe_w3r=r13(inp["moe_w3"][0]), moe_w2r=r2(inp["moe_w2"][0]))


NB_PER_CORE = 2
N_CORES = 4


def _view(p, buf, bi, name):
    b = Buf(name, buf.t[bi], 1)
    p.bufs.append(b)
    return b


def build_full(nb=NB_PER_CORE):
    nc = bass.Bass("TRN2", target_bir_lowering=False)
    p = Prog(nc)
    p.init_psum()
    cT_a = p.dr("cT", [nb, 128, 32], F32, kind="ExternalInput")
    w_ada_d = p.dr("w_ada", [2 * D, NMOD * D], F32, kind="ExternalInput")
    b_ada_d = p.dr("b_ada", [2, NMOD * D], F32, kind="ExternalInput")
    x_a = p.dr("x", [nb, SEQ, D], F32, kind="ExternalInput")
    ctx_a = p.dr("ctx", [nb, CTX, D], F32, kind="ExternalInput")
    W0 = declare_l0_weights(p)
    W1 = declare_l1_weights(p)
    WM = declare_moe_weights(p)
    out_a = p.dr("out", [nb, SEQ, D], F32, kind="ExternalOutput")
    modd = p.dr("modd", [2, 2, NMOD * D], F32)
    x1_d = p.dr("x1", [SEQ, D], F32)
    xc1_d = p.dr("xc1", [CTX, D], F32)
    qT_d = p.dr("qT", [NH, 128, SEQ], BF16)
    kT_d = p.dr("kT", [NH, 128, NKEY], BF16)
    v_d = p.dr("v", [NKEY, D], BF16)
    OT_d = p.dr("OT", [NH, 128, SEQ], BF16)
    x2_d = p.dr("x2", [SEQ, D], F32)
    toks_d = p.dr("toks", [SEQ, D], BF16)
    rt_d = p.dr("rt", [SEQ, 18], F32)
    ys_d = p.dr("ys", [NSLOT, D], F32)
    stab_d = p.dr("stab", [NSLOT, 2], I32)
    idx_d = p.dr("idx", [128, 64], I32)
    for bi in range(nb):
        cT_d = _view(p, cT_a, bi, "cT%d" % bi)
        x_d = _view(p, x_a, bi, "x%d" % bi)
        ctx_d = _view(p, ctx_a, bi, "ctx%d" % bi)
        out_d = _view(p, out_a, bi, "out%d" % bi)
        phase_adaln(p, cT_d, w_ada_d, b_ada_d, modd)
        phase_l0(p, W0, x_d, ctx_d, x1_d, xc1_d, modd)
        phase_l1a(p, W1, x1_d, xc1_d, qT_d, kT_d, v_d, modd)
        phase_l1b(p, W1, qT_d, kT_d, v_d, OT_d)
        phase_l1c(p, W1, x1_d, OT_d, x2_d, toks_d, rt_d, modd)
        phase_moe(p, WM, toks_d, rt_d, ys_d, stab_d, idx_d)
        phase_combine(p, x2_d, ys_d, idx_d, out_d, modd)
    p.emit()
    return nc


def kernel(**inp):
    inp = {k: np.asarray(v) for k, v in inp.items()}
    nb = NB_PER_CORE
    nc = build_full(nb)
    shared = dict(w_ada=np.ascontiguousarray(inp["w_ada"].reshape(2 * D, NMOD * D)),
                  b_ada=np.ascontiguousarray(inp["b_ada"]))
    shared.update(host_l0_weights(inp))
    shared.update(host_l1_weights(inp))
    shared.update(host_moe_weights(inp))
    maps = []
    for c in range(N_CORES):
        m = dict(shared)
        bs = list(range(c * nb, (c + 1) * nb))
        m["cT"] = np.stack([host_cT(inp, b) for b in bs], axis=0)
        m["x"] = np.ascontiguousarray(inp["x"][bs[0]:bs[-1] + 1])
        m["ctx"] = np.ascontiguousarray(inp["ctx"][bs[0]:bs[-1] + 1])
        maps.append(m)
    res = run_bass_kernel_spmd(nc, maps, core_ids=list(range(N_CORES)))
    out = np.concatenate([np.asarray(res.results[c]["out"]) for c in range(N_CORES)], axis=0)
    return out.astype(np.float32)
```

```python
import contextlib
import numpy as np
import concourse.bass as bass
import concourse.mybir as mybir
from concourse.bass_utils import run_bass_kernel_spmd

F32 = mybir.dt.float32
BF16 = mybir.dt.bfloat16
I32 = mybir.dt.int32
AF = mybir.ActivationFunctionType
ALU = mybir.AluOpType
AX = mybir.AxisListType

D = 2048
KC = 16
SEQ = 4096
CTX = 256
FFN = 5632
NMOD = 6
EPS = 1e-6
ENGS = ("pe", "act", "dve", "pool", "sp")


class Buf:
    def __init__(self, name, t, n=1):
        self.name = name
        self.t = t
        self.n = n
        self.lw = [None] * n
        self.rd = [dict() for _ in range(n)]
        self.sem = None
        self.dmacnt = 0

    def __getitem__(self, k):
        return self.t[k]

    def r(self, lo=0, hi=None):
        return (self, lo, self.n if hi is None else hi)


class Op:
    __slots__ = ("eng", "fn", "deps", "sig", "sigval", "dma_buf", "phase")

    def __init__(self, eng, fn, dma_buf):
        self.eng = eng
        self.fn = fn
        self.deps = None
        self.sig = False
        self.sigval = None
        self.dma_buf = dma_buf


class Prog:
    def __init__(self, nc):
        self.nc = nc
        self.ops = {e: [] for e in ENGS}
        self.allops = []
        self.bufs = []
        self.pending = {e: set() for e in ENGS}
        self.since_barrier = []
        self.last = {e: None for e in ENGS}
        self.stack = contextlib.ExitStack()
        self.pstack = None
        self.banks = []
        self.bi = 0
        self.uid = 0
        self.phase_id = 0

    def init_psum(self):
        for i in range(8):
            t = self.stack.enter_context(self.nc.psum_tensor("bank%d" % i, [128, 512], F32))
            b = Buf("bank%d" % i, t, 1)
            self.bufs.append(b)
            self.banks.append(b)

    def bank(self):
        b = self.banks[self.bi % 8]
        self.bi += 1
        return b

    @contextlib.contextmanager
    def phase(self):
        self.barrier()
        self.phase_id += 1
        self.pstack = contextlib.ExitStack()
        try:
            yield
        finally:
            self.barrier()
            self.pstack.close()
            self.pstack = None

    def sb(self, name, shape, dt, n=1):
        self.uid += 1
        nm = "%s_%d" % (name, self.uid)
        st = self.pstack if self.pstack is not None else self.stack
        t = st.enter_context(self.nc.sbuf_tensor(nm, list(shape), dt))
        b = Buf(nm, t, n)
        self.bufs.append(b)
        return b

    def dr(self, name, shape, dt, kind="Internal", n=1):
        t = self.nc.dram_tensor(name, list(shape), dt, kind=kind)
        b = Buf(name, t, n)
        self.bufs.append(b)
        return b

    def barrier(self):
        deps = set(o for o in self.last.values() if o is not None)
        deps.update(self.since_barrier)
        self.since_barrier = []
        for e in ENGS:
            self.pending[e] = set(deps)

    def op(self, eng, fn, reads=(), writes=(), dma=None):
        o = Op(eng, fn, dma)
        o.phase = self.phase_id
        deps = set()
        rkey = eng if dma is None else ("dma", id(dma))
        for (b, lo, hi) in reads:
            for c in range(lo, hi):
                w = b.lw[c]
                if w is not None:
                    deps.add(w)
                b.rd[c][rkey] = o
        for (b, lo, hi) in writes:
            for c in range(lo, hi):
                w = b.lw[c]
                if w is not None:
                    deps.add(w)
                rd = b.rd[c]
                if rd:
                    deps.update(rd.values())
                    b.rd[c] = dict()
                b.lw[c] = o
        if self.pending[eng]:
            deps.update(self.pending[eng])
            self.pending[eng] = set()
        deps.discard(o)
        if eng == "pe":
            deps = {d for d in deps if not (d.eng == "pe" and d.dma_buf is None)}
        for d in deps:
            d.sig = True
        o.deps = deps
        self.ops[eng].append(o)
        self.allops.append(o)
        self.last[eng] = o
        if dma is not None:
            self.since_barrier.append(o)
        return o

    def dma(self, eng, out_ap, in_ap, sbuf_buf, reads=(), writes=(), **kw):
        def fn(e):
            return e.dma_start(out=out_ap, in_=in_ap, **kw)
        return self.op(eng, fn, reads, writes, dma=sbuf_buf)

    def emit(self):
        nc = self.nc
        st = self.stack
        esem = {}
        for e in ENGS:
            esem[e] = st.enter_context(nc.semaphore("s_" + e))
        free = []
        cur_phase = None
        phase_bufs = []
        for o in self.allops:
            if o.phase != cur_phase:
                for b in phase_bufs:
                    free.append(b.semrec)
                phase_bufs = []
                cur_phase = o.phase
            if o.dma_buf is not None:
                b = o.dma_buf
                if b.sem is None:
                    rec = free.pop() if free else [st.enter_context(nc.semaphore("d_" + b.name)), 0]
                    b.semrec = rec
                    b.sem = rec[0]
                    b.dmacnt = rec[1]
                    phase_bufs.append(b)
                b.dmacnt += 16
                b.semrec[1] = b.dmacnt
                o.sigval = (b.sem, b.dmacnt)
        for e in ENGS:
            cnt = 0
            for o in self.ops[e]:
                if o.dma_buf is not None:
                    pass
                elif o.sig:
                    cnt += 1
                    o.sigval = (esem[e], cnt)
            assert cnt < 65000, (e, cnt)
        block = st.enter_context(nc.Block())
        prog = self

        def run(e, eng):
            waited = {}
            for o in prog.ops[e]:
                need = {}
                for d in o.deps:
                    sem, val = d.sigval
                    k = id(sem)
                    if k not in need or need[k][1] < val:
                        need[k] = (sem, val)
                for k, (sem, val) in need.items():
                    if waited.get(k, 0) < val:
                        eng.wait_ge(sem, val)
                        waited[k] = val
                inst = o.fn(eng)
                if o.sigval is not None:
                    sem, val = o.sigval
                    inst.then_inc(sem, 16 if o.dma_buf is not None else 1)
            if e == "sp":
                for b in prog.bufs:
                    if b.sem is not None and waited.get(id(b.sem), 0) < b.dmacnt:
                        eng.wait_ge(b.sem, b.dmacnt)

        @block.tensor
        def _(eng):
            run("pe", eng)

        @block.scalar
        def _(eng):
            run("act", eng)

        @block.vector
        def _(eng):
            run("dve", eng)

        @block.gpsimd
        def _(eng):
            run("pool", eng)

        @block.sync
        def _(eng):
            run("sp", eng)

        st.close()


class NS:
    pass


def make_ident(p, dt=BF16):
    ident = p.sb("ident", [128, 128], dt)
    p.op("pool", lambda e: e.memset(ident[:], 0.0), writes=[ident.r()])
    p.op("pool", lambda e: e.affine_select(out=ident[:], in_=ident[:], pattern=[[-1, 128]],
                                           compare_op=ALU.not_equal, fill=1.0, base=0,
                                           channel_multiplier=1),
         reads=[ident.r()], writes=[ident.r()])
    return ident


def load_cols(p, dst, src_ap_1d):
    p.dma("sp", dst[:], src_ap_1d.rearrange("(kc p) -> p kc", p=128), dst, writes=[dst.r()],
          allow_slow_non_contiguous=True)


def norm_T(p, S, xres, nsub, Ac, Bc, outT):
    T = nsub * 128
    for j in range(nsub):
        p.op("act", lambda e, j=j: e.activation(out=S.junk[:], in_=xres[:, j, :], func=AF.Square,
                                                accum_out=S.ss[:, j:j + 1]),
             reads=[xres.r(j, j + 1)], writes=[S.ss.r()])
    p.op("act", lambda e: e.activation(out=S.rstd[:, 0:nsub], in_=S.ss[:, 0:nsub], func=AF.Sqrt,
                                       scale=1.0 / D, bias=EPS),
         reads=[S.ss.r()], writes=[S.rstd.r()])
    p.op("dve", lambda e: e.reciprocal(out=S.rstd[:, 0:nsub], in_=S.rstd[:, 0:nsub]),
         reads=[S.rstd.r()], writes=[S.rstd.r()])
    for j in range(nsub):
        p.op("dve", lambda e, j=j: e.tensor_scalar(out=S.xn[:, j, :], in0=xres[:, j, :],
                                                   scalar1=S.rstd[:, j:j + 1], scalar2=None,
                                                   op0=ALU.mult),
             reads=[xres.r(j, j + 1), S.rstd.r()], writes=[S.xn.r(j, j + 1)])
    for kc in range(KC):
        bank = p.bank()
        bb = bank[:].bitcast(BF16)
        for j in range(nsub):
            p.op("pe", lambda e, j=j, kc=kc, bb=bb: e.transpose(
                out=bb[:, j * 128:(j + 1) * 128], in_=S.xn[:, j, kc * 128:(kc + 1) * 128],
                identity=S.ident[:]),
                reads=[S.xn.r(j, j + 1), S.ident.r()], writes=[bank.r()])
        p.op("act", lambda e, kc=kc, bb=bb: e.activation(
            out=outT[:, kc, 0:T], in_=bb[:, 0:T], func=AF.Identity,
            scale=Ac[:, kc:kc + 1], bias=Bc[:, kc:kc + 1]),
            reads=[bank.r(), Ac.r(), Bc.r()], writes=[outT.r(kc, kc + 1)])


def wload(p, S, src_ap, nk, eng="pool"):
    slot = S.wslots[S.wi % len(S.wslots)]
    S.wi += 1
    p.dma(eng, slot[:, 0:nk, :], src_ap, slot, writes=[slot.r()])
    return slot


def wview(w_d, r0, nk, c0, nc_=512):
    return w_d[r0:r0 + nk * 128, c0:c0 + nc_].rearrange("(k p) n -> p k n", p=128)


def phase_adaln(p, cT_d, w_ada_d, b_ada_d, modd, layers=(0, 1)):
    with p.phase():
        cT = p.sb("cT", [128, 32], F32)
        brow = p.sb("brow", [2, NMOD * D], F32)
        modr = p.sb("modr", [2, NMOD * D], F32)
        ws = [p.sb("wada%d" % i, [128, KC, 512], F32) for i in range(2)]
        p.dma("sp", cT[:], cT_d[:], cT, writes=[cT.r()])
        p.op("act", lambda e: e.activation(out=cT[:], in_=cT[:], func=AF.Silu),
             reads=[cT.r()], writes=[cT.r()])
        cT3 = cT[:].rearrange("p (k j) -> p k j", j=2)
        wi = 0
        for i in layers:
            p.dma("sp", brow[:], b_ada_d[i:i + 1, :].partition_broadcast(2), brow, writes=[brow.r()])
            for nt in range(NMOD * D // 512):
                slot = ws[wi % 2]
                wi += 1
                p.dma("sp", slot[:], wview(w_ada_d, i * D, KC, nt * 512), slot, writes=[slot.r()])
                bank = p.bank()
                for kc in range(KC):
                    p.op("pe", lambda e, kc=kc, slot=slot, bank=bank: e.matmul(
                        bank[0:2, :], lhsT=cT3[:, kc, :], rhs=slot[:, kc, :],
                        start=(kc == 0), stop=(kc == KC - 1)),
                        reads=[cT.r(), slot.r()], writes=[bank.r()])
                p.op("dve", lambda e, nt=nt, bank=bank: e.tensor_tensor(
                    out=modr[:, nt * 512:(nt + 1) * 512], in0=bank[0:2, :],
                    in1=brow[:, nt * 512:(nt + 1) * 512], op=ALU.add),
                    reads=[bank.r(), brow.r()], writes=[modr.r()])
            p.dma("sp", modd[i], modr[:], modr, reads=[modr.r()], writes=[modd.r()])


def load_modset(p, S, modd, nwT_d, layer, s):
    m = modd.t
    rd = [modd.r()]
    for (dst, off) in ((S.B1c, 0), (S.sc1, 1), (S.B2c, 3), (S.sc2, 4)):
        p.dma("sp", dst[:], m[layer, s, off * D:(off + 1) * D].rearrange("(kc p) -> p kc", p=128),
              dst, reads=rd, writes=[dst.r()], allow_slow_non_contiguous=True)
    for (dst, off) in ((S.G1b, 2), (S.G2b, 5)):
        p.dma("sp", dst[:], m[layer, s:s + 1, off * D:(off + 1) * D].partition_broadcast(128),
              dst, reads=rd, writes=[dst.r()])
    for (A, sc, k) in ((S.A1c, S.sc1, 0), (S.A2c, S.sc2, 1)):
        p.op("dve", lambda e, A=A, sc=sc, k=k: e.scalar_tensor_tensor(
            out=A[:], in0=sc[:], scalar=1.0, in1=S.nwc[:, k, :], op0=ALU.add, op1=ALU.mult),
            reads=[sc.r(), S.nwc.r()], writes=[A.r()])


def alloc_mod(p, S, nwT_d, layer):
    for nm in ("A1c", "B1c", "A2c", "B2c", "sc1", "sc2"):
        setattr(S, nm, p.sb(nm, [128, KC], F32))
    S.G1b = p.sb("G1b", [128, D], F32)
    S.G2b = p.sb("G2b", [128, D], F32)
    S.nwc = p.sb("nwc", [128, 2, KC], F32)
    p.dma("sp", S.nwc[:], nwT_d[layer], S.nwc, writes=[S.nwc.r()])


def resid_update(p, S, bank, xres, j, nt, Gb):
    cs = slice(nt * 512, (nt + 1) * 512)
    tmp = S.tmps[S.ti % len(S.tmps)]
    S.ti += 1
    p.op("dve", lambda e, tmp=tmp: e.tensor_tensor(out=tmp[:], in0=bank[:], in1=Gb[:, cs], op=ALU.mult),
         reads=[bank.r(), Gb.r()], writes=[tmp.r()])
    p.op("dve", lambda e, tmp=tmp: e.tensor_tensor(out=xres[:, j, cs], in0=xres[:, j, cs], in1=tmp[:],
                                                    op=ALU.add),
         reads=[tmp.r(), xres.r(j, j + 1)], writes=[xres.r(j, j + 1)])


def phase_l0(p, W, x_d, ctx_d, x1_d, xc1_d, modd, tiles=None):
    with p.phase():
        S = NS()
        S.ident = make_ident(p)
        alloc_mod(p, S, W.nwT, 0)
        S.junk = p.sb("junk", [128, D], BF16)
        S.ss = p.sb("ss", [128, 4], F32)
        S.rstd = p.sb("rstd", [128, 4], F32)
        S.xn = p.sb("xn", [128, 4, D], BF16, n=4)
        xres = p.sb("xres", [128, 4, D], F32, n=4)
        actT = p.sb("actT", [128, KC, 512], BF16, n=KC)
        big = p.sb("big", [128, 48, 512], BF16, n=48)
        S.wslots = [p.sb("wslot%d" % i, [128, KC, 512], BF16) for i in range(3)]
        S.wi = 0
        S.tmps = [p.sb("tmp%d" % i, [128, 512], F32) for i in range(2)]
        S.ti = 0
        gvb = p.sb("gvb", [128, D], F32)
        wsT = p.sb("wsT", [128, 16, 128], BF16)
        bsr = p.sb("bsr", [1, 16, 128], F32)
        ones = p.sb("ones", [1, 128], F32)
        p.dma("sp", gvb[:], W.gv[0:1, :].partition_broadcast(128), gvb, writes=[gvb.r()])
        p.dma("pool", wsT[:], W.wsT[:], wsT, writes=[wsT.r()])
        p.dma("sp", bsr[:], W.bs[:], bsr, writes=[bsr.r()])
        p.op("dve", lambda e: e.memset(ones[:], 1.0), writes=[ones.r()])

        if tiles is None:
            tiles = [(0, t * 512, 4) for t in range(SEQ // 512)] + [(1, 0, 2)]
        def do_tile(s, t0, nsub, newset):
            T = nsub * 128
            src = x_d if s == 0 else ctx_d
            dst = x1_d if s == 0 else xc1_d
            if newset:
                load_modset(p, S, modd, W.nwT, 0, s)
            p.dma("sp", xres[:, 0:nsub, :], src[t0:t0 + T, :].rearrange("(j p) d -> p j d", p=128),
                  xres, reads=[src.r()], writes=[xres.r(0, nsub)])
            norm_T(p, S, xres, nsub, S.A1c, S.B1c, actT)
            for fg in range(4):
                slot = wload(p, S, wview(W.w_in, 0, KC, fg * 512), KC)
                for fi in range(4):
                    ft = fg * 4 + fi
                    bank = p.bank()
                    for kc in range(KC):
                        p.op("pe", lambda e, kc=kc, fi=fi, slot=slot, bank=bank: e.matmul(
                            bank[:, 0:T], lhsT=slot[:, kc, fi * 128:(fi + 1) * 128], rhs=actT[:, kc, 0:T],
                            start=(kc == 0), stop=(kc == KC - 1)),
                            reads=[slot.r(), actT.r(kc, kc + 1)], writes=[bank.r()])
                    p.op("act", lambda e, ft=ft, bank=bank: e.activation(
                        out=big[:, ft, 0:T], in_=bank[:, 0:T], func=AF.Gelu),
                        reads=[bank.r()], writes=[big.r(ft, ft + 1)])
            for nt in range(4):
                slot = wload(p, S, wview(W.w_in, 0, KC, D + nt * 512), KC)
                for j in range(nsub):
                    bank = p.bank()
                    for kc in range(KC):
                        p.op("pe", lambda e, kc=kc, j=j, slot=slot, bank=bank: e.matmul(
                            bank[:], lhsT=actT[:, kc, j * 128:(j + 1) * 128], rhs=slot[:, kc, :],
                            start=(kc == 0), stop=(kc == KC - 1)),
                            reads=[slot.r(), actT.r(kc, kc + 1)], writes=[bank.r()])
                    c = 16 + 4 * j + nt
                    p.op("act", lambda e, c=c, bank=bank: e.activation(
                        out=big[:, c, :], in_=bank[:], func=AF.Gelu),
                        reads=[bank.r()], writes=[big.r(c, c + 1)])
            for j in range(nsub):
                vj = big[:, 16 + 4 * j:20 + 4 * j, :]
                p.op("act", lambda e, j=j, vj=vj: e.activation(
                    out=S.junk[:].rearrange("p (a b) -> p a b", a=4), in_=vj, func=AF.Square,
                    accum_out=S.ss[:, j:j + 1]),
                    reads=[big.r(16 + 4 * j, 20 + 4 * j)], writes=[S.ss.r()])
            p.op("act", lambda e: e.activation(out=S.rstd[:, 0:nsub], in_=S.ss[:, 0:nsub], func=AF.Sqrt,
                                               scale=1.0 / D, bias=EPS),
                 reads=[S.ss.r()], writes=[S.rstd.r()])
            p.op("dve", lambda e: e.reciprocal(out=S.rstd[:, 0:nsub], in_=S.rstd[:, 0:nsub]),
                 reads=[S.rstd.r()], writes=[S.rstd.r()])
            for j in range(nsub):
                vj = big[:, 16 + 4 * j:20 + 4 * j, :]
                p.op("dve", lambda e, j=j, vj=vj: e.scalar_tensor_tensor(
                    out=vj, in0=vj, scalar=S.rstd[:, j:j + 1],
                    in1=gvb[:].rearrange("p (a b) -> p a b", a=4), op0=ALU.mult, op1=ALU.mult),
                    reads=[big.r(16 + 4 * j, 20 + 4 * j), S.rstd.r(), gvb.r()],
                    writes=[big.r(16 + 4 * j, 20 + 4 * j)])
            for g in range(16):
                bank = p.bank()
                p.op("pe", lambda e, g=g, bank=bank: e.matmul(
                    bank[:, 0:T].rearrange("p (j q) -> p j q", q=128), lhsT=ones[0:1, :],
                    rhs=bsr[0:1, g:g + 1, :].to_broadcast([1, nsub, 128]),
                    start=True, stop=False),
                    reads=[ones.r(), bsr.r()], writes=[bank.r()])
                for j in range(nsub):
                    c = 16 + 4 * j + g // 4
                    p.op("pe", lambda e, g=g, j=j, c=c, bank=bank: e.matmul(
                        bank[:, j * 128:(j + 1) * 128],
                        lhsT=big[:, c, (g % 4) * 128:(g % 4 + 1) * 128], rhs=wsT[:, g, :],
                        start=False, stop=(j == nsub - 1)),
                        reads=[big.r(c, c + 1), wsT.r()], writes=[bank.r()])
                p.op("dve", lambda e, g=g, bank=bank: e.tensor_tensor(
                    out=big[:, 32 + g, 0:T], in0=bank[:, 0:T], in1=big[:, g, 0:T], op=ALU.mult),
                    reads=[bank.r(), big.r(g, g + 1)], writes=[big.r(32 + g, 33 + g)])
            for nt in range(4):
                slot = wload(p, S, wview(W.w_out, 0, KC, nt * 512), KC)
                for j in range(nsub):
                    bank = p.bank()
                    for kc in range(KC):
                        p.op("pe", lambda e, kc=kc, j=j, slot=slot, bank=bank: e.matmul(
                            bank[:], lhsT=big[:, 32 + kc, j * 128:(j + 1) * 128], rhs=slot[:, kc, :],
                            start=(kc == 0), stop=(kc == KC - 1)),
                            reads=[slot.r(), big.r(32 + kc, 33 + kc)], writes=[bank.r()])
                    resid_update(p, S, bank, xres, j, nt, S.G1b)
            norm_T(p, S, xres, nsub, S.A2c, S.B2c, actT)
            for fg in range(FFN // 512):
                s1 = wload(p, S, wview(W.w1, 0, KC, fg * 512), KC)
                s3 = wload(p, S, wview(W.w3, 0, KC, fg * 512), KC)
                for fi in range(4):
                    ft = fg * 4 + fi
                    bA = p.bank()
                    bB = p.bank()
                    for (bk, sl) in ((bA, s1), (bB, s3)):
                        for kc in range(KC):
                            p.op("pe", lambda e, kc=kc, fi=fi, sl=sl, bk=bk: e.matmul(
                                bk[:, 0:T], lhsT=sl[:, kc, fi * 128:(fi + 1) * 128], rhs=actT[:, kc, 0:T],
                                start=(kc == 0), stop=(kc == KC - 1)),
                                reads=[sl.r(), actT.r(kc, kc + 1)], writes=[bk.r()])
                    tmp = S.tmps[S.ti % 2]
                    S.ti += 1
                    p.op("act", lambda e, bA=bA, tmp=tmp: e.activation(out=tmp[:, 0:T], in_=bA[:, 0:T], func=AF.Silu),
                         reads=[bA.r()], writes=[tmp.r()])
                    p.op("dve", lambda e, bB=bB, tmp=tmp, ft=ft: e.tensor_tensor(
                        out=big[:, ft, 0:T], in0=bB[:, 0:T], in1=tmp[:, 0:T], op=ALU.mult),
                        reads=[bB.r(), tmp.r()], writes=[big.r(ft, ft + 1)])
            for nt in range(4):
                bks = [p.bank() for _ in range(nsub)]
                for kg in range(4):
                    slot = wload(p, S, wview(W.w2, kg * 11 * 128, 11, nt * 512), 11)
                    for j in range(nsub):
                        for i in range(11):
                            kc = kg * 11 + i
                            p.op("pe", lambda e, kc=kc, i=i, j=j, slot=slot, bk=bks[j]: e.matmul(
                                bk[:], lhsT=big[:, kc, j * 128:(j + 1) * 128], rhs=slot[:, i, :],
                                start=(kc == 0), stop=(kc == 43)),
                                reads=[slot.r(), big.r(kc, kc + 1)], writes=[bks[j].r()])
                for j in range(nsub):
                    resid_update(p, S, bks[j], xres, j, nt, S.G2b)
            p.dma("sp", dst[t0:t0 + T, :].rearrange("(j p) d -> p j d", p=128), xres[:, 0:nsub, :],
                  xres, reads=[xres.r(0, nsub)], writes=[dst.r()])

        cur_set = None
        for (s_, t0_, nsub_) in tiles:
            do_tile(s_, t0_, nsub_, s_ != cur_set)
            cur_set = s_


def declare_l0_weights(p, kind="ExternalInput"):
    W = NS()
    W.nwT = p.dr("nwT", [2, 128, 2, KC], F32, kind=kind)
    W.w_in = p.dr("mlp_w_in", [D, 2 * D], F32, kind=kind)
    W.gv = p.dr("mlp_g_v", [1, D], F32, kind=kind)
    W.wsT = p.dr("mlp_wsT", [128, 16, 128], F32, kind=kind)
    W.bs = p.dr("mlp_b_s", [1, 16, 128], F32, kind=kind)
    W.w_out = p.dr("mlp_w_out", [D, D], F32, kind=kind)
    W.w1 = p.dr("ffn_w1", [D, FFN], F32, kind=kind)
    W.w3 = p.dr("ffn_w3", [D, FFN], F32, kind=kind)
    W.w2 = p.dr("ffn_w2", [FFN, D], F32, kind=kind)
    return W


def host_l0_weights(inp):
    nw = inp["norm_w"]
    nwT = np.ascontiguousarray(nw.reshape(2, 2, KC, 128).transpose(0, 3, 1, 2))
    return dict(
        nwT=nwT,
        mlp_w_in=np.ascontiguousarray(inp["mlp_w_in"][0]),
        mlp_g_v=np.ascontiguousarray(inp["mlp_g_v"][0:1]),
        mlp_wsT=np.ascontiguousarray(inp["mlp_w_s"][0].transpose(2, 0, 1)),
        mlp_b_s=np.ascontiguousarray(inp["mlp_b_s"][0][None]),
        mlp_w_out=np.ascontiguousarray(inp["mlp_w_out"][0]),
        ffn_w1=np.ascontiguousarray(inp["ffn_w1"][0]),
        ffn_w3=np.ascontiguousarray(inp["ffn_w3"][0]),
        ffn_w2=np.ascontiguousarray(inp["ffn_w2"][0]),
    )


def host_cT(inp, b):
    cc = np.stack([inp["c"][b], inp["c_ctx"]], axis=-1)
    return np.ascontiguousarray(cc.reshape(KC, 128, 2).transpose(1, 0, 2).reshape(128, 32))


def build_l0(tiles=None, layers=(0, 1)):
    nc = bass.Bass("TRN2", target_bir_lowering=False)
    p = Prog(nc)
    p.init_psum()
    cT_d = p.dr("cT", [128, 32], F32, kind="ExternalInput")
    w_ada_d = p.dr("w_ada", [2 * D, NMOD * D], F32, kind="ExternalInput")
    b_ada_d = p.dr("b_ada", [2, NMOD * D], F32, kind="ExternalInput")
    x_d = p.dr("x", [SEQ, D], F32, kind="ExternalInput")
    ctx_d = p.dr("ctx", [CTX, D], F32, kind="ExternalInput")
    W = declare_l0_weights(p)
    modd = p.dr("modd", [2, 2, NMOD * D], F32, kind="ExternalOutput")
    x1_d = p.dr("x1", [SEQ, D], F32, kind="ExternalOutput")
    xc1_d = p.dr("xc1", [CTX, D], F32, kind="ExternalOutput")
    phase_adaln(p, cT_d, w_ada_d, b_ada_d, modd, layers)
    phase_l0(p, W, x_d, ctx_d, x1_d, xc1_d, modd, tiles)
    p.emit()
    return nc


NH = 16
NKEY = SEQ + CTX
NEG = -30000.0


class BankPool:
    def __init__(self, p, ids):
        self.b = [p.banks[i] for i in ids]
        self.i = 0

    def next(self):
        b = self.b[self.i % len(self.b)]
        self.i += 1
        return b


def l1_variant(g, j):
    if g == 0:
        return j, j
    if g == 7:
        return 26 + j, 14 + j
    return 4 * g - 2 + j, 6 + j


def l1_ntiles(g):
    return 6 if g in (0, 7) else 8


def host_bias_table(rpb):
    out = np.empty((NH, 20, 128, 512), np.float32)
    kk = np.arange(128)
    qq = np.arange(512)
    for g in (0, 1, 7):
        for j in range(l1_ntiles(g)):
            kt, var = l1_variant(g, j)
            kr = (2 * kt + kk // 64)[:, None]
            kc = (kk % 64)[:, None]
            qr = (8 * g + qq // 64)[None, :]
            qc = (qq % 64)[None, :]
            rs = np.clip(qr - 4, 0, 56)
            cs = np.clip(qc - 8, 0, 48)
            valid = (kr >= rs) & (kr < rs + 8) & (kc >= cs) & (kc < cs + 16)
            dr = np.clip(kr - qr + 7, 0, 14)
            dc = np.clip(kc - qc + 15, 0, 30)
            out[:, var] = np.where(valid[None], rpb[:, dr, dc], np.float32(NEG))
    return out


def phase_l1a(p, W, x1_d, xc1_d, qT_d, kT_d, v_d, modd, tiles=None):
    with p.phase():
        S = NS()
        S.ident = make_ident(p)
        alloc_mod(p, S, W.nwT, 1)
        S.junk = p.sb("junk", [128, D], BF16)
        S.ss = p.sb("ss", [128, 4], F32)
        S.rstd = p.sb("rstd", [128, 4], F32)
        S.xn = p.sb("xn", [128, 4, D], BF16, n=4)
        xres = p.sb("xres", [128, 4, D], F32, n=4)
        actT = p.sb("actT", [128, KC, 512], BF16, n=KC)
        S.wslots = [p.sb("wslot%d" % i, [128, KC, 512], BF16) for i in range(3)]
        S.wi = 0
        qk_sb = [p.sb("qsb", [128, NH, 512], BF16, n=NH), p.sb("ksb", [128, NH, 512], BF16, n=NH)]
        v_sb = p.sb("vsb", [128, 4, D], BF16, n=4)
        sqs = [p.sb("sq%d" % i, [128, 512], BF16) for i in range(2)]
        rr = [p.sb("rr%d" % i, [128, 512], F32) for i in range(2)]
        ones = p.sb("onesb", [128, 128], BF16)
        gcol = p.sb("gcol", [128, 2], F32)
        p.op("dve", lambda e: e.memset(ones[:], 1.0), writes=[ones.r()])
        p.dma("sp", gcol[:], W.gqk[:], gcol, writes=[gcol.r()])
        p.op("dve", lambda e: e.tensor_scalar(out=gcol[:, 0:1], in0=gcol[:, 0:1], scalar1=128.0 ** -0.5,
                                              scalar2=None, op0=ALU.mult),
             reads=[gcol.r()], writes=[gcol.r()])
        if tiles is None:
            tiles = [(0, t * 512, 4) for t in range(SEQ // 512)] + [(1, 0, 2)]
        cnt_box = [0]

        def do_tile(s, t0, nsub, newset):
            T = nsub * 128
            src = x1_d if s == 0 else xc1_d
            if newset:
                load_modset(p, S, modd, W.nwT, 1, s)
            p.dma("sp", xres[:, 0:nsub, :], src[t0:t0 + T, :].rearrange("(j p) d -> p j d", p=128),
                  xres, reads=[src.r()], writes=[xres.r(0, nsub)])
            norm_T(p, S, xres, nsub, S.A1c, S.B1c, actT)
            parts = (0, 1) if s == 0 else (1,)
            pend = None

            def finish(pd):
                (part, h, bankQ, sq, r) = pd
                bankS = p.bank()
                p.op("pe", lambda e: e.matmul(bankS[:, 0:T], lhsT=ones[:], rhs=sq[:, 0:T], start=True, stop=True),
                     reads=[ones.r(), sq.r()], writes=[bankS.r()])
                p.op("act", lambda e: e.activation(out=r[:, 0:T], in_=bankS[:, 0:T], func=AF.Sqrt,
                                                   scale=1.0 / 128, bias=EPS),
                     reads=[bankS.r()], writes=[r.r()])
                p.op("dve", lambda e: e.reciprocal(out=r[:, 0:T], in_=r[:, 0:T]), reads=[r.r()], writes=[r.r()])
                p.op("dve", lambda e: e.scalar_tensor_tensor(
                    out=qk_sb[part][:, h, 0:T], in0=bankQ[:, 0:T], scalar=gcol[:, part:part + 1],
                    in1=r[:, 0:T], op0=ALU.mult, op1=ALU.mult),
                    reads=[bankQ.r(), gcol.r(), r.r()], writes=[qk_sb[part].r(h, h + 1)])

            for part in parts:
                for hg in range(4):
                    slot = wload(p, S, wview(W.w_qkv, 0, KC, part * D + hg * 512), KC)
                    for hi in range(4):
                        h = hg * 4 + hi
                        bankQ = p.bank()
                        for kc in range(KC):
                            p.op("pe", lambda e, kc=kc, hi=hi, slot=slot, bankQ=bankQ: e.matmul(
                                bankQ[:, 0:T], lhsT=slot[:, kc, hi * 128:(hi + 1) * 128], rhs=actT[:, kc, 0:T],
                                start=(kc == 0), stop=(kc == KC - 1)),
                                reads=[slot.r(), actT.r(kc, kc + 1)], writes=[bankQ.r()])
                        sq = sqs[cnt_box[0] % 2]
                        r = rr[cnt_box[0] % 2]
                        cnt_box[0] += 1
                        p.op("act", lambda e, sq=sq, bankQ=bankQ: e.activation(out=sq[:, 0:T], in_=bankQ[:, 0:T],
                                                                               func=AF.Square),
                             reads=[bankQ.r()], writes=[sq.r()])
                        if pend is not None:
                            finish(pend)
                        pend = (part, h, bankQ, sq, r)
            finish(pend)
            for nt in range(4):
                slot = wload(p, S, wview(W.w_qkv, 0, KC, 2 * D + nt * 512), KC)
                for j in range(nsub):
                    bank = p.bank()
                    for kc in range(KC):
                        p.op("pe", lambda e, kc=kc, j=j, slot=slot, bank=bank: e.matmul(
                            bank[:], lhsT=actT[:, kc, j * 128:(j + 1) * 128], rhs=slot[:, kc, :],
                            start=(kc == 0), stop=(kc == KC - 1)),
                            reads=[slot.r(), actT.r(kc, kc + 1)], writes=[bank.r()])
                    p.op("act", lambda e, j=j, nt=nt, bank=bank: e.activation(
                        out=v_sb[:, j, nt * 512:(nt + 1) * 512], in_=bank[:], func=AF.Copy),
                        reads=[bank.r()], writes=[v_sb.r(j, j + 1)])
            kcol = t0 if s == 0 else SEQ + t0
            if s == 0:
                p.dma("sp", qT_d[:, :, t0:t0 + T].rearrange("h p t -> p h t"), qk_sb[0][:, :, 0:T],
                      qk_sb[0], reads=[qk_sb[0].r()], writes=[qT_d.r()])
            p.dma("sp", kT_d[:, :, kcol:kcol + T].rearrange("h p t -> p h t"), qk_sb[1][:, :, 0:T],
                  qk_sb[1], reads=[qk_sb[1].r()], writes=[kT_d.r()])
            p.dma("sp", v_d[kcol:kcol + T, :].rearrange("(j p) d -> p j d", p=128), v_sb[:, 0:nsub, :],
                  v_sb, reads=[v_sb.r(0, nsub)], writes=[v_d.r()])

        cur_set = None
        for (s_, t0_, nsub_) in tiles:
            do_tile(s_, t0_, nsub_, s_ != cur_set)
            cur_set = s_


def phase_l1b(p, W, qT_d, kT_d, v_d, OT_d, heads=None, groups=None):
    with p.phase():
        ones = p.sb("onesb", [128, 128], BF16)
        p.op("dve", lambda e: e.memset(ones[:], 1.0), writes=[ones.r()])
        NB = 2
        qh = [p.sb("qh%d" % i, [128, SEQ], BF16) for i in range(NB)]
        kh = [p.sb("kh%d" % i, [128, NKEY], BF16) for i in range(NB)]
        vh = [p.sb("vh%d" % i, [128, NKEY // 128, 128], BF16) for i in range(NB)]
        tb = [p.sb("tb%d" % i, [128, 20, 512], F32) for i in range(NB)]
        OTs = [p.sb("OTs%d" % i, [128, SEQ], BF16) for i in range(NB)]
        pTs = [p.sb("pT%d" % i, [128, 512], BF16) for i in range(4)]
        tmps = [p.sb("tmpa%d" % i, [128, 512], F32) for i in range(3)]
        rDs = [p.sb("rD%d" % i, [128, 512], F32) for i in range(2)]
        od_pool = BankPool(p, [0, 1, 2, 3])
        s_pool = BankPool(p, [4, 5, 6, 7])
        ci = 0
        if heads is None:
            heads = list(range(NH))
        if groups is None:
            groups = list(range(8))
        ci_box = [0]

        def do_head(hn, h):
            b_ = hn % NB
            q_, k_, v_, t_, O_ = qh[b_], kh[b_], vh[b_], tb[b_], OTs[b_]
            p.dma("sp", q_[:], qT_d[h], q_, reads=[qT_d.r()], writes=[q_.r()])
            p.dma("sp", k_[:], kT_d[h], k_, reads=[kT_d.r()], writes=[k_.r()])
            p.dma("sp", v_[:], v_d[:, h * 128:(h + 1) * 128].rearrange("(kt p) c -> p kt c", p=128), v_,
                  reads=[v_d.r()], writes=[v_.r()])
            p.dma("sp", t_[:], W.tbl[h].rearrange("v p q -> p v q"), t_, writes=[t_.r()])
            def do_group(g):
                tl = [l1_variant(g, j) for j in range(l1_ntiles(g))] + [(32, None), (33, None)]
                bankO = od_pool.next()
                bankD = od_pool.next()
                pend = None
                n = len(tl)

                def pv(pd):
                    (idx, kt, pT) = pd
                    p.op("pe", lambda e: e.matmul(bankO[:], lhsT=v_[:, kt, :], rhs=pT[:], start=(idx == 0),
                                                  stop=(idx == n - 1)),
                         reads=[v_.r(), pT.r()], writes=[bankO.r()])
                    p.op("pe", lambda e: e.matmul(bankD[:], lhsT=ones[:], rhs=pT[:], start=(idx == 0),
                                                  stop=(idx == n - 1)),
                         reads=[ones.r(), pT.r()], writes=[bankD.r()])

                for idx, (kt, var) in enumerate(tl):
                    bankS = s_pool.next()
                    p.op("pe", lambda e, kt=kt, bankS=bankS: e.matmul(
                        bankS[:], lhsT=k_[:, kt * 128:(kt + 1) * 128], rhs=q_[:, g * 512:(g + 1) * 512],
                        start=True, stop=True),
                        reads=[k_.r(), q_.r()], writes=[bankS.r()])
                    ci = ci_box[0]
                    pT = pTs[ci % 4]
                    if var is not None:
                        tmp = tmps[ci % 3]
                        p.op("dve", lambda e, var=var, bankS=bankS, tmp=tmp: e.tensor_tensor(
                            out=tmp[:], in0=bankS[:], in1=t_[:, var, :], op=ALU.add),
                            reads=[bankS.r(), t_.r()], writes=[tmp.r()])
                        p.op("act", lambda e, tmp=tmp, pT=pT: e.activation(out=pT[:], in_=tmp[:], func=AF.Exp),
                             reads=[tmp.r()], writes=[pT.r()])
                    else:
                        p.op("act", lambda e, bankS=bankS, pT=pT: e.activation(out=pT[:], in_=bankS[:], func=AF.Exp),
                             reads=[bankS.r()], writes=[pT.r()])
                    ci_box[0] += 1
                    if pend is not None:
                        pv(pend)
                    pend = (idx, kt, pT)
                pv(pend)
                rD = rDs[ci_box[0] % 2]
                p.op("dve", lambda e, rD=rD: e.reciprocal(out=rD[:], in_=bankD[:]), reads=[bankD.r()], writes=[rD.r()])
                p.op("dve", lambda e, rD=rD, g=g: e.tensor_tensor(out=O_[:, g * 512:(g + 1) * 512], in0=bankO[:],
                                                                   in1=rD[:], op=ALU.mult),
                     reads=[bankO.r(), rD.r()], writes=[O_.r()])
            for g_ in groups:
                do_group(g_)
            p.dma("sp", OT_d[h], O_[:], O_, reads=[O_.r()], writes=[OT_d.r()])

        for hn_, h_ in enumerate(heads):
            do_head(hn_, h_)


def phase_l1c(p, W, x1_d, OT_d, x2_d, toks_d, rt_d, modd, ntiles=None):
    with p.phase():
        S = NS()
        xres = p.sb("xres", [128, 4, D], F32, n=4)
        OTt = p.sb("OTt", [128, NH, 512], BF16)
        S.wslots = [p.sb("wslot%d" % i, [128, KC, 512], BF16) for i in range(2)]
        S.wi = 0
        S.tmps = [p.sb("tmp%d" % i, [128, 512], F32) for i in range(2)]
        S.ti = 0
        G1b = p.sb("G1b", [128, D], F32)
        A2b = p.sb("A2b", [128, D], F32)
        B2b = p.sb("B2b", [128, D], F32)
        nwb = p.sb("nwb", [128, D], BF16)
        wrb = p.sb("wrb", [128, 8, D], F32)
        tk = [p.sb("tk%d" % i, [128, D], F32) for i in range(2)]
        ss = p.sb("ss", [128, 4], F32)
        rstd = p.sb("rstd", [128, 4], F32)
        lg = p.sb("lg", [128, 8], F32)
        srt = p.sb("srt", [128, 8], F32)
        dl = p.sb("dl", [128, 1], F32)
        rt = p.sb("rt", [128, 4, 18], F32)
        m = modd.t
        p.dma("sp", G1b[:], m[1, 0:1, 2 * D:3 * D].partition_broadcast(128), G1b, reads=[modd.r()], writes=[G1b.r()])
        p.dma("sp", B2b[:], m[1, 0:1, 3 * D:4 * D].partition_broadcast(128), B2b, reads=[modd.r()], writes=[B2b.r()])
        p.dma("sp", A2b[:], m[1, 0:1, 4 * D:5 * D].partition_broadcast(128), A2b, reads=[modd.r()], writes=[A2b.r()])
        p.dma("sp", tk[0][:], W.nwrow[1:2, :].partition_broadcast(128), tk[0], writes=[tk[0].r()])
        p.op("dve", lambda e: e.scalar_tensor_tensor(out=A2b[:], in0=A2b[:], scalar=1.0, in1=tk[0][:],
                                                     op0=ALU.add, op1=ALU.mult),
             reads=[A2b.r(), tk[0].r()], writes=[A2b.r()])
        p.dma("sp", wrb[:].rearrange("p e d -> p (e d)"),
              W.wrT[:].rearrange("(o e) d -> o (e d)", o=1).partition_broadcast(128), wrb, writes=[wrb.r()])
        if ntiles is None:
            ntiles = list(range(SEQ // 512))
        ti_box = [0]

        def do_tile(t):
            t0 = t * 512
            p.dma("sp", xres[:], x1_d[t0:t0 + 512, :].rearrange("(j p) d -> p j d", p=128),
                  xres, reads=[x1_d.r()], writes=[xres.r()])
            p.dma("sp", OTt[:], OT_d[:, :, t0:t0 + 512].rearrange("h p t -> p h t"), OTt,
                  reads=[OT_d.r()], writes=[OTt.r()])
            for nt in range(4):
                slot = wload(p, S, wview(W.w_o, 0, KC, nt * 512), KC)
                for j in range(4):
                    bank = p.bank()
                    for h in range(NH):
                        p.op("pe", lambda e, h=h, j=j, slot=slot, bank=bank: e.matmul(
                            bank[:], lhsT=OTt[:, h, j * 128:(j + 1) * 128], rhs=slot[:, h, :],
                            start=(h == 0), stop=(h == NH - 1)),
                            reads=[slot.r(), OTt.r()], writes=[bank.r()])
                    resid_update(p, S, bank, xres, j, nt, G1b)
            p.dma("sp", x2_d[t0:t0 + 512, :].rearrange("(j p) d -> p j d", p=128), xres[:], xres,
                  reads=[xres.r()], writes=[x2_d.r()])
            for j in range(4):
                p.op("act", lambda e, j=j: e.activation(out=nwb[:], in_=xres[:, j, :], func=AF.Square,
                                                        accum_out=ss[:, j:j + 1]),
                     reads=[xres.r(j, j + 1)], writes=[ss.r()])
            p.op("act", lambda e: e.activation(out=rstd[:], in_=ss[:], func=AF.Sqrt, scale=1.0 / D, bias=EPS),
                 reads=[ss.r()], writes=[rstd.r()])
            p.op("dve", lambda e: e.reciprocal(out=rstd[:], in_=rstd[:]), reads=[rstd.r()], writes=[rstd.r()])
            for j in range(4):
                tkb = tk[ti_box[0] % 2]
                ti_box[0] += 1
                p.op("dve", lambda e, j=j, tkb=tkb: e.scalar_tensor_tensor(
                    out=tkb[:], in0=xres[:, j, :], scalar=rstd[:, j:j + 1], in1=A2b[:], op0=ALU.mult, op1=ALU.mult),
                    reads=[xres.r(j, j + 1), rstd.r(), A2b.r()], writes=[tkb.r()])
                p.op("dve", lambda e, tkb=tkb: e.tensor_tensor(out=tkb[:], in0=tkb[:], in1=B2b[:], op=ALU.add),
                     reads=[tkb.r(), B2b.r()], writes=[tkb.r()])
                p.dma("pool", toks_d[t0 + j * 128:t0 + (j + 1) * 128, :], tkb[:], tkb, reads=[tkb.r()],
                      writes=[toks_d.r()])
                for e_ in range(8):
                    p.op("dve", lambda e, e_=e_, tkb=tkb: e.scalar_tensor_tensor(
                        out=nwb[:], in0=tkb[:], scalar=1.0, in1=wrb[:, e_, :], op0=ALU.mult, op1=ALU.mult,
                        accum_out=lg[:, e_:e_ + 1]),
                        reads=[tkb.r(), wrb.r()], writes=[lg.r()])
                p.op("dve", lambda e: e.max(out=srt[:], in_=lg[:]), reads=[lg.r()], writes=[srt.r()])
                p.op("dve", lambda e, j=j: e.tensor_scalar(out=rt[:, j, 0:8], in0=lg[:], scalar1=srt[:, 0:1],
                                                           scalar2=None, op0=ALU.is_equal),
                     reads=[lg.r(), srt.r()], writes=[rt.r()])
                p.op("dve", lambda e, j=j: e.tensor_scalar(out=rt[:, j, 8:16], in0=lg[:], scalar1=srt[:, 1:2],
                                                           scalar2=None, op0=ALU.is_equal),
                     reads=[lg.r(), srt.r()], writes=[rt.r()])
                p.op("dve", lambda e: e.tensor_tensor(out=dl[:], in0=srt[:, 0:1], in1=srt[:, 1:2], op=ALU.subtract),
                     reads=[srt.r()], writes=[dl.r()])
                p.op("act", lambda e, j=j: e.activation(out=rt[:, j, 16:17], in_=dl[:], func=AF.Sigmoid),
                     reads=[dl.r()], writes=[rt.r()])
                p.op("act", lambda e, j=j: e.activation(out=rt[:, j, 17:18], in_=dl[:], func=AF.Sigmoid, scale=-1.0),
                     reads=[dl.r()], writes=[rt.r()])
            p.dma("sp", rt_d[t0:t0 + 512, :].rearrange("(j p) c -> p j c", p=128), rt[:], rt,
                  reads=[rt.r()], writes=[rt_d.r()])

        for t_ in ntiles:
            do_tile(t_)


def declare_l1_weights(p, kind="ExternalInput"):
    W = NS()
    W.nwT = p.dr("nwT1", [2, 128, 2, KC], F32, kind=kind)
    W.nwrow = p.dr("nwrow", [2, D], F32, kind=kind)
    W.w_qkv = p.dr("na_w_qkv", [D, 3 * D], F32, kind=kind)
    W.gqk = p.dr("na_gqk", [128, 2], F32, kind=kind)
    W.tbl = p.dr("na_tbl", [NH, 20, 128, 512], F32, kind=kind)
    W.w_o = p.dr("na_w_o", [D, D], F32, kind=kind)
    W.wrT = p.dr("moe_wrT", [8, D], F32, kind=kind)
    return W


def host_l1_weights(inp):
    nw = inp["norm_w"]
    nwT = np.ascontiguousarray(nw.reshape(2, 2, KC, 128).transpose(0, 3, 1, 2))
    return dict(
        nwT1=nwT,
        nwrow=np.ascontiguousarray(nw[1]),
        na_w_qkv=np.ascontiguousarray(inp["na_w_qkv"][0]),
        na_gqk=np.ascontiguousarray(np.stack([inp["na_g_q"][0], inp["na_g_k"][0]], axis=-1)),
        na_tbl=host_bias_table(inp["na_rpb"][0]),
        na_w_o=np.ascontiguousarray(inp["na_w_o"][0]),
        moe_wrT=np.ascontiguousarray(inp["moe_w_router"][0].T),
    )


NE = 8
MOE = 7168
BS = 512
NBLK = SEQ * 2 // BS + NE
NSLOT = NBLK * BS
NM = 56
IOA = bass.IndirectOffsetOnAxis


def phase_moe(p, W, toks_d, rt_d, ys_d, stab_d, idx_d, nblk=NBLK):
    with p.phase():
        ident = make_ident(p)
        ones = p.sb("onesb", [128, 128], BF16)
        p.op("dve", lambda e: e.memset(ones[:], 1.0), writes=[ones.r()])
        rt_all = p.sb("rt_all", [128, 32, 18], F32)
        p.dma("sp", rt_all[:], rt_d[:, :].rearrange("(i p) c -> p i c", p=128), rt_all,
              reads=[rt_d.r()], writes=[rt_all.r()])
        Mb = p.sb("Mb", [128, 32, 8], BF16)
        p.op("dve", lambda e: e.tensor_tensor(out=Mb[:], in0=rt_all[:, :, 0:8], in1=rt_all[:, :, 8:16], op=ALU.add),
             reads=[rt_all.r()], writes=[Mb.r()])
        Ls = p.sb("Ls", [128, 128], BF16)
        p.op("pool", lambda e: e.memset(Ls[:], 0.0), writes=[Ls.r()])
        p.op("pool", lambda e: e.affine_select(out=Ls[:], in_=Ls[:], pattern=[[-1, 128]], compare_op=ALU.is_ge,
                                               fill=1.0, base=0, channel_multiplier=1),
             reads=[Ls.r()], writes=[Ls.r()])
        bankC = p.banks[0]
        bankW = p.banks[1]
        p.op("pe", lambda e: e.matmul(bankC[:, 0:256], lhsT=ones[:], rhs=Mb[:].rearrange("p i e -> p (i e)"),
                                      start=True, stop=True),
             reads=[ones.r(), Mb.r()], writes=[bankC.r()])
        for i in range(32):
            p.op("pe", lambda e, i=i: e.matmul(bankW[:, i * 8:(i + 1) * 8], lhsT=Ls[:], rhs=Mb[:, i, :],
                                               start=True, stop=True),
                 reads=[Ls.r(), Mb.r()], writes=[bankW.r()])
        cs = p.sb("cs", [128, 32, 8], F32)
        off = p.sb("off", [128, 32, 8], F32)
        p.op("act", lambda e: e.activation(out=cs[:].rearrange("p i e -> p (i e)"), in_=bankC[:, 0:256], func=AF.Copy),
             reads=[bankC.r()], writes=[cs.r()])
        p.op("dve", lambda e: e.memset(off[:, 0, :], 0.0), writes=[off.r()])
        for i in range(1, 32):
            p.op("dve", lambda e, i=i: e.tensor_tensor(out=off[:, i, :], in0=off[:, i - 1, :], in1=cs[:, i - 1, :],
                                                       op=ALU.add),
                 reads=[off.r(), cs.r()], writes=[off.r()])
        cnt = p.sb("cnt", [128, 8], F32)
        p.op("dve", lambda e: e.tensor_tensor(out=cnt[:], in0=off[:, 31, :], in1=cs[:, 31, :], op=ALU.add),
             reads=[off.r(), cs.r()], writes=[cnt.r()])
        io8 = p.sb("io8", [128, 8], I32)
        thr = p.sb("thr", [128, 8], F32)
        p.op("pool", lambda e: e.iota(io8[:], pattern=[[BS, 8]], base=0, channel_multiplier=0), writes=[io8.r()])
        p.op("dve", lambda e: e.tensor_copy(out=thr[:], in_=io8[:]), reads=[io8.r()], writes=[thr.r()])
        cmp = p.sb("cmp", [128, 8, 8], F32)
        p.op("dve", lambda e: e.tensor_tensor(out=cmp[:], in0=cnt[:].unsqueeze(2).to_broadcast([128, 8, 8]),
                                              in1=thr[:].unsqueeze(1).to_broadcast([128, 8, 8]), op=ALU.is_gt),
             reads=[cnt.r(), thr.r()], writes=[cmp.r()])
        pad = p.sb("pad", [128, 8], F32)
        p.op("dve", lambda e: e.tensor_reduce(out=pad[:], in_=cmp[:], axis=AX.X, op=ALU.add),
             reads=[cmp.r()], writes=[pad.r()])
        p.op("dve", lambda e: e.tensor_scalar(out=pad[:], in0=pad[:], scalar1=float(BS), scalar2=None, op0=ALU.mult),
             reads=[pad.r()], writes=[pad.r()])
        pst = p.sb("pst", [128, 8], F32)
        pen = p.sb("pen", [128, 8], F32)
        p.op("dve", lambda e: e.memset(pst[:, 0:1], 0.0), writes=[pst.r()])
        for e_ in range(1, 8):
            p.op("dve", lambda e, e_=e_: e.tensor_tensor(out=pst[:, e_:e_ + 1], in0=pst[:, e_ - 1:e_],
                                                         in1=pad[:, e_ - 1:e_], op=ALU.add),
                 reads=[pst.r(), pad.r()], writes=[pst.r()])
        p.op("dve", lambda e: e.tensor_tensor(out=pen[:], in0=pst[:], in1=pad[:], op=ALU.add),
             reads=[pst.r(), pad.r()], writes=[pen.r()])
        iob = p.sb("iob", [128, NBLK], I32)
        bthr = p.sb("bthr", [128, NBLK], F32)
        p.op("pool", lambda e: e.iota(iob[:], pattern=[[BS, NBLK]], base=0, channel_multiplier=0), writes=[iob.r()])
        p.op("dve", lambda e: e.tensor_copy(out=bthr[:], in_=iob[:]), reads=[iob.r()], writes=[bthr.r()])
        cmp2 = p.sb("cmp2", [128, NBLK, 8], F32)
        p.op("dve", lambda e: e.tensor_tensor(out=cmp2[:], in0=pen[:].unsqueeze(1).to_broadcast([128, NBLK, 8]),
                                              in1=bthr[:].unsqueeze(2).to_broadcast([128, NBLK, 8]), op=ALU.is_le),
             reads=[pen.r(), bthr.r()], writes=[cmp2.r()])
        be = p.sb("be", [128, NBLK], F32)
        p.op("dve", lambda e: e.tensor_reduce(out=be[:], in_=cmp2[:], axis=AX.X, op=ALU.add),
             reads=[cmp2.r()], writes=[be.r()])
        p.op("dve", lambda e: e.tensor_scalar(out=be[:], in0=be[:], scalar1=float(NE - 1), scalar2=float(MOE),
                                              op0=ALU.min, op1=ALU.mult),
             reads=[be.r()], writes=[be.r()])
        slotf = p.sb("slotf", [128, 32, 8], F32)
        p.op("dve", lambda e: e.tensor_tensor(out=slotf[:].rearrange("p i e -> p (i e)"), in0=bankW[:, 0:256],
                                              in1=off[:].rearrange("p i e -> p (i e)"), op=ALU.add),
             reads=[bankW.r(), off.r()], writes=[slotf.r()])
        p.op("dve", lambda e: e.tensor_tensor(out=slotf[:], in0=slotf[:],
                                              in1=pst[:].unsqueeze(1).to_broadcast([128, 32, 8]), op=ALU.add),
             reads=[slotf.r(), pst.r()], writes=[slotf.r()])
        tmp3 = p.sb("tmp3", [128, 32, 8], F32)
        slab = p.sb("slab", [128, 64], F32)
        idxab = p.sb("idxab", [128, 64], I32)
        for k in range(2):
            p.op("dve", lambda e, k=k: e.tensor_tensor(out=tmp3[:], in0=rt_all[:, :, 8 * k:8 * k + 8], in1=slotf[:],
                                                       op=ALU.mult),
                 reads=[rt_all.r(), slotf.r()], writes=[tmp3.r()])
            p.op("dve", lambda e, k=k: e.tensor_reduce(out=slab[:, 32 * k:32 * k + 32], in_=tmp3[:], axis=AX.X,
                                                       op=ALU.add),
                 reads=[tmp3.r()], writes=[slab.r()])
        p.op("dve", lambda e: e.tensor_copy(out=idxab[:], in_=slab[:]), reads=[slab.r()], writes=[idxab.r()])
        p.dma("sp", idx_d[:, :], idxab[:], idxab, reads=[idxab.r()], writes=[idx_d.r()])
        tokid = p.sb("tokid", [128, 32], I32)
        p.op("pool", lambda e: e.iota(tokid[:], pattern=[[128, 32]], base=0, channel_multiplier=1), writes=[tokid.r()])
        rows = p.sb("rows", [128, 64, 2], I32)
        for k in range(2):
            p.op("dve", lambda e, k=k: e.tensor_copy(out=rows[:, 32 * k:32 * k + 32, 0:1], in_=tokid[:].unsqueeze(2)),
                 reads=[tokid.r()], writes=[rows.r()])
            p.op("dve", lambda e, k=k: e.tensor_copy(out=rows[:, 32 * k:32 * k + 32, 1:2].bitcast(F32),
                                                     in_=rt_all[:, :, 16 + k:17 + k]),
                 reads=[rt_all.r()], writes=[rows.r()])
        zer = p.sb("zer", [128, NSLOT // 128 * 2], I32)
        p.op("dve", lambda e: e.memset(zer[:], 0), writes=[zer.r()])
        p.dma("sp", stab_d[:, :].rearrange("(p a) c -> p (a c)", p=128), zer[:], zer, reads=[zer.r()],
              writes=[stab_d.r()])
        for i in range(64):
            p.op("pool", lambda e, i=i: e.indirect_dma_start(
                out=stab_d[:, :], out_offset=IOA(ap=idxab[:, i:i + 1], axis=0), in_=rows[:, i, :], in_offset=None),
                reads=[idxab.r(), rows.r()], writes=[stab_d.r()], dma=rows)
        wi_i = p.sb("wi_i", [128, NM], I32)
        wi_f = p.sb("wi_f", [128, NM], F32)
        p.op("pool", lambda e: e.iota(wi_i[:], pattern=[[128, NM]], base=0, channel_multiplier=1), writes=[wi_i.r()])
        p.op("dve", lambda e: e.tensor_copy(out=wi_f[:], in_=wi_i[:]), reads=[wi_i.r()], writes=[wi_f.r()])
        widx_f = p.sb("widx_f", [128, NBLK, NM], F32)
        widx = p.sb("widx", [128, NBLK, NM], I32)
        p.op("dve", lambda e: e.tensor_tensor(out=widx_f[:], in0=wi_f[:].unsqueeze(1).to_broadcast([128, NBLK, NM]),
                                              in1=be[:].unsqueeze(2).to_broadcast([128, NBLK, NM]), op=ALU.add),
             reads=[wi_f.r(), be.r()], writes=[widx_f.r()])
        p.op("dve", lambda e: e.tensor_copy(out=widx[:], in_=widx_f[:]), reads=[widx_f.r()], writes=[widx.r()])

        sidxs = [p.sb("sidx%d" % i, [128, 4, 2], I32) for i in range(2)]
        xs_tok = p.sb("xs_tok", [128, 4, D], BF16)
        xsT = p.sb("xsT", [128, KC, BS], BF16, n=KC)
        hT = p.sb("hT", [128, NM, BS], BF16, n=NM)
        wsl = [p.sb("mslot%d" % i, [128, KC, 512], BF16, n=4) for i in range(3)]
        ys_sb = p.sb("ys_sb", [128, 4, D], F32, n=4)
        tmps = [p.sb("mtmp%d" % i, [128, BS], F32) for i in range(2)]
        st = NS()
        st.wi = 0
        st.ti = 0

        def wl(src_d, b, m, nq):
            slot = wsl[st.wi % len(wsl)]
            st.wi += 1
            for kq in range(nq):
                p.op("pool", lambda e, kq=kq, slot=slot: e.indirect_dma_start(
                    out=slot[:, 4 * kq:4 * kq + 4, :].rearrange("p a b -> p (a b)"), out_offset=None,
                    in_=src_d[:, :], in_offset=IOA(ap=widx[:, b, m + kq:m + kq + 1], axis=0)),
                    reads=[widx.r()], writes=[slot.r(kq, kq + 1)], dma=slot)
            return slot

        def do_block(b):
            sidx = sidxs[b % 2]
            p.dma("sp", sidx[:], stab_d[b * BS:(b + 1) * BS, :].rearrange("(j p) c -> p j c", p=128), sidx,
                  reads=[stab_d.r()], writes=[sidx.r()])
            for j in range(4):
                p.op("pool", lambda e, j=j: e.indirect_dma_start(
                    out=xs_tok[:, j, :], out_offset=None, in_=toks_d[:, :],
                    in_offset=IOA(ap=sidx[:, j, 0:1], axis=0)),
                    reads=[sidx.r(), toks_d.r()], writes=[xs_tok.r()], dma=xs_tok)
            for kc in range(KC):
                bank = p.bank()
                bb = bank[:].bitcast(BF16)
                for j in range(4):
                    p.op("pe", lambda e, j=j, kc=kc, bb=bb: e.transpose(
                        out=bb[:, j * 128:(j + 1) * 128], in_=xs_tok[:, j, kc * 128:(kc + 1) * 128], identity=ident[:]),
                        reads=[xs_tok.r(), ident.r()], writes=[bank.r()])
                p.op("act", lambda e, kc=kc, bb=bb: e.activation(out=xsT[:, kc, :], in_=bb[:, 0:BS], func=AF.Copy),
                     reads=[bank.r()], writes=[xsT.r(kc, kc + 1)])
            for fg in range(14):
                s1 = wl(W.w1r, b, fg * 4, 4)
                s3 = wl(W.w3r, b, fg * 4, 4)
                for fi in range(4):
                    ft = fg * 4 + fi
                    bA = p.bank()
                    bB = p.bank()
                    for (bk, sl) in ((bA, s1), (bB, s3)):
                        for kc in range(KC):
                            p.op("pe", lambda e, kc=kc, fi=fi, sl=sl, bk=bk: e.matmul(
                                bk[:], lhsT=sl[:, kc, fi * 128:(fi + 1) * 128], rhs=xsT[:, kc, :],
                                start=(kc == 0), stop=(kc == KC - 1)),
                                reads=[sl.r(), xsT.r(kc, kc + 1)], writes=[bk.r()])
                    tmp = tmps[st.ti % 2]
                    st.ti += 1
                    p.op("act", lambda e, bA=bA, tmp=tmp: e.activation(out=tmp[:], in_=bA[:], func=AF.Silu),
                         reads=[bA.r()], writes=[tmp.r()])
                    p.op("dve", lambda e, bB=bB, tmp=tmp, ft=ft: e.tensor_tensor(
                        out=hT[:, ft, :], in0=bB[:], in1=tmp[:], op=ALU.mult),
                        reads=[bB.r(), tmp.r()], writes=[hT.r(ft, ft + 1)])
            for nt in range(4):
                bks = [p.bank() for _ in range(4)]
                for jg in range(14):
                    slot = wl(W.w2r, b, nt * 14 + jg, 1)
                    for j in range(4):
                        for i in range(4):
                            jt = jg * 4 + i
                            p.op("pe", lambda e, jt=jt, i=i, j=j, slot=slot, bk=bks[j]: e.matmul(
                                bk[:], lhsT=hT[:, jt, j * 128:(j + 1) * 128], rhs=slot[:, i, :],
                                start=(jt == 0), stop=(jt == NM - 1)),
                                reads=[slot.r(0, 1), hT.r(jt, jt + 1)], writes=[bks[j].r()])
                for j in range(4):
                    p.op("dve", lambda e, j=j, nt=nt, bk=bks[j]: e.tensor_scalar(
                        out=ys_sb[:, j, nt * 512:(nt + 1) * 512], in0=bk[:],
                        scalar1=sidx[:, j, 1:2].bitcast(F32), scalar2=None, op0=ALU.mult),
                        reads=[bks[j].r(), sidx.r()], writes=[ys_sb.r(j, j + 1)])
            p.dma("sp", ys_d[b * BS:(b + 1) * BS, :].rearrange("(j p) d -> p j d", p=128), ys_sb[:], ys_sb,
                  reads=[ys_sb.r()], writes=[ys_d.r()])

        for b_ in range(nblk):
            do_block(b_)


def phase_combine(p, x2_d, ys_d, idx_d, out_d, modd, ntiles=32):
    with p.phase():
        G2b = p.sb("G2b", [128, D], F32)
        idx = p.sb("idx", [128, 64], I32)
        p.dma("sp", G2b[:], modd.t[1, 0:1, 5 * D:6 * D].partition_broadcast(128), G2b, reads=[modd.r()],
              writes=[G2b.r()])
        p.dma("sp", idx[:], idx_d[:, :], idx, reads=[idx_d.r()], writes=[idx.r()])
        xo = [p.sb("xo%d" % i, [128, D], F32) for i in range(2)]
        yA = [p.sb("yA%d" % i, [128, D], F32) for i in range(2)]
        yB = [p.sb("yB%d" % i, [128, D], F32) for i in range(2)]

        def do_tile(i):
            x_, a_, b_ = xo[i % 2], yA[i % 2], yB[i % 2]
            p.dma("sp", x_[:], x2_d[i * 128:(i + 1) * 128, :], x_, reads=[x2_d.r()], writes=[x_.r()])
            p.op("pool", lambda e: e.indirect_dma_start(out=a_[:], out_offset=None, in_=ys_d[:, :],
                                                        in_offset=IOA(ap=idx[:, i:i + 1], axis=0)),
                 reads=[idx.r(), ys_d.r()], writes=[a_.r()], dma=a_)
            p.op("pool", lambda e: e.indirect_dma_start(out=b_[:], out_offset=None, in_=ys_d[:, :],
                                                        in_offset=IOA(ap=idx[:, 32 + i:33 + i], axis=0)),
                 reads=[idx.r(), ys_d.r()], writes=[b_.r()], dma=b_)
            p.op("dve", lambda e: e.tensor_tensor(out=a_[:], in0=a_[:], in1=b_[:], op=ALU.add),
                 reads=[a_.r(), b_.r()], writes=[a_.r()])
            p.op("dve", lambda e: e.tensor_tensor(out=a_[:], in0=a_[:], in1=G2b[:], op=ALU.mult),
                 reads=[a_.r(), G2b.r()], writes=[a_.r()])
            p.op("dve", lambda e: e.tensor_tensor(out=x_[:], in0=x_[:], in1=a_[:], op=ALU.add),
                 reads=[a_.r(), x_.r()], writes=[x_.r()])
            p.dma("sp", out_d[i * 128:(i + 1) * 128, :], x_[:], x_, reads=[x_.r()], writes=[out_d.r()])

        for i_ in range(ntiles):
            do_tile(i_)


def declare_moe_weights(p, kind="ExternalInput"):
    W = NS()
    W.w1r = p.dr("moe_w1r", [NE * MOE, D], F32, kind=kind)
    W.w3r = p.dr("moe_w3r", [NE * MOE, D], F32, kind=kind)
    W.w2r = p.dr("moe_w2r", [NE * MOE, D], F32, kind=kind)
    return W


def host_moe_weights(inp):
    def r13(w):
        return np.ascontiguousarray(
            w.reshape(NE, 4, 4, 128, 14, 512).transpose(0, 4, 1, 3, 2, 5)).reshape(NE * MOE, D)

    def r2(w):
        return np.ascontiguousarray(
            w.reshape(NE, 14, 4, 128, 4, 512).transpose(0, 4, 1, 3, 2, 5)).reshape(NE * MOE, D)

    return dict(moe_w1r=r13(inp["moe_w1"][0]), moe_w3r=r13(inp["moe_w3"][0]), moe_w2r=r2(inp["moe_w2"][0]))


NB_PER_CORE = 1
N_CORES = 8


def _view(p, buf, bi, name):
    b = Buf(name, buf.t[bi], 1)
    p.bufs.append(b)
    return b


def build_full(nb=NB_PER_CORE):
    nc = bass.Bass("TRN2", target_bir_lowering=False)
    p = Prog(nc)
    p.init_psum()
    cT_a = p.dr("cT", [nb, 128, 32], F32, kind="ExternalInput")
    w_ada_d = p.dr("w_ada", [2 * D, NMOD * D], F32, kind="ExternalInput")
    b_ada_d = p.dr("b_ada", [2, NMOD * D], F32, kind="ExternalInput")
    x_a = p.dr("x", [nb, SEQ, D], F32, kind="ExternalInput")
    ctx_a = p.dr("ctx", [nb, CTX, D], F32, kind="ExternalInput")
    W0 = declare_l0_weights(p)
    W1 = declare_l1_weights(p)
    WM = declare_moe_weights(p)
    out_a = p.dr("out", [nb, SEQ, D], F32, kind="ExternalOutput")
    modd = p.dr("modd", [2, 2, NMOD * D], F32)
    x1_d = p.dr("x1", [SEQ, D], F32)
    xc1_d = p.dr("xc1", [CTX, D], F32)
    qT_d = p.dr("qT", [NH, 128, SEQ], BF16)
    kT_d = p.dr("kT", [NH, 128, NKEY], BF16)
    v_d = p.dr("v", [NKEY, D], BF16)
    OT_d = p.dr("OT", [NH, 128, SEQ], BF16)
    x2_d = p.dr("x2", [SEQ, D], F32)
    toks_d = p.dr("toks", [SEQ, D], BF16)
    rt_d = p.dr("rt", [SEQ, 18], F32)
    ys_d = p.dr("ys", [NSLOT, D], F32)
    stab_d = p.dr("stab", [NSLOT, 2], I32)
    idx_d = p.dr("idx", [128, 64], I32)
    for bi in range(nb):
        cT_d = _view(p, cT_a, bi, "cT%d" % bi)
        x_d = _view(p, x_a, bi, "x%d" % bi)
        ctx_d = _view(p, ctx_a, bi, "ctx%d" % bi)
        out_d = _view(p, out_a, bi, "out%d" % bi)
        phase_adaln(p, cT_d, w_ada_d, b_ada_d, modd)
        phase_l0(p, W0, x_d, ctx_d, x1_d, xc1_d, modd)
        phase_l1a(p, W1, x1_d, xc1_d, qT_d, kT_d, v_d, modd)
        phase_l1b(p, W1, qT_d, kT_d, v_d, OT_d)
        phase_l1c(p, W1, x1_d, OT_d, x2_d, toks_d, rt_d, modd)
        phase_moe(p, WM, toks_d, rt_d, ys_d, stab_d, idx_d)
        phase_combine(p, x2_d, ys_d, idx_d, out_d, modd)
    p.emit()
    return nc


def kernel(**inp):
    inp = {k: np.asarray(v) for k, v in inp.items()}
    nb = NB_PER_CORE
    nc = build_full(nb)
    shared = dict(w_ada=np.ascontiguousarray(inp["w_ada"].reshape(2 * D, NMOD * D)),
                  b_ada=np.ascontiguousarray(inp["b_ada"]))
    shared.update(host_l0_weights(inp))
    shared.update(host_l1_weights(inp))
    shared.update(host_moe_weights(inp))
    maps = []
    for c in range(N_CORES):
        m = dict(shared)
        bs = list(range(c * nb, (c + 1) * nb))
        m["cT"] = np.stack([host_cT(inp, b) for b in bs], axis=0)
        m["x"] = np.ascontiguousarray(inp["x"][bs[0]:bs[-1] + 1])
        m["ctx"] = np.ascontiguousarray(inp["ctx"][bs[0]:bs[-1] + 1])
        maps.append(m)
    res = run_bass_kernel_spmd(nc, maps, core_ids=list(range(N_CORES)))
    out = np.concatenate([np.asarray(res.results[c]["out"]) for c in range(N_CORES)], axis=0)
    return out.astype(np.float32)
```

```python
import contextlib
import numpy as np
import concourse.bass as bass
import concourse.mybir as mybir
from concourse.bass_utils import run_bass_kernel_spmd

F32 = mybir.dt.float32
BF16 = mybir.dt.bfloat16
I32 = mybir.dt.int32
AF = mybir.ActivationFunctionType
ALU = mybir.AluOpType
AX = mybir.AxisListType

D = 2048
KC = 16
SEQ = 4096
CTX = 256
FFN = 5632
NMOD = 6
EPS = 1e-6
ENGS = ("pe", "act", "dve", "pool", "sp")


class Buf:
    def __init__(self, name, t, n=1):
        self.name = name
        self.t = t
        self.n = n
        self.lw = [None] * n
        self.rd = [dict() for _ in range(n)]
        self.sem = None
        self.dmacnt = 0

    def __getitem__(self, k):
        return self.t[k]

    def r(self, lo=0, hi=None):
        return (self, lo, self.n if hi is None else hi)


class Op:
    __slots__ = ("eng", "fn", "deps", "sig", "sigval", "dma_buf", "phase")

    def __init__(self, eng, fn, dma_buf):
        self.eng = eng
        self.fn = fn
        self.deps = None
        self.sig = False
        self.sigval = None
        self.dma_buf = dma_buf


class Prog:
    def __init__(self, nc):
        self.nc = nc
        self.ops = {e: [] for e in ENGS}
        self.allops = []
        self.bufs = []
        self.pending = {e: set() for e in ENGS}
        self.since_barrier = []
        self.last = {e: None for e in ENGS}
        self.stack = contextlib.ExitStack()
        self.pstack = None
        self.banks = []
        self.bi = 0
        self.uid = 0
        self.phase_id = 0

    def init_psum(self):
        for i in range(8):
            t = self.stack.enter_context(self.nc.psum_tensor("bank%d" % i, [128, 512], F32))
            b = Buf("bank%d" % i, t, 1)
            self.bufs.append(b)
            self.banks.append(b)

    def bank(self):
        b = self.banks[self.bi % 8]
        self.bi += 1
        return b

    @contextlib.contextmanager
    def phase(self):
        self.barrier()
        self.phase_id += 1
        self.pstack = contextlib.ExitStack()
        try:
            yield
        finally:
            self.barrier()
            self.pstack.close()
            self.pstack = None

    def sb(self, name, shape, dt, n=1):
        self.uid += 1
        nm = "%s_%d" % (name, self.uid)
        st = self.pstack if self.pstack is not None else self.stack
        t = st.enter_context(self.nc.sbuf_tensor(nm, list(shape), dt))
        b = Buf(nm, t, n)
        self.bufs.append(b)
        return b

    def dr(self, name, shape, dt, kind="Internal", n=1):
        t = self.nc.dram_tensor(name, list(shape), dt, kind=kind)
        b = Buf(name, t, n)
        self.bufs.append(b)
        return b

    def barrier(self):
        deps = set(o for o in self.last.values() if o is not None)
        deps.update(self.since_barrier)
        self.since_barrier = []
        for e in ENGS:
            self.pending[e] = set(deps)

    def op(self, eng, fn, reads=(), writes=(), dma=None):
        o = Op(eng, fn, dma)
        o.phase = self.phase_id
        deps = set()
        rkey = eng if dma is None else ("dma", id(dma))
        for (b, lo, hi) in reads:
            for c in range(lo, hi):
                w = b.lw[c]
                if w is not None:
                    deps.add(w)
                b.rd[c][rkey] = o
        for (b, lo, hi) in writes:
            for c in range(lo, hi):
                w = b.lw[c]
                if w is not None:
                    deps.add(w)
                rd = b.rd[c]
                if rd:
                    deps.update(rd.values())
                    b.rd[c] = dict()
                b.lw[c] = o
        if self.pending[eng]:
            deps.update(self.pending[eng])
            self.pending[eng] = set()
        deps.discard(o)
        if eng == "pe":
            deps = {d for d in deps if not (d.eng == "pe" and d.dma_buf is None)}
        for d in deps:
            d.sig = True
        o.deps = deps
        self.ops[eng].append(o)
        self.allops.append(o)
        self.last[eng] = o
        if dma is not None:
            self.since_barrier.append(o)
        return o

    def dma(self, eng, out_ap, in_ap, sbuf_buf, reads=(), writes=(), **kw):
        def fn(e):
            return e.dma_start(out=out_ap, in_=in_ap, **kw)
        return self.op(eng, fn, reads, writes, dma=sbuf_buf)

    def emit(self):
        nc = self.nc
        st = self.stack
        esem = {}
        for e in ENGS:
            esem[e] = st.enter_context(nc.semaphore("s_" + e))
        free = []
        cur_phase = None
        phase_bufs = []
        for o in self.allops:
            if o.phase != cur_phase:
                for b in phase_bufs:
                    free.append(b.semrec)
                phase_bufs = []
                cur_phase = o.phase
            if o.dma_buf is not None:
                b = o.dma_buf
                if b.sem is None:
                    rec = free.pop() if free else [st.enter_context(nc.semaphore("d_" + b.name)), 0]
                    b.semrec = rec
                    b.sem = rec[0]
                    b.dmacnt = rec[1]
                    phase_bufs.append(b)
                b.dmacnt += 16
                b.semrec[1] = b.dmacnt
                o.sigval = (b.sem, b.dmacnt)
        for e in ENGS:
            cnt = 0
            for o in self.ops[e]:
                if o.dma_buf is not None:
                    pass
                elif o.sig:
                    cnt += 1
                    o.sigval = (esem[e], cnt)
            assert cnt < 65000, (e, cnt)
        block = st.enter_context(nc.Block())
        prog = self

        def run(e, eng):
            waited = {}
            for o in prog.ops[e]:
                need = {}
                for d in o.deps:
                    sem, val = d.sigval
                    k = id(sem)
                    if k not in need or need[k][1] < val:
                        need[k] = (sem, val)
                for k, (sem, val) in need.items():
                    if waited.get(k, 0) < val:
                        eng.wait_ge(sem, val)
                        waited[k] = val
                inst = o.fn(eng)
                if o.sigval is not None:
                    sem, val = o.sigval
                    inst.then_inc(sem, 16 if o.dma_buf is not None else 1)
            if e == "sp":
                for b in prog.bufs:
                    if b.sem is not None and waited.get(id(b.sem), 0) < b.dmacnt:
                        eng.wait_ge(b.sem, b.dmacnt)

        @block.tensor
        def _(eng):
            run("pe", eng)

        @block.scalar
        def _(eng):
            run("act", eng)

        @block.vector
        def _(eng):
            run("dve", eng)

        @block.gpsimd
        def _(eng):
            run("pool", eng)

        @block.sync
        def _(eng):
            run("sp", eng)

        st.close()


class NS:
    pass


def make_ident(p, dt=BF16):
    ident = p.sb("ident", [128, 128], dt)
    p.op("pool", lambda e: e.memset(ident[:], 0.0), writes=[ident.r()])
    p.op("pool", lambda e: e.affine_select(out=ident[:], in_=ident[:], pattern=[[-1, 128]],
                                           compare_op=ALU.not_equal, fill=1.0, base=0,
                                           channel_multiplier=1),
         reads=[ident.r()], writes=[ident.r()])
    return ident


def load_cols(p, dst, src_ap_1d):
    p.dma("sp", dst[:], src_ap_1d.rearrange("(kc p) -> p kc", p=128), dst, writes=[dst.r()],
          allow_slow_non_contiguous=True)


def norm_T(p, S, xres, nsub, Ac, Bc, outT):
    T = nsub * 128
    for j in range(nsub):
        p.op("act", lambda e, j=j: e.activation(out=S.xn[:, j, :], in_=xres[:, j, :], func=AF.Square,
                                                accum_out=S.ss[:, j:j + 1]),
             reads=[xres.r(j, j + 1)], writes=[S.ss.r(), S.xn.r(j, j + 1)])
    p.op("act", lambda e: e.activation(out=S.rstd[:, 0:nsub], in_=S.ss[:, 0:nsub], func=AF.Sqrt,
                                       scale=1.0 / D, bias=EPS),
         reads=[S.ss.r()], writes=[S.rstd.r()])
    p.op("dve", lambda e: e.reciprocal(out=S.rstd[:, 0:nsub], in_=S.rstd[:, 0:nsub]),
         reads=[S.rstd.r()], writes=[S.rstd.r()])
    for j in range(nsub):
        p.op("dve", lambda e, j=j: e.tensor_scalar(out=S.xn[:, j, :], in0=xres[:, j, :],
                                                   scalar1=S.rstd[:, j:j + 1], scalar2=None,
                                                   op0=ALU.mult),
             reads=[xres.r(j, j + 1), S.rstd.r()], writes=[S.xn.r(j, j + 1)])
    for kc in range(KC):
        bank = p.bank()
        bb = bank[:].bitcast(BF16)
        for j in range(nsub):
            p.op("pe", lambda e, j=j, kc=kc, bb=bb: e.transpose(
                out=bb[:, j * 128:(j + 1) * 128], in_=S.xn[:, j, kc * 128:(kc + 1) * 128],
                identity=S.ident[:]),
                reads=[S.xn.r(j, j + 1), S.ident.r()], writes=[bank.r()])
        p.op("act", lambda e, kc=kc, bb=bb: e.activation(
            out=outT[:, kc, 0:T], in_=bb[:, 0:T], func=AF.Identity,
            scale=Ac[:, kc:kc + 1], bias=Bc[:, kc:kc + 1]),
            reads=[bank.r(), Ac.r(), Bc.r()], writes=[outT.r(kc, kc + 1)])


def wload(p, S, src_ap, nk, eng="pool"):
    slot = S.wslots[S.wi % len(S.wslots)]
    S.wi += 1
    p.dma(eng, slot[:, 0:nk, :], src_ap, slot, writes=[slot.r()])
    return slot


def wview(w_d, r0, nk, c0, nc_=512):
    return w_d[r0:r0 + nk * 128, c0:c0 + nc_].rearrange("(k p) n -> p k n", p=128)


def phase_adaln(p, cT_d, w_ada_d, b_ada_d, modd, layers=(0, 1)):
    with p.phase():
        cT = p.sb("cT", [128, 32], F32)
        brow = p.sb("brow", [2, NMOD * D], F32)
        modr = p.sb("modr", [2, NMOD * D], F32)
        ws = [p.sb("wada%d" % i, [128, KC, 512], F32) for i in range(2)]
        p.dma("sp", cT[:], cT_d[:], cT, writes=[cT.r()])
        p.op("act", lambda e: e.activation(out=cT[:], in_=cT[:], func=AF.Silu),
             reads=[cT.r()], writes=[cT.r()])
        cT3 = cT[:].rearrange("p (k j) -> p k j", j=2)
        wi = 0
        for i in layers:
            p.dma("sp", brow[:], b_ada_d[i:i + 1, :].partition_broadcast(2), brow, writes=[brow.r()])
            for nt in range(NMOD * D // 512):
                slot = ws[wi % 2]
                wi += 1
                p.dma("sp", slot[:], wview(w_ada_d, i * D, KC, nt * 512), slot, writes=[slot.r()])
                bank = p.bank()
                for kc in range(KC):
                    p.op("pe", lambda e, kc=kc, slot=slot, bank=bank: e.matmul(
                        bank[0:2, :], lhsT=cT3[:, kc, :], rhs=slot[:, kc, :],
                        start=(kc == 0), stop=(kc == KC - 1)),
                        reads=[cT.r(), slot.r()], writes=[bank.r()])
                p.op("dve", lambda e, nt=nt, bank=bank: e.tensor_tensor(
                    out=modr[:, nt * 512:(nt + 1) * 512], in0=bank[0:2, :],
                    in1=brow[:, nt * 512:(nt + 1) * 512], op=ALU.add),
                    reads=[bank.r(), brow.r()], writes=[modr.r()])
            p.dma("sp", modd[i], modr[:], modr, reads=[modr.r()], writes=[modd.r()])


def load_modset(p, S, modd, nwT_d, layer, s):
    m = modd.t
    rd = [modd.r()]
    for (dst, off) in ((S.B1c, 0), (S.sc1, 1), (S.B2c, 3), (S.sc2, 4)):
        p.dma("sp", dst[:], m[layer, s, off * D:(off + 1) * D].rearrange("(kc p) -> p kc", p=128),
              dst, reads=rd, writes=[dst.r()], allow_slow_non_contiguous=True)
    for (dst, off) in ((S.G1b, 2), (S.G2b, 5)):
        p.dma("sp", dst[:], m[layer, s:s + 1, off * D:(off + 1) * D].partition_broadcast(128),
              dst, reads=rd, writes=[dst.r()])
    for (A, sc, k) in ((S.A1c, S.sc1, 0), (S.A2c, S.sc2, 1)):
        p.op("dve", lambda e, A=A, sc=sc, k=k: e.scalar_tensor_tensor(
            out=A[:], in0=sc[:], scalar=1.0, in1=S.nwc[:, k, :], op0=ALU.add, op1=ALU.mult),
            reads=[sc.r(), S.nwc.r()], writes=[A.r()])


def alloc_mod(p, S, nwT_d, layer):
    for nm in ("A1c", "B1c", "A2c", "B2c", "sc1", "sc2"):
        setattr(S, nm, p.sb(nm, [128, KC], F32))
    S.G1b = p.sb("G1b", [128, D], F32)
    S.G2b = p.sb("G2b", [128, D], F32)
    S.nwc = p.sb("nwc", [128, 2, KC], F32)
    p.dma("sp", S.nwc[:], nwT_d[layer], S.nwc, writes=[S.nwc.r()])


def resid_update(p, S, bank, xres, j, nt, Gb):
    cs = slice(nt * 512, (nt + 1) * 512)
    tmp = S.tmps[S.ti % len(S.tmps)]
    S.ti += 1
    p.op("dve", lambda e, tmp=tmp: e.tensor_tensor(out=tmp[:], in0=bank[:], in1=Gb[:, cs], op=ALU.mult),
         reads=[bank.r(), Gb.r()], writes=[tmp.r()])
    p.op("dve", lambda e, tmp=tmp: e.tensor_tensor(out=xres[:, j, cs], in0=xres[:, j, cs], in1=tmp[:],
                                                    op=ALU.add),
         reads=[tmp.r(), xres.r(j, j + 1)], writes=[xres.r(j, j + 1)])


def phase_l0(p, W, x_d, ctx_d, x1_d, xc1_d, modd, tiles=None):
    with p.phase():
        S = NS()
        S.ident = make_ident(p)
        alloc_mod(p, S, W.nwT, 0)
        S.ss = p.sb("ss", [128, 4], F32)
        S.rstd = p.sb("rstd", [128, 4], F32)
        S.xn = p.sb("xn", [128, 4, D], BF16, n=4)
        xres = p.sb("xres", [128, 4, D], F32, n=4)
        actT = p.sb("actT", [128, KC, 512], BF16, n=KC)
        big = p.sb("big", [128, 48, 512], BF16, n=48)
        S.wslots = [p.sb("wslot%d" % i, [128, KC, 512], BF16) for i in range(3)]
        S.wi = 0
        S.tmps = [p.sb("tmp%d" % i, [128, 512], F32) for i in range(2)]
        S.tmps4 = [p.sb("tmpb%d" % i, [128, 512], BF16) for i in range(4)]
        S.ti = 0
        gvb = p.sb("gvb", [128, D], F32)
        wsT = p.sb("wsT", [128, 16, 128], BF16)
        bsr = p.sb("bsr", [1, 16, 128], F32)
        ones = p.sb("ones", [1, 128], F32)
        p.dma("sp", gvb[:], W.gv[0:1, :].partition_broadcast(128), gvb, writes=[gvb.r()])
        p.dma("pool", wsT[:], W.wsT[:], wsT, writes=[wsT.r()])
        p.dma("sp", bsr[:], W.bs[:], bsr, writes=[bsr.r()])
        p.op("dve", lambda e: e.memset(ones[:], 1.0), writes=[ones.r()])

        if tiles is None:
            tiles = [(0, t * 512, 4) for t in range(SEQ // 512)] + [(1, 0, 2)]
        def do_tile(s, t0, nsub, newset):
            T = nsub * 128
            src = x_d if s == 0 else ctx_d
            dst = x1_d if s == 0 else xc1_d
            if newset:
                load_modset(p, S, modd, W.nwT, 0, s)
            p.dma("sp", xres[:, 0:nsub, :], src[t0:t0 + T, :].rearrange("(j p) d -> p j d", p=128),
                  xres, reads=[src.r()], writes=[xres.r(0, nsub)])
            norm_T(p, S, xres, nsub, S.A1c, S.B1c, actT)
            for fg in range(4):
                slot = wload(p, S, wview(W.w_in, 0, KC, fg * 512), KC)
                for fi in range(4):
                    ft = fg * 4 + fi
                    bank = p.bank()
                    for kc in range(KC):
                        p.op("pe", lambda e, kc=kc, fi=fi, slot=slot, bank=bank: e.matmul(
                            bank[:, 0:T], lhsT=slot[:, kc, fi * 128:(fi + 1) * 128], rhs=actT[:, kc, 0:T],
                            start=(kc == 0), stop=(kc == KC - 1)),
                            reads=[slot.r(), actT.r(kc, kc + 1)], writes=[bank.r()])
                    p.op("act", lambda e, ft=ft, bank=bank: e.activation(
                        out=big[:, ft, 0:T], in_=bank[:, 0:T], func=AF.Gelu),
                        reads=[bank.r()], writes=[big.r(ft, ft + 1)])
            for nt in range(4):
                slot = wload(p, S, wview(W.w_in, 0, KC, D + nt * 512), KC)
                for j in range(nsub):
                    bank = p.bank()
                    for kc in range(KC):
                        p.op("pe", lambda e, kc=kc, j=j, slot=slot, bank=bank: e.matmul(
                            bank[:], lhsT=actT[:, kc, j * 128:(j + 1) * 128], rhs=slot[:, kc, :],
                            start=(kc == 0), stop=(kc == KC - 1)),
                            reads=[slot.r(), actT.r(kc, kc + 1)], writes=[bank.r()])
                    c = 16 + 4 * j + nt
                    p.op("act", lambda e, c=c, bank=bank: e.activation(
                        out=big[:, c, :], in_=bank[:], func=AF.Gelu),
                        reads=[bank.r()], writes=[big.r(c, c + 1)])
            for j in range(nsub):
                vj = big[:, 16 + 4 * j:20 + 4 * j, :]
                p.op("act", lambda e, j=j, vj=vj: e.activation(
                    out=S.xn[:, j, :].rearrange("p (a b) -> p a b", a=4), in_=vj, func=AF.Square,
                    accum_out=S.ss[:, j:j + 1]),
                    reads=[big.r(16 + 4 * j, 20 + 4 * j)], writes=[S.ss.r(), S.xn.r(j, j + 1)])
            p.op("act", lambda e: e.activation(out=S.rstd[:, 0:nsub], in_=S.ss[:, 0:nsub], func=AF.Sqrt,
                                               scale=1.0 / D, bias=EPS),
                 reads=[S.ss.r()], writes=[S.rstd.r()])
            p.op("dve", lambda e: e.reciprocal(out=S.rstd[:, 0:nsub], in_=S.rstd[:, 0:nsub]),
                 reads=[S.rstd.r()], writes=[S.rstd.r()])
            for j in range(nsub):
                vj = big[:, 16 + 4 * j:20 + 4 * j, :]
                p.op("dve", lambda e, j=j, vj=vj: e.scalar_tensor_tensor(
                    out=vj, in0=vj, scalar=S.rstd[:, j:j + 1],
                    in1=gvb[:].rearrange("p (a b) -> p a b", a=4), op0=ALU.mult, op1=ALU.mult),
                    reads=[big.r(16 + 4 * j, 20 + 4 * j), S.rstd.r(), gvb.r()],
                    writes=[big.r(16 + 4 * j, 20 + 4 * j)])
            for g in range(16):
                bank = p.bank()
                p.op("pe", lambda e, g=g, bank=bank: e.matmul(
                    bank[:, 0:T].rearrange("p (j q) -> p j q", q=128), lhsT=ones[0:1, :],
                    rhs=bsr[0:1, g:g + 1, :].to_broadcast([1, nsub, 128]),
                    start=True, stop=False),
                    reads=[ones.r(), bsr.r()], writes=[bank.r()])
                for j in range(nsub):
                    c = 16 + 4 * j + g // 4
                    p.op("pe", lambda e, g=g, j=j, c=c, bank=bank: e.matmul(
                        bank[:, j * 128:(j + 1) * 128],
                        lhsT=big[:, c, (g % 4) * 128:(g % 4 + 1) * 128], rhs=wsT[:, g, :],
                        start=False, stop=(j == nsub - 1)),
                        reads=[big.r(c, c + 1), wsT.r()], writes=[bank.r()])
                p.op("dve", lambda e, g=g, bank=bank: e.tensor_tensor(
                    out=big[:, 32 + g, 0:T], in0=bank[:, 0:T], in1=big[:, g, 0:T], op=ALU.mult),
                    reads=[bank.r(), big.r(g, g + 1)], writes=[big.r(32 + g, 33 + g)])
            for nt in range(4):
                slot = wload(p, S, wview(W.w_out, 0, KC, nt * 512), KC)
                for j in range(nsub):
                    bank = p.bank()
                    for kc in range(KC):
                        p.op("pe", lambda e, kc=kc, j=j, slot=slot, bank=bank: e.matmul(
                            bank[:], lhsT=big[:, 32 + kc, j * 128:(j + 1) * 128], rhs=slot[:, kc, :],
                            start=(kc == 0), stop=(kc == KC - 1)),
                            reads=[slot.r(), big.r(32 + kc, 33 + kc)], writes=[bank.r()])
                    resid_update(p, S, bank, xres, j, nt, S.G1b)
            norm_T(p, S, xres, nsub, S.A2c, S.B2c, actT)
            for fg in range(FFN // 512):
                s1 = wload(p, S, wview(W.w1, 0, KC, fg * 512), KC)
                s3 = wload(p, S, wview(W.w3, 0, KC, fg * 512), KC)
                bAs = [p.bank() for _ in range(4)]
                bBs = [p.bank() for _ in range(4)]
                for (bks_, sl) in ((bAs, s1), (bBs, s3)):
                    for fi in range(4):
                        bk = bks_[fi]
                        for kc in range(KC):
                            p.op("pe", lambda e, kc=kc, fi=fi, sl=sl, bk=bk: e.matmul(
                                bk[:, 0:T], lhsT=sl[:, kc, fi * 128:(fi + 1) * 128], rhs=actT[:, kc, 0:T],
                                start=(kc == 0), stop=(kc == KC - 1)),
                                reads=[sl.r(), actT.r(kc, kc + 1)], writes=[bk.r()])
                        if bks_ is bAs:
                            tmp = S.tmps4[S.ti % 4]
                            S.ti += 1
                            p.op("act", lambda e, bk=bk, tmp=tmp: e.activation(out=tmp[:, 0:T], in_=bk[:, 0:T], func=AF.Silu),
                                 reads=[bk.r()], writes=[tmp.r()])
                            bk.silu_tmp = tmp
                        else:
                            ft = fg * 4 + fi
                            tmp = bAs[fi].silu_tmp
                            p.op("dve", lambda e, bk=bk, tmp=tmp, ft=ft: e.tensor_tensor(
                                out=big[:, ft, 0:T], in0=bk[:, 0:T], in1=tmp[:, 0:T], op=ALU.mult),
                                reads=[bk.r(), tmp.r()], writes=[big.r(ft, ft + 1)])
            for nt in range(4):
                bks = [p.bank() for _ in range(nsub)]
                for kg in range(4):
                    slot = wload(p, S, wview(W.w2, kg * 11 * 128, 11, nt * 512), 11)
                    for j in range(nsub):
                        for i in range(11):
                            kc = kg * 11 + i
                            p.op("pe", lambda e, kc=kc, i=i, j=j, slot=slot, bk=bks[j]: e.matmul(
                                bk[:], lhsT=big[:, kc, j * 128:(j + 1) * 128], rhs=slot[:, i, :],
                                start=(kc == 0), stop=(kc == 43)),
                                reads=[slot.r(), big.r(kc, kc + 1)], writes=[bks[j].r()])
                for j in range(nsub):
                    resid_update(p, S, bks[j], xres, j, nt, S.G2b)
            p.dma("sp", dst[t0:t0 + T, :].rearrange("(j p) d -> p j d", p=128), xres[:, 0:nsub, :],
                  xres, reads=[xres.r(0, nsub)], writes=[dst.r()])

        cur_set = None
        for (s_, t0_, nsub_) in tiles:
            do_tile(s_, t0_, nsub_, s_ != cur_set)
            cur_set = s_


def declare_l0_weights(p, kind="ExternalInput"):
    W = NS()
    W.nwT = p.dr("nwT", [2, 128, 2, KC], F32, kind=kind)
    W.w_in = p.dr("mlp_w_in", [D, 2 * D], F32, kind=kind)
    W.gv = p.dr("mlp_g_v", [1, D], F32, kind=kind)
    W.wsT = p.dr("mlp_wsT", [128, 16, 128], F32, kind=kind)
    W.bs = p.dr("mlp_b_s", [1, 16, 128], F32, kind=kind)
    W.w_out = p.dr("mlp_w_out", [D, D], F32, kind=kind)
    W.w1 = p.dr("ffn_w1", [D, FFN], F32, kind=kind)
    W.w3 = p.dr("ffn_w3", [D, FFN], F32, kind=kind)
    W.w2 = p.dr("ffn_w2", [FFN, D], F32, kind=kind)
    return W


def host_l0_weights(inp):
    nw = inp["norm_w"]
    nwT = np.ascontiguousarray(nw.reshape(2, 2, KC, 128).transpose(0, 3, 1, 2))
    return dict(
        nwT=nwT,
        mlp_w_in=np.ascontiguousarray(inp["mlp_w_in"][0]),
        mlp_g_v=np.ascontiguousarray(inp["mlp_g_v"][0:1]),
        mlp_wsT=np.ascontiguousarray(inp["mlp_w_s"][0].transpose(2, 0, 1)),
        mlp_b_s=np.ascontiguousarray(inp["mlp_b_s"][0][None]),
        mlp_w_out=np.ascontiguousarray(inp["mlp_w_out"][0]),
        ffn_w1=np.ascontiguousarray(inp["ffn_w1"][0]),
        ffn_w3=np.ascontiguousarray(inp["ffn_w3"][0]),
        ffn_w2=np.ascontiguousarray(inp["ffn_w2"][0]),
    )


def host_cT(inp, b):
    cc = np.stack([inp["c"][b], inp["c_ctx"]], axis=-1)
    return np.ascontiguousarray(cc.reshape(KC, 128, 2).transpose(1, 0, 2).reshape(128, 32))


def build_l0(tiles=None, layers=(0, 1)):
    nc = bass.Bass("TRN2", target_bir_lowering=False)
    p = Prog(nc)
    p.init_psum()
    cT_d = p.dr("cT", [128, 32], F32, kind="ExternalInput")
    w_ada_d = p.dr("w_ada", [2 * D, NMOD * D], F32, kind="ExternalInput")
    b_ada_d = p.dr("b_ada", [2, NMOD * D], F32, kind="ExternalInput")
    x_d = p.dr("x", [SEQ, D], F32, kind="ExternalInput")
    ctx_d = p.dr("ctx", [CTX, D], F32, kind="ExternalInput")
    W = declare_l0_weights(p)
    modd = p.dr("modd", [2, 2, NMOD * D], F32, kind="ExternalOutput")
    x1_d = p.dr("x1", [SEQ, D], F32, kind="ExternalOutput")
    xc1_d = p.dr("xc1", [CTX, D], F32, kind="ExternalOutput")
    phase_adaln(p, cT_d, w_ada_d, b_ada_d, modd, layers)
    phase_l0(p, W, x_d, ctx_d, x1_d, xc1_d, modd, tiles)
    p.emit()
    return nc


NH = 16
NKEY = SEQ + CTX
NEG = -30000.0


class BankPool:
    def __init__(self, p, ids):
        self.b = [p.banks[i] for i in ids]
        self.i = 0

    def next(self):
        b = self.b[self.i % len(self.b)]
        self.i += 1
        return b


def l1_variant(g, j):
    if g == 0:
        return j, j
    if g == 7:
        return 26 + j, 14 + j
    return 4 * g - 2 + j, 6 + j


def l1_ntiles(g):
    return 6 if g in (0, 7) else 8


def host_bias_table(rpb):
    out = np.empty((NH, 20, 128, 512), np.float32)
    kk = np.arange(128)
    qq = np.arange(512)
    for g in (0, 1, 7):
        for j in range(l1_ntiles(g)):
            kt, var = l1_variant(g, j)
            kr = (2 * kt + kk // 64)[:, None]
            kc = (kk % 64)[:, None]
            qr = (8 * g + qq // 64)[None, :]
            qc = (qq % 64)[None, :]
            rs = np.clip(qr - 4, 0, 56)
            cs = np.clip(qc - 8, 0, 48)
            valid = (kr >= rs) & (kr < rs + 8) & (kc >= cs) & (kc < cs + 16)
            dr = np.clip(kr - qr + 7, 0, 14)
            dc = np.clip(kc - qc + 15, 0, 30)
            out[:, var] = np.where(valid[None], rpb[:, dr, dc], np.float32(NEG))
    return out


def phase_l1a(p, W, x1_d, xc1_d, qT_d, kT_d, v_d, modd, tiles=None):
    with p.phase():
        S = NS()
        S.ident = make_ident(p)
        alloc_mod(p, S, W.nwT, 1)
        S.junk = p.sb("junk", [128, D], BF16)
        S.ss = p.sb("ss", [128, 4], F32)
        S.rstd = p.sb("rstd", [128, 4], F32)
        S.xn = p.sb("xn", [128, 4, D], BF16, n=4)
        xres = p.sb("xres", [128, 4, D], F32, n=4)
        actT = p.sb("actT", [128, KC, 512], BF16, n=KC)
        S.wslots = [p.sb("wslot%d" % i, [128, KC, 512], BF16) for i in range(3)]
        S.wi = 0
        qk_sb = [p.sb("qsb", [128, NH, 512], BF16, n=NH), p.sb("ksb", [128, NH, 512], BF16, n=NH)]
        v_sb = p.sb("vsb", [128, 4, D], BF16, n=4)
        sqs = [p.sb("sq%d" % i, [128, 512], BF16) for i in range(2)]
        rr = [p.sb("rr%d" % i, [128, 512], F32) for i in range(2)]
        ones = p.sb("onesb", [128, 128], BF16)
        gcol = p.sb("gcol", [128, 2], F32)
        p.op("dve", lambda e: e.memset(ones[:], 1.0), writes=[ones.r()])
        p.dma("sp", gcol[:], W.gqk[:], gcol, writes=[gcol.r()])
        p.op("dve", lambda e: e.tensor_scalar(out=gcol[:, 0:1], in0=gcol[:, 0:1], scalar1=128.0 ** -0.5,
                                              scalar2=None, op0=ALU.mult),
             reads=[gcol.r()], writes=[gcol.r()])
        if tiles is None:
            tiles = [(0, t * 512, 4) for t in range(SEQ // 512)] + [(1, 0, 2)]
        cnt_box = [0]

        def do_tile(s, t0, nsub, newset):
            T = nsub * 128
            src = x1_d if s == 0 else xc1_d
            if newset:
                load_modset(p, S, modd, W.nwT, 1, s)
            p.dma("sp", xres[:, 0:nsub, :], src[t0:t0 + T, :].rearrange("(j p) d -> p j d", p=128),
                  xres, reads=[src.r()], writes=[xres.r(0, nsub)])
            norm_T(p, S, xres, nsub, S.A1c, S.B1c, actT)
            parts = (0, 1) if s == 0 else (1,)
            pend = None

            def finish(pd):
                (part, h, bankQ, sq, r) = pd
                bankS = p.bank()
                p.op("pe", lambda e: e.matmul(bankS[:, 0:T], lhsT=ones[:], rhs=sq[:, 0:T], start=True, stop=True),
                     reads=[ones.r(), sq.r()], writes=[bankS.r()])
                p.op("act", lambda e: e.activation(out=r[:, 0:T], in_=bankS[:, 0:T], func=AF.Sqrt,
                                                   scale=1.0 / 128, bias=EPS),
                     reads=[bankS.r()], writes=[r.r()])
                p.op("dve", lambda e: e.reciprocal(out=r[:, 0:T], in_=r[:, 0:T]), reads=[r.r()], writes=[r.r()])
                p.op("dve", lambda e: e.scalar_tensor_tensor(
                    out=qk_sb[part][:, h, 0:T], in0=bankQ[:, 0:T], scalar=gcol[:, part:part + 1],
                    in1=r[:, 0:T], op0=ALU.mult, op1=ALU.mult),
                    reads=[bankQ.r(), gcol.r(), r.r()], writes=[qk_sb[part].r(h, h + 1)])

            for part in parts:
                for hg in range(4):
                    slot = wload(p, S, wview(W.w_qkv, 0, KC, part * D + hg * 512), KC)
                    for hi in range(4):
                        h = hg * 4 + hi
                        bankQ = p.bank()
                        for kc in range(KC):
                            p.op("pe", lambda e, kc=kc, hi=hi, slot=slot, bankQ=bankQ: e.matmul(
                                bankQ[:, 0:T], lhsT=slot[:, kc, hi * 128:(hi + 1) * 128], rhs=actT[:, kc, 0:T],
                                start=(kc == 0), stop=(kc == KC - 1)),
                                reads=[slot.r(), actT.r(kc, kc + 1)], writes=[bankQ.r()])
                        sq = sqs[cnt_box[0] % 2]
                        r = rr[cnt_box[0] % 2]
                        cnt_box[0] += 1
                        p.op("act", lambda e, sq=sq, bankQ=bankQ: e.activation(out=sq[:, 0:T], in_=bankQ[:, 0:T],
                                                                               func=AF.Square),
                             reads=[bankQ.r()], writes=[sq.r()])
                        if pend is not None:
                            finish(pend)
                        pend = (part, h, bankQ, sq, r)
            finish(pend)
            for nt in range(4):
                slot = wload(p, S, wview(W.w_qkv, 0, KC, 2 * D + nt * 512), KC)
                for j in range(nsub):
                    bank = p.bank()
                    for kc in range(KC):
                        p.op("pe", lambda e, kc=kc, j=j, slot=slot, bank=bank: e.matmul(
                            bank[:], lhsT=actT[:, kc, j * 128:(j + 1) * 128], rhs=slot[:, kc, :],
                            start=(kc == 0), stop=(kc == KC - 1)),
                            reads=[slot.r(), actT.r(kc, kc + 1)], writes=[bank.r()])
                    p.op("act", lambda e, j=j, nt=nt, bank=bank: e.activation(
                        out=v_sb[:, j, nt * 512:(nt + 1) * 512], in_=bank[:], func=AF.Copy),
                        reads=[bank.r()], writes=[v_sb.r(j, j + 1)])
            kcol = t0 if s == 0 else SEQ + t0
            if s == 0:
                p.dma("sp", qT_d[:, :, t0:t0 + T].rearrange("h p t -> p h t"), qk_sb[0][:, :, 0:T],
                      qk_sb[0], reads=[qk_sb[0].r()], writes=[qT_d.r()])
            p.dma("sp", kT_d[:, :, kcol:kcol + T].rearrange("h p t -> p h t"), qk_sb[1][:, :, 0:T],
                  qk_sb[1], reads=[qk_sb[1].r()], writes=[kT_d.r()])
            p.dma("sp", v_d[kcol:kcol + T, :].rearrange("(j p) d -> p j d", p=128), v_sb[:, 0:nsub, :],
                  v_sb, reads=[v_sb.r(0, nsub)], writes=[v_d.r()])

        cur_set = None
        for (s_, t0_, nsub_) in tiles:
            do_tile(s_, t0_, nsub_, s_ != cur_set)
            cur_set = s_


def phase_l1b(p, W, qT_d, kT_d, v_d, OT_d, heads=None, groups=None):
    with p.phase():
        ones = p.sb("onesb", [128, 128], BF16)
        p.op("dve", lambda e: e.memset(ones[:], 1.0), writes=[ones.r()])
        NB = 2
        qh = [p.sb("qh%d" % i, [128, SEQ], BF16) for i in range(NB)]
        kh = [p.sb("kh%d" % i, [128, NKEY], BF16) for i in range(NB)]
        vh = [p.sb("vh%d" % i, [128, NKEY // 128, 128], BF16) for i in range(NB)]
        tb = [p.sb("tb%d" % i, [128, 20, 512], F32) for i in range(NB)]
        OTs = [p.sb("OTs%d" % i, [128, SEQ], BF16) for i in range(NB)]
        pTs = [p.sb("pT%d" % i, [128, 512], BF16) for i in range(4)]
        tmps = [p.sb("tmpa%d" % i, [128, 512], F32) for i in range(3)]
        rDs = [p.sb("rD%d" % i, [128, 512], F32) for i in range(2)]
        od_pool = BankPool(p, [0, 1, 2, 3])
        s_pool = BankPool(p, [4, 5, 6, 7])
        ci = 0
        if heads is None:
            heads = list(range(NH))
        if groups is None:
            groups = list(range(8))
        ci_box = [0]

        def do_head(hn, h):
            b_ = hn % NB
            q_, k_, v_, t_, O_ = qh[b_], kh[b_], vh[b_], tb[b_], OTs[b_]
            p.dma("sp", q_[:], qT_d[h], q_, reads=[qT_d.r()], writes=[q_.r()])
            p.dma("sp", k_[:], kT_d[h], k_, reads=[kT_d.r()], writes=[k_.r()])
            p.dma("sp", v_[:], v_d[:, h * 128:(h + 1) * 128].rearrange("(kt p) c -> p kt c", p=128), v_,
                  reads=[v_d.r()], writes=[v_.r()])
            p.dma("sp", t_[:], W.tbl[h].rearrange("v p q -> p v q"), t_, writes=[t_.r()])
            def do_group(g):
                tl = [l1_variant(g, j) for j in range(l1_ntiles(g))] + [(32, None), (33, None)]
                bankO = od_pool.next()
                bankD = od_pool.next()
                pend = None
                n = len(tl)

                def pv(pd):
                    (idx, kt, pT) = pd
                    p.op("pe", lambda e: e.matmul(bankO[:], lhsT=v_[:, kt, :], rhs=pT[:], start=(idx == 0),
                                                  stop=(idx == n - 1)),
                         reads=[v_.r(), pT.r()], writes=[bankO.r()])
                    p.op("pe", lambda e: e.matmul(bankD[:], lhsT=ones[:], rhs=pT[:], start=(idx == 0),
                                                  stop=(idx == n - 1)),
                         reads=[ones.r(), pT.r()], writes=[bankD.r()])

                for idx, (kt, var) in enumerate(tl):
                    bankS = s_pool.next()
                    p.op("pe", lambda e, kt=kt, bankS=bankS: e.matmul(
                        bankS[:], lhsT=k_[:, kt * 128:(kt + 1) * 128], rhs=q_[:, g * 512:(g + 1) * 512],
                        start=True, stop=True),
                        reads=[k_.r(), q_.r()], writes=[bankS.r()])
                    ci = ci_box[0]
                    pT = pTs[ci % 4]
                    if var is not None:
                        tmp = tmps[ci % 3]
                        p.op("dve", lambda e, var=var, bankS=bankS, tmp=tmp: e.tensor_tensor(
                            out=tmp[:], in0=bankS[:], in1=t_[:, var, :], op=ALU.add),
                            reads=[bankS.r(), t_.r()], writes=[tmp.r()])
                        p.op("act", lambda e, tmp=tmp, pT=pT: e.activation(out=pT[:], in_=tmp[:], func=AF.Exp),
                             reads=[tmp.r()], writes=[pT.r()])
                    else:
                        p.op("act", lambda e, bankS=bankS, pT=pT: e.activation(out=pT[:], in_=bankS[:], func=AF.Exp),
                             reads=[bankS.r()], writes=[pT.r()])
                    ci_box[0] += 1
                    if pend is not None:
                        pv(pend)
                    pend = (idx, kt, pT)
                pv(pend)
                rD = rDs[ci_box[0] % 2]
                p.op("dve", lambda e, rD=rD: e.reciprocal(out=rD[:], in_=bankD[:]), reads=[bankD.r()], writes=[rD.r()])
                p.op("dve", lambda e, rD=rD, g=g: e.tensor_tensor(out=O_[:, g * 512:(g + 1) * 512], in0=bankO[:],
                                                                   in1=rD[:], op=ALU.mult),
                     reads=[bankO.r(), rD.r()], writes=[O_.r()])
            for g_ in groups:
                do_group(g_)
            p.dma("sp", OT_d[h], O_[:], O_, reads=[O_.r()], writes=[OT_d.r()])

        for hn_, h_ in enumerate(heads):
            do_head(hn_, h_)


def phase_l1c(p, W, x1_d, OT_d, x2_d, toks_d, rt_d, modd, ntiles=None):
    with p.phase():
        S = NS()
        xres = p.sb("xres", [128, 4, D], F32, n=4)
        OTt = p.sb("OTt", [128, NH, 512], BF16)
        S.wslots = [p.sb("wslot%d" % i, [128, KC, 512], BF16) for i in range(2)]
        S.wi = 0
        S.tmps = [p.sb("tmp%d" % i, [128, 512], F32) for i in range(2)]
        S.ti = 0
        G1b = p.sb("G1b", [128, D], F32)
        A2b = p.sb("A2b", [128, D], F32)
        B2b = p.sb("B2b", [128, D], F32)
        nwb = p.sb("nwb", [128, D], BF16)
        wrb = p.sb("wrb", [128, 8, D], F32)
        tk = [p.sb("tk%d" % i, [128, D], F32) for i in range(2)]
        ss = p.sb("ss", [128, 4], F32)
        rstd = p.sb("rstd", [128, 4], F32)
        lg = p.sb("lg", [128, 8], F32)
        srt = p.sb("srt", [128, 8], F32)
        dl = p.sb("dl", [128, 1], F32)
        rt = p.sb("rt", [128, 4, 18], F32)
        m = modd.t
        p.dma("sp", G1b[:], m[1, 0:1, 2 * D:3 * D].partition_broadcast(128), G1b, reads=[modd.r()], writes=[G1b.r()])
        p.dma("sp", B2b[:], m[1, 0:1, 3 * D:4 * D].partition_broadcast(128), B2b, reads=[modd.r()], writes=[B2b.r()])
        p.dma("sp", A2b[:], m[1, 0:1, 4 * D:5 * D].partition_broadcast(128), A2b, reads=[modd.r()], writes=[A2b.r()])
        p.dma("sp", tk[0][:], W.nwrow[1:2, :].partition_broadcast(128), tk[0], writes=[tk[0].r()])
        p.op("dve", lambda e: e.scalar_tensor_tensor(out=A2b[:], in0=A2b[:], scalar=1.0, in1=tk[0][:],
                                                     op0=ALU.add, op1=ALU.mult),
             reads=[A2b.r(), tk[0].r()], writes=[A2b.r()])
        p.dma("sp", wrb[:].rearrange("p e d -> p (e d)"),
              W.wrT[:].rearrange("(o e) d -> o (e d)", o=1).partition_broadcast(128), wrb, writes=[wrb.r()])
        if ntiles is None:
            ntiles = list(range(SEQ // 512))
        ti_box = [0]

        def do_tile(t):
            t0 = t * 512
            p.dma("sp", xres[:], x1_d[t0:t0 + 512, :].rearrange("(j p) d -> p j d", p=128),
                  xres, reads=[x1_d.r()], writes=[xres.r()])
            p.dma("sp", OTt[:], OT_d[:, :, t0:t0 + 512].rearrange("h p t -> p h t"), OTt,
                  reads=[OT_d.r()], writes=[OTt.r()])
            for nt in range(4):
                slot = wload(p, S, wview(W.w_o, 0, KC, nt * 512), KC)
                for j in range(4):
                    bank = p.bank()
                    for h in range(NH):
                        p.op("pe", lambda e, h=h, j=j, slot=slot, bank=bank: e.matmul(
                            bank[:], lhsT=OTt[:, h, j * 128:(j + 1) * 128], rhs=slot[:, h, :],
                            start=(h == 0), stop=(h == NH - 1)),
                            reads=[slot.r(), OTt.r()], writes=[bank.r()])
                    resid_update(p, S, bank, xres, j, nt, G1b)
            p.dma("sp", x2_d[t0:t0 + 512, :].rearrange("(j p) d -> p j d", p=128), xres[:], xres,
                  reads=[xres.r()], writes=[x2_d.r()])
            for j in range(4):
                p.op("act", lambda e, j=j: e.activation(out=nwb[:], in_=xres[:, j, :], func=AF.Square,
                                                        accum_out=ss[:, j:j + 1]),
                     reads=[xres.r(j, j + 1)], writes=[ss.r()])
            p.op("act", lambda e: e.activation(out=rstd[:], in_=ss[:], func=AF.Sqrt, scale=1.0 / D, bias=EPS),
                 reads=[ss.r()], writes=[rstd.r()])
            p.op("dve", lambda e: e.reciprocal(out=rstd[:], in_=rstd[:]), reads=[rstd.r()], writes=[rstd.r()])
            for j in range(4):
                tkb = tk[ti_box[0] % 2]
                ti_box[0] += 1
                p.op("dve", lambda e, j=j, tkb=tkb: e.scalar_tensor_tensor(
                    out=tkb[:], in0=xres[:, j, :], scalar=rstd[:, j:j + 1], in1=A2b[:], op0=ALU.mult, op1=ALU.mult),
                    reads=[xres.r(j, j + 1), rstd.r(), A2b.r()], writes=[tkb.r()])
                p.op("dve", lambda e, tkb=tkb: e.tensor_tensor(out=tkb[:], in0=tkb[:], in1=B2b[:], op=ALU.add),
                     reads=[tkb.r(), B2b.r()], writes=[tkb.r()])
                p.dma("pool", toks_d[t0 + j * 128:t0 + (j + 1) * 128, :], tkb[:], tkb, reads=[tkb.r()],
                      writes=[toks_d.r()])
                for e_ in range(8):
                    p.op("dve", lambda e, e_=e_, tkb=tkb: e.scalar_tensor_tensor(
                        out=nwb[:], in0=tkb[:], scalar=1.0, in1=wrb[:, e_, :], op0=ALU.mult, op1=ALU.mult,
                        accum_out=lg[:, e_:e_ + 1]),
                        reads=[tkb.r(), wrb.r()], writes=[lg.r()])
                p.op("dve", lambda e: e.max(out=srt[:], in_=lg[:]), reads=[lg.r()], writes=[srt.r()])
                p.op("dve", lambda e, j=j: e.tensor_scalar(out=rt[:, j, 0:8], in0=lg[:], scalar1=srt[:, 0:1],
                                                           scalar2=None, op0=ALU.is_equal),
                     reads=[lg.r(), srt.r()], writes=[rt.r()])
                p.op("dve", lambda e, j=j: e.tensor_scalar(out=rt[:, j, 8:16], in0=lg[:], scalar1=srt[:, 1:2],
                                                           scalar2=None, op0=ALU.is_equal),
                     reads=[lg.r(), srt.r()], writes=[rt.r()])
                p.op("dve", lambda e: e.tensor_tensor(out=dl[:], in0=srt[:, 0:1], in1=srt[:, 1:2], op=ALU.subtract),
                     reads=[srt.r()], writes=[dl.r()])
                p.op("act", lambda e, j=j: e.activation(out=rt[:, j, 16:17], in_=dl[:], func=AF.Sigmoid),
                     reads=[dl.r()], writes=[rt.r()])
                p.op("act", lambda e, j=j: e.activation(out=rt[:, j, 17:18], in_=dl[:], func=AF.Sigmoid, scale=-1.0),
                     reads=[dl.r()], writes=[rt.r()])
            p.dma("sp", rt_d[t0:t0 + 512, :].rearrange("(j p) c -> p j c", p=128), rt[:], rt,
                  reads=[rt.r()], writes=[rt_d.r()])

        for t_ in ntiles:
            do_tile(t_)


def declare_l1_weights(p, kind="ExternalInput"):
    W = NS()
    W.nwT = p.dr("nwT1", [2, 128, 2, KC], F32, kind=kind)
    W.nwrow = p.dr("nwrow", [2, D], F32, kind=kind)
    W.w_qkv = p.dr("na_w_qkv", [D, 3 * D], F32, kind=kind)
    W.gqk = p.dr("na_gqk", [128, 2], F32, kind=kind)
    W.tbl = p.dr("na_tbl", [NH, 20, 128, 512], F32, kind=kind)
    W.w_o = p.dr("na_w_o", [D, D], F32, kind=kind)
    W.wrT = p.dr("moe_wrT", [8, D], F32, kind=kind)
    return W


def host_l1_weights(inp):
    nw = inp["norm_w"]
    nwT = np.ascontiguousarray(nw.reshape(2, 2, KC, 128).transpose(0, 3, 1, 2))
    return dict(
        nwT1=nwT,
        nwrow=np.ascontiguousarray(nw[1]),
        na_w_qkv=np.ascontiguousarray(inp["na_w_qkv"][0]),
        na_gqk=np.ascontiguousarray(np.stack([inp["na_g_q"][0], inp["na_g_k"][0]], axis=-1)),
        na_tbl=host_bias_table(inp["na_rpb"][0]),
        na_w_o=np.ascontiguousarray(inp["na_w_o"][0]),
        moe_wrT=np.ascontiguousarray(inp["moe_w_router"][0].T),
    )


NE = 8
MOE = 7168
BS = 512
NBLK = SEQ * 2 // BS + NE
NSLOT = NBLK * BS
NM = 56
IOA = bass.IndirectOffsetOnAxis


def phase_moe(p, W, toks_d, rt_d, ys_d, stab_d, idx_d, nblk=NBLK):
    with p.phase():
        ident = make_ident(p)
        ones = p.sb("onesb", [128, 128], BF16)
        p.op("dve", lambda e: e.memset(ones[:], 1.0), writes=[ones.r()])
        rt_all = p.sb("rt_all", [128, 32, 18], F32)
        p.dma("sp", rt_all[:], rt_d[:, :].rearrange("(i p) c -> p i c", p=128), rt_all,
              reads=[rt_d.r()], writes=[rt_all.r()])
        Mb = p.sb("Mb", [128, 32, 8], BF16)
        p.op("dve", lambda e: e.tensor_tensor(out=Mb[:], in0=rt_all[:, :, 0:8], in1=rt_all[:, :, 8:16], op=ALU.add),
             reads=[rt_all.r()], writes=[Mb.r()])
        Ls = p.sb("Ls", [128, 128], BF16)
        p.op("pool", lambda e: e.memset(Ls[:], 0.0), writes=[Ls.r()])
        p.op("pool", lambda e: e.affine_select(out=Ls[:], in_=Ls[:], pattern=[[-1, 128]], compare_op=ALU.is_ge,
                                               fill=1.0, base=0, channel_multiplier=1),
             reads=[Ls.r()], writes=[Ls.r()])
        bankC = p.banks[0]
        bankW = p.banks[1]
        p.op("pe", lambda e: e.matmul(bankC[:, 0:256], lhsT=ones[:], rhs=Mb[:].rearrange("p i e -> p (i e)"),
                                      start=True, stop=True),
             reads=[ones.r(), Mb.r()], writes=[bankC.r()])
        for i in range(32):
            p.op("pe", lambda e, i=i: e.matmul(bankW[:, i * 8:(i + 1) * 8], lhsT=Ls[:], rhs=Mb[:, i, :],
                                               start=True, stop=True),
                 reads=[Ls.r(), Mb.r()], writes=[bankW.r()])
        cs = p.sb("cs", [128, 32, 8], F32)
        off = p.sb("off", [128, 32, 8], F32)
        p.op("act", lambda e: e.activation(out=cs[:].rearrange("p i e -> p (i e)"), in_=bankC[:, 0:256], func=AF.Copy),
             reads=[bankC.r()], writes=[cs.r()])
        p.op("dve", lambda e: e.memset(off[:, 0, :], 0.0), writes=[off.r()])
        for i in range(1, 32):
            p.op("dve", lambda e, i=i: e.tensor_tensor(out=off[:, i, :], in0=off[:, i - 1, :], in1=cs[:, i - 1, :],
                                                       op=ALU.add),
                 reads=[off.r(), cs.r()], writes=[off.r()])
        cnt = p.sb("cnt", [128, 8], F32)
        p.op("dve", lambda e: e.tensor_tensor(out=cnt[:], in0=off[:, 31, :], in1=cs[:, 31, :], op=ALU.add),
             reads=[off.r(), cs.r()], writes=[cnt.r()])
        io8 = p.sb("io8", [128, 8], I32)
        thr = p.sb("thr", [128, 8], F32)
        p.op("pool", lambda e: e.iota(io8[:], pattern=[[BS, 8]], base=0, channel_multiplier=0), writes=[io8.r()])
        p.op("dve", lambda e: e.tensor_copy(out=thr[:], in_=io8[:]), reads=[io8.r()], writes=[thr.r()])
        cmp = p.sb("cmp", [128, 8, 8], F32)
        p.op("dve", lambda e: e.tensor_tensor(out=cmp[:], in0=cnt[:].unsqueeze(2).to_broadcast([128, 8, 8]),
                                              in1=thr[:].unsqueeze(1).to_broadcast([128, 8, 8]), op=ALU.is_gt),
             reads=[cnt.r(), thr.r()], writes=[cmp.r()])
        pad = p.sb("pad", [128, 8], F32)
        p.op("dve", lambda e: e.tensor_reduce(out=pad[:], in_=cmp[:], axis=AX.X, op=ALU.add),
             reads=[cmp.r()], writes=[pad.r()])
        p.op("dve", lambda e: e.tensor_scalar(out=pad[:], in0=pad[:], scalar1=float(BS), scalar2=None, op0=ALU.mult),
             reads=[pad.r()], writes=[pad.r()])
        pst = p.sb("pst", [128, 8], F32)
        pen = p.sb("pen", [128, 8], F32)
        p.op("dve", lambda e: e.memset(pst[:, 0:1], 0.0), writes=[pst.r()])
        for e_ in range(1, 8):
            p.op("dve", lambda e, e_=e_: e.tensor_tensor(out=pst[:, e_:e_ + 1], in0=pst[:, e_ - 1:e_],
                                                         in1=pad[:, e_ - 1:e_], op=ALU.add),
                 reads=[pst.r(), pad.r()], writes=[pst.r()])
        p.op("dve", lambda e: e.tensor_tensor(out=pen[:], in0=pst[:], in1=pad[:], op=ALU.add),
             reads=[pst.r(), pad.r()], writes=[pen.r()])
        iob = p.sb("iob", [128, NBLK], I32)
        bthr = p.sb("bthr", [128, NBLK], F32)
        p.op("pool", lambda e: e.iota(iob[:], pattern=[[BS, NBLK]], base=0, channel_multiplier=0), writes=[iob.r()])
        p.op("dve", lambda e: e.tensor_copy(out=bthr[:], in_=iob[:]), reads=[iob.r()], writes=[bthr.r()])
        cmp2 = p.sb("cmp2", [128, NBLK, 8], F32)
        p.op("dve", lambda e: e.tensor_tensor(out=cmp2[:], in0=pen[:].unsqueeze(1).to_broadcast([128, NBLK, 8]),
                                              in1=bthr[:].unsqueeze(2).to_broadcast([128, NBLK, 8]), op=ALU.is_le),
             reads=[pen.r(), bthr.r()], writes=[cmp2.r()])
        be = p.sb("be", [128, NBLK], F32)
        p.op("dve", lambda e: e.tensor_reduce(out=be[:], in_=cmp2[:], axis=AX.X, op=ALU.add),
             reads=[cmp2.r()], writes=[be.r()])
        p.op("dve", lambda e: e.tensor_scalar(out=be[:], in0=be[:], scalar1=float(NE - 1), scalar2=float(MOE),
                                              op0=ALU.min, op1=ALU.mult),
             reads=[be.r()], writes=[be.r()])
        slotf = p.sb("slotf", [128, 32, 8], F32)
        p.op("dve", lambda e: e.tensor_tensor(out=slotf[:].rearrange("p i e -> p (i e)"), in0=bankW[:, 0:256],
                                              in1=off[:].rearrange("p i e -> p (i e)"), op=ALU.add),
             reads=[bankW.r(), off.r()], writes=[slotf.r()])
        p.op("dve", lambda e: e.tensor_tensor(out=slotf[:], in0=slotf[:],
                                              in1=pst[:].unsqueeze(1).to_broadcast([128, 32, 8]), op=ALU.add),
             reads=[slotf.r(), pst.r()], writes=[slotf.r()])
        tmp3 = p.sb("tmp3", [128, 32, 8], F32)
        slab = p.sb("slab", [128, 64], F32)
        idxab = p.sb("idxab", [128, 64], I32)
        for k in range(2):
            p.op("dve", lambda e, k=k: e.tensor_tensor(out=tmp3[:], in0=rt_all[:, :, 8 * k:8 * k + 8], in1=slotf[:],
                                                       op=ALU.mult),
                 reads=[rt_all.r(), slotf.r()], writes=[tmp3.r()])
            p.op("dve", lambda e, k=k: e.tensor_reduce(out=slab[:, 32 * k:32 * k + 32], in_=tmp3[:], axis=AX.X,
                                                       op=ALU.add),
                 reads=[tmp3.r()], writes=[slab.r()])
        p.op("dve", lambda e: e.tensor_copy(out=idxab[:], in_=slab[:]), reads=[slab.r()], writes=[idxab.r()])
        p.dma("sp", idx_d[:, :], idxab[:], idxab, reads=[idxab.r()], writes=[idx_d.r()])
        tokid = p.sb("tokid", [128, 32], I32)
        p.op("pool", lambda e: e.iota(tokid[:], pattern=[[128, 32]], base=0, channel_multiplier=1), writes=[tokid.r()])
        rows = p.sb("rows", [128, 64, 2], I32)
        for k in range(2):
            p.op("dve", lambda e, k=k: e.tensor_copy(out=rows[:, 32 * k:32 * k + 32, 0:1], in_=tokid[:].unsqueeze(2)),
                 reads=[tokid.r()], writes=[rows.r()])
            p.op("dve", lambda e, k=k: e.tensor_copy(out=rows[:, 32 * k:32 * k + 32, 1:2].bitcast(F32),
                                                     in_=rt_all[:, :, 16 + k:17 + k]),
                 reads=[rt_all.r()], writes=[rows.r()])
        zer = p.sb("zer", [128, NSLOT // 128 * 2], I32)
        p.op("dve", lambda e: e.memset(zer[:], 0), writes=[zer.r()])
        p.dma("sp", stab_d[:, :].rearrange("(p a) c -> p (a c)", p=128), zer[:], zer, reads=[zer.r()],
              writes=[stab_d.r()])
        for i in range(64):
            p.op("pool", lambda e, i=i: e.indirect_dma_start(
                out=stab_d[:, :], out_offset=IOA(ap=idxab[:, i:i + 1], axis=0), in_=rows[:, i, :], in_offset=None),
                reads=[idxab.r(), rows.r()], writes=[stab_d.r()], dma=rows)
        wi_i = p.sb("wi_i", [128, NM], I32)
        wi_f = p.sb("wi_f", [128, NM], F32)
        p.op("pool", lambda e: e.iota(wi_i[:], pattern=[[128, NM]], base=0, channel_multiplier=1), writes=[wi_i.r()])
        p.op("dve", lambda e: e.tensor_copy(out=wi_f[:], in_=wi_i[:]), reads=[wi_i.r()], writes=[wi_f.r()])
        widx_f = p.sb("widx_f", [128, NBLK, NM], F32)
        widx = p.sb("widx", [128, NBLK, NM], I32)
        p.op("dve", lambda e: e.tensor_tensor(out=widx_f[:], in0=wi_f[:].unsqueeze(1).to_broadcast([128, NBLK, NM]),
                                              in1=be[:].unsqueeze(2).to_broadcast([128, NBLK, NM]), op=ALU.add),
             reads=[wi_f.r(), be.r()], writes=[widx_f.r()])
        p.op("dve", lambda e: e.tensor_copy(out=widx[:], in_=widx_f[:]), reads=[widx_f.r()], writes=[widx.r()])

        sidxs = [p.sb("sidx%d" % i, [128, 4, 2], I32) for i in range(2)]
        xs_tok = p.sb("xs_tok", [128, 4, D], BF16)
        xsT = p.sb("xsT", [128, KC, BS], BF16, n=KC)
        hT = p.sb("hT", [128, NM, BS], BF16, n=NM)
        wsl = [p.sb("mslot%d" % i, [128, KC, 512], BF16, n=4) for i in range(3)]
        ys_sb = p.sb("ys_sb", [128, 4, D], F32, n=4)
        tmps = [p.sb("mtmp%d" % i, [128, BS], BF16) for i in range(4)]
        st = NS()
        st.wi = 0
        st.ti = 0

        def wl(src_d, b, m, nq):
            slot = wsl[st.wi % len(wsl)]
            st.wi += 1
            for kq in range(nq):
                p.op("pool", lambda e, kq=kq, slot=slot: e.indirect_dma_start(
                    out=slot[:, 4 * kq:4 * kq + 4, :].rearrange("p a b -> p (a b)"), out_offset=None,
                    in_=src_d[:, :], in_offset=IOA(ap=widx[:, b, m + kq:m + kq + 1], axis=0)),
                    reads=[widx.r()], writes=[slot.r(kq, kq + 1)], dma=slot)
            return slot

        def do_block(b):
            sidx = sidxs[b % 2]
            p.dma("sp", sidx[:], stab_d[b * BS:(b + 1) * BS, :].rearrange("(j p) c -> p j c", p=128), sidx,
                  reads=[stab_d.r()], writes=[sidx.r()])
            for j in range(4):
                p.op("pool", lambda e, j=j: e.indirect_dma_start(
                    out=xs_tok[:, j, :], out_offset=None, in_=toks_d[:, :],
                    in_offset=IOA(ap=sidx[:, j, 0:1], axis=0)),
                    reads=[sidx.r(), toks_d.r()], writes=[xs_tok.r()], dma=xs_tok)
            for kc in range(KC):
                bank = p.bank()
                bb = bank[:].bitcast(BF16)
                for j in range(4):
                    p.op("pe", lambda e, j=j, kc=kc, bb=bb: e.transpose(
                        out=bb[:, j * 128:(j + 1) * 128], in_=xs_tok[:, j, kc * 128:(kc + 1) * 128], identity=ident[:]),
                        reads=[xs_tok.r(), ident.r()], writes=[bank.r()])
                p.op("act", lambda e, kc=kc, bb=bb: e.activation(out=xsT[:, kc, :], in_=bb[:, 0:BS], func=AF.Copy),
                     reads=[bank.r()], writes=[xsT.r(kc, kc + 1)])
            for fg in range(14):
                s1 = wl(W.w1r, b, fg * 4, 4)
                s3 = wl(W.w3r, b, fg * 4, 4)
                bAs = [p.bank() for _ in range(4)]
                bBs = [p.bank() for _ in range(4)]
                stmp = [None] * 4
                for (bks_, sl) in ((bAs, s1), (bBs, s3)):
                    for fi in range(4):
                        bk = bks_[fi]
                        for kc in range(KC):
                            p.op("pe", lambda e, kc=kc, fi=fi, sl=sl, bk=bk: e.matmul(
                                bk[:], lhsT=sl[:, kc, fi * 128:(fi + 1) * 128], rhs=xsT[:, kc, :],
                                start=(kc == 0), stop=(kc == KC - 1)),
                                reads=[sl.r(), xsT.r(kc, kc + 1)], writes=[bk.r()])
                        if bks_ is bAs:
                            tmp = tmps[st.ti % 4]
                            st.ti += 1
                            p.op("act", lambda e, bk=bk, tmp=tmp: e.activation(out=tmp[:], in_=bk[:], func=AF.Silu),
                                 reads=[bk.r()], writes=[tmp.r()])
                            stmp[fi] = tmp
                        else:
                            ft = fg * 4 + fi
                            tmp = stmp[fi]
                            p.op("dve", lambda e, bk=bk, tmp=tmp, ft=ft: e.tensor_tensor(
                                out=hT[:, ft, :], in0=bk[:], in1=tmp[:], op=ALU.mult),
                                reads=[bk.r(), tmp.r()], writes=[hT.r(ft, ft + 1)])
            for nt in range(4):
                bks = [p.bank() for _ in range(4)]
                for (jg0, nq) in ((0, 4), (4, 4), (8, 4), (12, 2)):
                    slot = wl(W.w2r, b, nt * 14 + jg0, nq)
                    for j in range(4):
                        for q in range(nq):
                            for i in range(4):
                                jt = (jg0 + q) * 4 + i
                                p.op("pe", lambda e, jt=jt, i=i, q=q, j=j, slot=slot, bk=bks[j]: e.matmul(
                                    bk[:], lhsT=hT[:, jt, j * 128:(j + 1) * 128], rhs=slot[:, 4 * q + i, :],
                                    start=(jt == 0), stop=(jt == NM - 1)),
                                    reads=[slot.r(q, q + 1), hT.r(jt, jt + 1)], writes=[bks[j].r()])
                for j in range(4):
                    p.op("dve", lambda e, j=j, nt=nt, bk=bks[j]: e.tensor_scalar(
                        out=ys_sb[:, j, nt * 512:(nt + 1) * 512], in0=bk[:],
                        scalar1=sidx[:, j, 1:2].bitcast(F32), scalar2=None, op0=ALU.mult),
                        reads=[bks[j].r(), sidx.r()], writes=[ys_sb.r(j, j + 1)])
            p.dma("sp", ys_d[b * BS:(b + 1) * BS, :].rearrange("(j p) d -> p j d", p=128), ys_sb[:], ys_sb,
                  reads=[ys_sb.r()], writes=[ys_d.r()])

        for b_ in range(nblk):
            do_block(b_)


def phase_combine(p, x2_d, ys_d, idx_d, out_d, modd, ntiles=32):
    with p.phase():
        G2b = p.sb("G2b", [128, D], F32)
        idx = p.sb("idx", [128, 64], I32)
        p.dma("sp", G2b[:], modd.t[1, 0:1, 5 * D:6 * D].partition_broadcast(128), G2b, reads=[modd.r()],
              writes=[G2b.r()])
        p.dma("sp", idx[:], idx_d[:, :], idx, reads=[idx_d.r()], writes=[idx.r()])
        xo = [p.sb("xo%d" % i, [128, D], F32) for i in range(2)]
        yA = [p.sb("yA%d" % i, [128, D], F32) for i in range(2)]
        yB = [p.sb("yB%d" % i, [128, D], F32) for i in range(2)]

        def do_tile(i):
            x_, a_, b_ = xo[i % 2], yA[i % 2], yB[i % 2]
            p.dma("sp", x_[:], x2_d[i * 128:(i + 1) * 128, :], x_, reads=[x2_d.r()], writes=[x_.r()])
            p.op("pool", lambda e: e.indirect_dma_start(out=a_[:], out_offset=None, in_=ys_d[:, :],
                                                        in_offset=IOA(ap=idx[:, i:i + 1], axis=0)),
                 reads=[idx.r(), ys_d.r()], writes=[a_.r()], dma=a_)
            p.op("pool", lambda e: e.indirect_dma_start(out=b_[:], out_offset=None, in_=ys_d[:, :],
                                                        in_offset=IOA(ap=idx[:, 32 + i:33 + i], axis=0)),
                 reads=[idx.r(), ys_d.r()], writes=[b_.r()], dma=b_)
            p.op("dve", lambda e: e.tensor_tensor(out=a_[:], in0=a_[:], in1=b_[:], op=ALU.add),
                 reads=[a_.r(), b_.r()], writes=[a_.r()])
            p.op("dve", lambda e: e.tensor_tensor(out=a_[:], in0=a_[:], in1=G2b[:], op=ALU.mult),
                 reads=[a_.r(), G2b.r()], writes=[a_.r()])
            p.op("dve", lambda e: e.tensor_tensor(out=x_[:], in0=x_[:], in1=a_[:], op=ALU.add),
                 reads=[a_.r(), x_.r()], writes=[x_.r()])
            p.dma("sp", out_d[i * 128:(i + 1) * 128, :], x_[:], x_, reads=[x_.r()], writes=[out_d.r()])

        for i_ in range(ntiles):
            do_tile(i_)


def declare_moe_weights(p, kind="ExternalInput"):
    W = NS()
    W.w1r = p.dr("moe_w1r", [NE * MOE, D], F32, kind=kind)
    W.w3r = p.dr("moe_w3r", [NE * MOE, D], F32, kind=kind)
    W.w2r = p.dr("moe_w2r", [NE * MOE, D], F32, kind=kind)
    return W


def host_moe_weights(inp):
    def r13(w):
        return np.ascontiguousarray(
            w.reshape(NE, 4, 4, 128, 14, 512).transpose(0, 4, 1, 3, 2, 5)).reshape(NE * MOE, D)

    def r2(w):
        return np.ascontiguousarray(
            w.reshape(NE, 14, 4, 128, 4, 512).transpose(0, 4, 1, 3, 2, 5)).reshape(NE * MOE, D)

    return dict(moe_w1r=r13(inp["moe_w1"][0]), moe_w3r=r13(inp["moe_w3"][0]), moe_w2r=r2(inp["moe_w2"][0]))


NB_PER_CORE = 1
N_CORES = 8


def _view(p, buf, bi, name):
    b = Buf(name, buf.t[bi], 1)
    p.bufs.append(b)
    return b


def build_full(nb=NB_PER_CORE):
    nc = bass.Bass("TRN2", target_bir_lowering=False)
    p = Prog(nc)
    p.init_psum()
    cT_a = p.dr("cT", [nb, 128, 32], F32, kind="ExternalInput")
    w_ada_d = p.dr("w_ada", [2 * D, NMOD * D], F32, kind="ExternalInput")
    b_ada_d = p.dr("b_ada", [2, NMOD * D], F32, kind="ExternalInput")
    x_a = p.dr("x", [nb, SEQ, D], F32, kind="ExternalInput")
    ctx_a = p.dr("ctx", [nb, CTX, D], F32, kind="ExternalInput")
    W0 = declare_l0_weights(p)
    W1 = declare_l1_weights(p)
    WM = declare_moe_weights(p)
    out_a = p.dr("out", [nb, SEQ, D], F32, kind="ExternalOutput")
    modd = p.dr("modd", [2, 2, NMOD * D], F32)
    x1_d = p.dr("x1", [SEQ, D], F32)
    xc1_d = p.dr("xc1", [CTX, D], F32)
    qT_d = p.dr("qT", [NH, 128, SEQ], BF16)
    kT_d = p.dr("kT", [NH, 128, NKEY], BF16)
    v_d = p.dr("v", [NKEY, D], BF16)
    OT_d = p.dr("OT", [NH, 128, SEQ], BF16)
    x2_d = p.dr("x2", [SEQ, D], F32)
    toks_d = p.dr("toks", [SEQ, D], BF16)
    rt_d = p.dr("rt", [SEQ, 18], F32)
    ys_d = p.dr("ys", [NSLOT, D], F32)
    stab_d = p.dr("stab", [NSLOT, 2], I32)
    idx_d = p.dr("idx", [128, 64], I32)
    for bi in range(nb):
        cT_d = _view(p, cT_a, bi, "cT%d" % bi)
        x_d = _view(p, x_a, bi, "x%d" % bi)
        ctx_d = _view(p, ctx_a, bi, "ctx%d" % bi)
        out_d = _view(p, out_a, bi, "out%d" % bi)
        phase_adaln(p, cT_d, w_ada_d, b_ada_d, modd)
        phase_l0(p, W0, x_d, ctx_d, x1_d, xc1_d, modd)
        phase_l1a(p, W1, x1_d, xc1_d, qT_d, kT_d, v_d, modd)
        phase_l1b(p, W1, qT_d, kT_d, v_d, OT_d)
        phase_l1c(p, W1, x1_d, OT_d, x2_d, toks_d, rt_d, modd)
        phase_moe(p, WM, toks_d, rt_d, ys_d, stab_d, idx_d)
        phase_combine(p, x2_d, ys_d, idx_d, out_d, modd)
    p.emit()
    return nc


def kernel(**inp):
    inp = {k: np.asarray(v) for k, v in inp.items()}
    nb = NB_PER_CORE
    nc = build_full(nb)
    shared = dict(w_ada=np.ascontiguousarray(inp["w_ada"].reshape(2 * D, NMOD * D)),
                  b_ada=np.ascontiguousarray(inp["b_ada"]))
    shared.update(host_l0_weights(inp))
    shared.update(host_l1_weights(inp))
    shared.update(host_moe_weights(inp))
    maps = []
    for c in range(N_CORES):
        m = dict(shared)
        bs = list(range(c * nb, (c + 1) * nb))
        m["cT"] = np.stack([host_cT(inp, b) for b in bs], axis=0)
        m["x"] = np.ascontiguousarray(inp["x"][bs[0]:bs[-1] + 1])
        m["ctx"] = np.ascontiguousarray(inp["ctx"][bs[0]:bs[-1] + 1])
        maps.append(m)
    res = run_bass_kernel_spmd(nc, maps, core_ids=list(range(N_CORES)))
    out = np.concatenate([np.asarray(res.results[c]["out"]) for c in range(N_CORES)], axis=0)
    return out.astype(np.float32)
```
